# Optimizing a Trainium2 kernel written in Bass

```python
import math
import jax, jax.numpy as jnp
from jax import lax
import numpy as np

D_MODEL = 1024
BATCH = 4
SEQ = 8192
DEPTH = 1

SSM_WIDTH = 512
SSM_GROUP = 16
SSM_GROUPS = SSM_WIDTH // SSM_GROUP
SSM_STATE = 64
DT_MIN = 1e-3
DT_MAX = 1e-1
N_HEADS = 8
HEAD_DIM = 64
ATTN_WIDTH = N_HEADS * HEAD_DIM
MOBA_BLOCK = 256
MOBA_TOPK = 3
QUERY_CHUNK = 32
ROPE_THETA = 500000.0
ROPE_DIM = HEAD_DIM // 4
N_GROUPS = 4
EXPERTS_PER_GROUP = 8
N_EXPERTS = N_GROUPS * EXPERTS_PER_GROUP
TOPK_INNER = 2
D_EXPERT = 128
EPS = 1e-6
IN_WIDTH = SSM_WIDTH + 3 * ATTN_WIDTH + 2 * D_MODEL

kernel_name = "hybrid_s5_moba_hmoe_block"


def _rmsnorm(x, g):
    xf = x.astype(jnp.float32)
    y = xf * lax.rsqrt(jnp.mean(xf * xf, axis=-1, keepdims=True) + EPS)
    return (y * g.astype(jnp.float32)).astype(x.dtype)


def _partial_rope(x, pos):
    half = ROPE_DIM // 2
    inv_freq = ROPE_THETA ** (-jnp.arange(half, dtype=jnp.float32) * 2.0 / ROPE_DIM)
    ang = pos[:, None] * inv_freq[None, :]
    cos, sin = jnp.cos(ang), jnp.sin(ang)
    x1 = x[..., :half]
    x2 = x[..., half:ROPE_DIM]
    rot = jnp.concatenate([x1 * cos - x2 * sin, x2 * cos + x1 * sin], axis=-1)
    return jnp.concatenate([rot, x[..., ROPE_DIM:]], axis=-1)


def _ssm_combine(e1, e2):
    a1r, a1i, b1r, b1i = e1
    a2r, a2i, b2r, b2i = e2
    return (a1r * a2r - a1i * a2i,
            a1r * a2i + a1i * a2r,
            a2r * b1r - a2i * b1i + b2r,
            a2r * b1i + a2i * b1r + b2i)


def _s5_branch(u, lam_re, lam_im, log_dt, b_re, b_im, c_re, c_im, d_skip, w_glu, b_glu):
    Bsz, S, _ = u.shape
    f32 = jnp.float32
    uf = u.astype(f32).reshape(Bsz, S, SSM_GROUPS, SSM_GROUP)
    dt = jnp.exp(log_dt.astype(f32))[:, None]
    lr = jnp.minimum(lam_re.astype(f32), -1e-4)
    li = lam_im.astype(f32)
    mag = jnp.exp(lr * dt)
    ar = mag * jnp.cos(li * dt)
    ai = mag * jnp.sin(li * dt)
    den = lr * lr + li * li
    nr = ar - 1.0
    ni = ai
    cr = (nr * lr + ni * li) / den
    ci = (ni * lr - nr * li) / den
    brf, bif = b_re.astype(f32), b_im.astype(f32)
    bbr = cr[..., None] * brf - ci[..., None] * bif
    bbi = cr[..., None] * bif + ci[..., None] * brf
    bu_r = jnp.einsum('bsgc,gnc->bsgn', uf, bbr)
    bu_i = jnp.einsum('bsgc,gnc->bsgn', uf, bbi)
    a_r = jnp.broadcast_to(ar, bu_r.shape)
    a_i = jnp.broadcast_to(ai, bu_i.shape)
    _, _, st_r, st_i = lax.associative_scan(_ssm_combine, (a_r, a_i, bu_r, bu_i), axis=1)
    y = (jnp.einsum('bsgn,gcn->bsgc', st_r, c_re.astype(f32))
         - jnp.einsum('bsgn,gcn->bsgc', st_i, c_im.astype(f32)))
    y = y.reshape(Bsz, S, SSM_WIDTH) + d_skip.astype(f32) * uf.reshape(Bsz, S, SSM_WIDTH)
    z = jax.nn.gelu(y)
    out = z * jax.nn.sigmoid(z @ w_glu.astype(f32) + b_glu.astype(f32))
    return out.astype(u.dtype)


def _moba_attention(q, k, v):
    Bsz, H, S, Dh = q.shape
    nb = -(-S // MOBA_BLOCK)
    s_pad = nb * MOBA_BLOCK
    padw = ((0, 0), (0, 0), (0, s_pad - S), (0, 0))
    q = jnp.pad(q, padw)
    k = jnp.pad(k, padw)
    v = jnp.pad(v, padw)
    kb = k.reshape(Bsz, H, nb, MOBA_BLOCK, Dh)
    vb = v.reshape(Bsz, H, nb, MOBA_BLOCK, Dh)
    kmean = jnp.mean(kb, axis=3)
    qblk = jnp.arange(s_pad) // MOBA_BLOCK
    scores = jnp.einsum('bhsd,bhnd->bhsn', q, kmean)
    past = jnp.arange(nb)[None, :] < qblk[:, None]
    scores = jnp.where(past, scores, -jnp.inf)
    k_sel = min(MOBA_TOPK, nb)
    _, top_idx = lax.top_k(scores, k_sel)
    valid = top_idx < qblk[:, None]
    own = jnp.broadcast_to(qblk[:, None], (Bsz, H, s_pad, 1)).astype(top_idx.dtype)
    sel = jnp.concatenate([top_idx, own], axis=-1)
    sel_valid = jnp.concatenate([valid, jnp.ones((Bsz, H, s_pad, 1), dtype=bool)], axis=-1)
    m = k_sel + 1
    scale = HEAD_DIM ** -0.5
    gather = jax.vmap(jax.vmap(lambda blocks, idx: blocks[idx]))
    key_off = jnp.arange(MOBA_BLOCK)

    def chunk(ci):
        start = ci * QUERY_CHUNK
        qc = lax.dynamic_slice_in_dim(q, start, QUERY_CHUNK, axis=2)
        sc = lax.dynamic_slice_in_dim(sel, start, QUERY_CHUNK, axis=2)
        vc = lax.dynamic_slice_in_dim(sel_valid, start, QUERY_CHUNK, axis=2)
        kg = gather(kb, sc)
        vg = gather(vb, sc)
        logits = jnp.einsum('bhqd,bhqmld->bhqml', qc, kg) * scale
        qpos = start + jnp.arange(QUERY_CHUNK)
        kpos = sc[..., None] * MOBA_BLOCK + key_off
        allowed = vc[..., None] & (kpos <= qpos[:, None, None])
        logits = jnp.where(allowed, logits, -jnp.inf)
        p = jax.nn.softmax(logits.reshape(Bsz, H, QUERY_CHUNK, m * MOBA_BLOCK), axis=-1)
        p = p.reshape(Bsz, H, QUERY_CHUNK, m, MOBA_BLOCK)
        return jnp.einsum('bhqml,bhqmld->bhqd', p, vg)

    outs = lax.map(chunk, jnp.arange(s_pad // QUERY_CHUNK))
    out = outs.transpose(1, 2, 0, 3, 4).reshape(Bsz, H, s_pad, Dh)
    return out[:, :, :S]


def _hier_moe(h, w_rg, b_rg, w_re, b_re, w_gate, w_up, w_down):
    Bsz, S, D = h.shape
    f32 = jnp.float32
    t = h.reshape(-1, D)
    g_prob = jax.nn.softmax((t @ w_rg).astype(f32) + b_rg.astype(f32), axis=-1)
    g_p, g_idx = lax.top_k(g_prob, 1)
    e_logits = ((t @ w_re).astype(f32) + b_re.astype(f32)).reshape(-1, N_GROUPS, EXPERTS_PER_GROUP)
    g_onehot = jax.nn.one_hot(g_idx[:, 0], N_GROUPS, dtype=f32)
    e_in = jnp.einsum('tge,tg->te', e_logits, g_onehot)
    e_val, e_idx = lax.top_k(e_in, TOPK_INNER)
    e_w = jax.nn.softmax(e_val, axis=-1) * g_p
    flat = g_idx * EXPERTS_PER_GROUP + e_idx
    comb = jnp.sum(jax.nn.one_hot(flat, N_EXPERTS, dtype=f32) * e_w[..., None], axis=1)
    hg = jnp.einsum('td,edf->tef', t, w_gate)
    hu = jnp.einsum('td,edf->tef', t, w_up)
    act = jax.nn.silu(hg) * hu * comb[:, :, None].astype(t.dtype)
    y = jnp.einsum('tef,efd->td', act, w_down)
    return y.reshape(Bsz, S, D)


def setup_inputs(seed: int = 0) -> dict:
    key = jax.random.key(seed)
    ks = jax.random.split(key, 32)
    f32 = jnp.float32
    nrm = lambda k, shape, s: jax.random.normal(k, shape, f32) * s
    L = DEPTH
    lam_im = (jnp.pi * jnp.arange(SSM_STATE, dtype=f32))[None, None, :] + nrm(ks[4], (L, SSM_GROUPS, SSM_STATE), 0.01)
    return {
        "x": nrm(ks[0], (BATCH, SEQ, D_MODEL), 1.0),
        "norm1_g": 1.0 + nrm(ks[1], (L, D_MODEL), 0.01),
        "w_in": nrm(ks[2], (L, D_MODEL, IN_WIDTH), D_MODEL ** -0.5),
        "lam_re": -0.5 + nrm(ks[3], (L, SSM_GROUPS, SSM_STATE), 0.01),
        "lam_im": lam_im,
        "log_dt": jax.random.uniform(ks[5], (L, SSM_GROUPS), f32, math.log(DT_MIN), math.log(DT_MAX)),
        "ssm_b_re": nrm(ks[6], (L, SSM_GROUPS, SSM_STATE, SSM_GROUP), (2 * SSM_GROUP) ** -0.5),
        "ssm_b_im": nrm(ks[7], (L, SSM_GROUPS, SSM_STATE, SSM_GROUP), (2 * SSM_GROUP) ** -0.5),
        "ssm_c_re": nrm(ks[8], (L, SSM_GROUPS, SSM_GROUP, SSM_STATE), 0.5),
        "ssm_c_im": nrm(ks[9], (L, SSM_GROUPS, SSM_GROUP, SSM_STATE), 0.5),
        "ssm_d": nrm(ks[10], (L, SSM_WIDTH), 1.0),
        "w_glu": nrm(ks[11], (L, SSM_WIDTH, SSM_WIDTH), SSM_WIDTH ** -0.5),
        "b_glu": nrm(ks[12], (L, SSM_WIDTH), 0.01),
        "q_norm_g": 1.0 + nrm(ks[13], (L, HEAD_DIM), 0.01),
        "k_norm_g": 1.0 + nrm(ks[14], (L, HEAD_DIM), 0.01),
        "w_proj_ssm": nrm(ks[15], (L, SSM_WIDTH, D_MODEL), SSM_WIDTH ** -0.5),
        "w_proj_attn": nrm(ks[16], (L, ATTN_WIDTH, D_MODEL), ATTN_WIDTH ** -0.5),
        "w_out": nrm(ks[17], (L, D_MODEL, D_MODEL), D_MODEL ** -0.5),
        "norm2_g": 1.0 + nrm(ks[18], (L, D_MODEL), 0.01),
        "w_router_group": nrm(ks[19], (L, D_MODEL, N_GROUPS), D_MODEL ** -0.5),
        "b_router_group": nrm(ks[20], (L, N_GROUPS), 0.01),
        "w_router_expert": nrm(ks[21], (L, D_MODEL, N_EXPERTS), D_MODEL ** -0.5),
        "b_router_expert": nrm(ks[22], (L, N_EXPERTS), 0.01),
        "w_gate": nrm(ks[23], (L, N_EXPERTS, D_MODEL, D_EXPERT), D_MODEL ** -0.5),
        "w_up": nrm(ks[24], (L, N_EXPERTS, D_MODEL, D_EXPERT), D_MODEL ** -0.5),
        "w_down": nrm(ks[25], (L, N_EXPERTS, D_EXPERT, D_MODEL), D_EXPERT ** -0.5),
    }


def reference(x, norm1_g, w_in, lam_re, lam_im, log_dt, ssm_b_re, ssm_b_im, ssm_c_re, ssm_c_im,
              ssm_d, w_glu, b_glu, q_norm_g, k_norm_g, w_proj_ssm, w_proj_attn, w_out, norm2_g,
              w_router_group, b_router_group, w_router_expert, b_router_expert, w_gate, w_up, w_down):
    Bsz, S, D = x.shape
    pos = jnp.arange(S, dtype=jnp.float32)
    offs = [SSM_WIDTH, SSM_WIDTH + ATTN_WIDTH, SSM_WIDTH + 2 * ATTN_WIDTH,
            SSM_WIDTH + 3 * ATTN_WIDTH, SSM_WIDTH + 3 * ATTN_WIDTH + D_MODEL]
    for l in range(DEPTH):
        h = _rmsnorm(x, norm1_g[l])
        proj = h @ w_in[l]
        u, q, k, v, g_ssm, g_attn = jnp.split(proj, offs, axis=-1)
        y_ssm = _s5_branch(u, lam_re[l], lam_im[l], log_dt[l], ssm_b_re[l], ssm_b_im[l],
                           ssm_c_re[l], ssm_c_im[l], ssm_d[l], w_glu[l], b_glu[l]) @ w_proj_ssm[l]
        heads = lambda t: t.astype(jnp.float32).reshape(Bsz, S, N_HEADS, HEAD_DIM).transpose(0, 2, 1, 3)
        qh = _partial_rope(_rmsnorm(heads(q), q_norm_g[l]), pos)
        kh = _partial_rope(_rmsnorm(heads(k), k_norm_g[l]), pos)
        vh = heads(v)
        att = _moba_attention(qh, kh, vh)
        att = att.transpose(0, 2, 1, 3).reshape(Bsz, S, ATTN_WIDTH).astype(x.dtype)
        y_attn = att @ w_proj_attn[l]
        merged = jax.nn.sigmoid(g_ssm) * y_ssm + jax.nn.sigmoid(g_attn) * y_attn
        x = x + merged @ w_out[l]
        h2 = _rmsnorm(x, norm2_g[l])
        x = x + _hier_moe(h2, w_router_group[l], b_router_group[l], w_router_expert[l],
                          b_router_expert[l], w_gate[l], w_up[l], w_down[l])
    return x
```

```python
import math
import os
from contextlib import ExitStack

import numpy as np
import ml_dtypes

import concourse.bass as bass
import concourse.mybir as mybir
from concourse.bass_utils import run_bass_kernel_spmd

F32 = mybir.dt.float32
BF16 = mybir.dt.bfloat16
I32 = mybir.dt.int32
AF = mybir.ActivationFunctionType
ALU = mybir.AluOpType
AX = mybir.AxisListType

NCORES = 8
D = 1024
S_OWN = 4096
S_CTX = 4096
NST_CTX = 4
NST_OWN = 4
NST = NST_CTX + NST_OWN
NEG = -30000.0
EPS = 1e-6
TWO_PI = 2.0 * math.pi


class Res:
    __slots__ = ("name", "last_w", "readers", "stream")

    def __init__(self, name):
        self.name = name
        self.last_w = None
        self.readers = []
        self.stream = None


class Ins:
    __slots__ = ("idx", "eng", "fn", "deps", "marked", "count", "dma_sem", "dma_val", "is_dma",
                 "bar_streams")


class Prog:
    ENGS = ("pe", "act", "dve", "pool", "sp")

    def __init__(self, nc, sbuf_elems):
        self.nc = nc
        self.ins = []
        self.per_eng = {e: [] for e in self.ENGS}
        self.stack = ExitStack()
        self.dma_streams = []
        self.nres = 0
        self.bar = None
        self.recording = None
        self.arena = self.stack.enter_context(nc.sbuf_tensor("arena", [128, sbuf_elems], BF16))
        self.arena_off = 0
        self.arena_size = sbuf_elems
        self.arena_peak = 0

    def tile(self, shape, dtype):
        n = 1
        for s in shape:
            n *= s
        nb = n * (2 if dtype == F32 or dtype == I32 else 1)
        nb = (nb + 15) // 16 * 16
        off = self.arena_off
        self.arena_off += nb
        self.arena_peak = max(self.arena_peak, self.arena_off)
        assert self.arena_off <= self.arena_size, f"SBUF arena overflow {self.arena_off}"
        ap = self.arena[:, off:off + nb]
        if dtype != BF16:
            ap = ap.bitcast(dtype)
            nb //= 2
        ap = ap[:, 0:n]
        if len(shape) == 2:
            ap = ap.rearrange("p (a b) -> p a b", b=shape[1])
        elif len(shape) == 3:
            ap = ap.rearrange("p (a b c) -> p a b c", b=shape[1], c=shape[2])
        elif len(shape) == 4:
            ap = ap.rearrange("p (a b c d) -> p a b c d", b=shape[1], c=shape[2], d=shape[3])
        return ap

    def mark(self):
        return self.arena_off

    def release(self, m):
        self.arena_off = m

    def sem(self, name):
        return self.stack.enter_context(self.nc.semaphore(name))

    def res(self, name=None):
        self.nres += 1
        return Res(name or f"r{self.nres}")

    def stream(self, name):
        st = {"sem": self.sem(f"dq{len(self.dma_streams)}"), "val": 0}
        self.dma_streams.append(st)
        return st

    def barrier(self):
        last = []
        for e in ("pe", "act", "dve", "pool"):
            for I in reversed(self.per_eng[e]):
                if not I.is_dma:
                    last.append(I)
                    break
        snap = [(st, st["val"]) for st in self.dma_streams if st["val"] > 0]
        self.bar = {e: (list(last), snap) for e in self.ENGS}

    def _add(self, eng, fn, reads, writes, is_dma=False, stream=None):
        I = Ins()
        I.idx = len(self.ins)
        I.eng = eng
        I.fn = fn
        I.marked = False
        I.count = None
        I.is_dma = is_dma
        I.dma_sem = None
        I.dma_val = None
        I.bar_streams = None
        if is_dma:
            stream["val"] += 16
            I.dma_sem = stream["sem"]
            I.dma_val = stream["val"]
        deps = {}

        def add_dep(d, raw):
            if d is None:
                return
            if (not d.is_dma) and (not is_dma) and d.eng == eng:
                if eng == "pe" or (not raw and not SYNC_SAME_ENGINE_WAR):
                    return
            deps[d.idx] = d

        for r in reads:
            add_dep(r.last_w, True)
        for w in writes:
            add_dep(w.last_w, False)
            for rd in w.readers:
                add_dep(rd, False)
        for r in reads:
            r.readers.append(I)
        for w in writes:
            w.last_w = I
            w.readers = []
        if self.bar is not None and eng in self.bar:
            last, snap = self.bar.pop(eng)
            for d in last:
                if d.eng != eng or is_dma:
                    deps[d.idx] = d
            I.bar_streams = snap
        I.deps = list(deps.values())
        self.ins.append(I)
        self.per_eng[eng].append(I)
        return I

    def op(self, eng, fn, reads=(), writes=()):
        if self.recording is not None:
            reads, writes = list(reads), list(writes)
            self.recording.append(lambda: self._add(eng, fn, reads, writes))
            return None
        return self._add(eng, fn, reads, writes)

    def dma(self, eng, out, in_, reads=(), writes=(), slow=False):
        if self.recording is not None:
            reads, writes = list(reads), list(writes)
            rec = self.recording
            self.recording = None
            rec.append(lambda: self.dma(eng, out, in_, reads, writes, slow))
            self.recording = rec
            return None
        w = writes[0]
        if w.stream is None:
            w.stream = self.stream(w.name)
        if slow:
            f = lambda e: e.dma_start(out=out, in_=in_, allow_slow_non_contiguous=True)
        else:
            f = lambda e: e.dma_start(out=out, in_=in_)
        return self._add(eng, f, reads, writes, is_dma=True, stream=w.stream)

    def idma(self, out, in_, idx, gather, reads=(), writes=()):
        w = writes[0]
        if w.stream is None:
            w.stream = self.stream(w.name)
        if gather:
            f = lambda e: e.indirect_dma_start(out=out, out_offset=None, in_=in_,
                                               in_offset=bass.IndirectOffsetOnAxis(ap=idx, axis=0))
        else:
            f = lambda e: e.indirect_dma_start(out=out, out_offset=bass.IndirectOffsetOnAxis(ap=idx, axis=0),
                                               in_=in_, in_offset=None)
        return self._add("pool", f, reads, writes, is_dma=True, stream=w.stream)

    def finalize(self):
        nc = self.nc
        final_waits = [(st["sem"], st["val"]) for st in self.dma_streams if st["val"] > 0]
        for I in self.ins:
            for d in I.deps:
                if not d.is_dma:
                    d.marked = True
        engsem = {}
        for e in ("pe", "act", "dve", "pool"):
            engsem[e] = self.sem("es_" + e)
            c = 0
            for I in self.per_eng[e]:
                if I.is_dma:
                    continue
                if I.marked:
                    c += 1
                    I.count = c
        nwaits = 0
        with nc.Block() as block:
            def emit(ename):
                def body(e):
                    nonlocal nwaits
                    waited = {}
                    for I in self.per_eng[ename]:
                        need = {}
                        for d in I.deps:
                            if d.is_dma:
                                key = ("d", id(d.dma_sem))
                                sem, val = d.dma_sem, d.dma_val
                            else:
                                key = ("e", d.eng)
                                sem, val = engsem[d.eng], d.count
                            if key not in need or need[key][1] < val:
                                need[key] = (sem, val)
                        if I.bar_streams:
                            for st, val in I.bar_streams:
                                key = ("d", id(st["sem"]))
                                if key not in need or need[key][1] < val:
                                    need[key] = (st["sem"], val)
                        for key, (sem, val) in need.items():
                            if waited.get(key, 0) >= val:
                                continue
                            e.wait_ge(sem, val)
                            nwaits += 1
                            waited[key] = val
                        bi = I.fn(e)
                        if I.is_dma:
                            bi.then_inc(I.dma_sem, 16)
                        elif I.marked:
                            bi.then_inc(engsem[ename], 1)
                    if ename == "sp":
                        for (sem, val) in final_waits:
                            e.wait_ge(sem, val)
                return body
            block.tensor(emit("pe"))
            block.scalar(emit("act"))
            block.vector(emit("dve"))
            block.gpsimd(emit("pool"))
            block.sync(emit("sp"))
        self.nwaits = nwaits
        self.stack.close()


class B:
    def __init__(self, P):
        self.P = P

    def mm(self, out, lhsT, rhs, start, stop, reads, writes):
        self.P.op("pe", lambda e: e.matmul(out, lhsT, rhs, start=start, stop=stop), reads, writes)

    def tr(self, out, in_, ident, reads, writes):
        self.P.op("pe", lambda e: e.transpose(out, in_, ident), reads, writes)

    def act(self, out, in_, func, reads, writes, bias=0.0, scale=1.0, accum=None):
        if accum is None:
            self.P.op("act", lambda e: e.activation(out=out, in_=in_, func=func, bias=bias, scale=scale),
                      reads, writes)
        else:
            self.P.op("act", lambda e: e.activation(out=out, in_=in_, func=func, bias=bias, scale=scale,
                                                    accum_out=accum), reads, writes)

    def tt(self, eng, out, in0, in1, op, reads, writes):
        self.P.op(eng, lambda e: e.tensor_tensor(out=out, in0=in0, in1=in1, op=op), reads, writes)

    def ts(self, eng, out, in0, s1, s2, op0, op1, reads, writes):
        if s2 is None:
            self.P.op(eng, lambda e: e.tensor_scalar(out=out, in0=in0, scalar1=s1, scalar2=None, op0=op0),
                      reads, writes)
        else:
            self.P.op(eng, lambda e: e.tensor_scalar(out=out, in0=in0, scalar1=s1, scalar2=s2, op0=op0, op1=op1),
                      reads, writes)

    def stt(self, eng, out, in0, scalar, in1, op0, op1, reads, writes):
        self.P.op(eng, lambda e: e.scalar_tensor_tensor(out=out, in0=in0, scalar=scalar, in1=in1,
                                                         op0=op0, op1=op1), reads, writes)

    def cp(self, eng, out, in_, reads, writes):
        if eng == "act":
            self.P.op("act", lambda e: e.copy(out=out, in_=in_), reads, writes)
        else:
            self.P.op(eng, lambda e: e.tensor_copy(out=out, in_=in_), reads, writes)

    def memset(self, eng, out, val, writes):
        self.P.op(eng, lambda e: e.memset(out, val), (), writes)

    def recip(self, out, in_, reads, writes):
        self.P.op("dve", lambda e: e.reciprocal(out=out, in_=in_), reads, writes)

    def reduce(self, out, in_, op, reads, writes, eng="dve"):
        self.P.op(eng, lambda e: e.tensor_reduce(out=out, in_=in_, axis=AX.X, op=op), reads, writes)


def is_own(s):
    return s % 2 == 1


def q_block(ob):
    return 4 * (2 * (ob // 4) + 1) + ob % 4


def bc(ap, shape):
    return ap.to_broadcast(list(shape))


SYNC_SAME_ENGINE_WAR = True
ARENA_ELEMS = 106400
P0_BASE = 74272


def build(debug=(), stop_after="D"):
    nc = bass.Bass("TRN2", target_bir_lowering=False)
    P = Prog(nc, ARENA_ELEMS)
    b = B(P)
    T = P.tile
    R = P.res
    dbg_out = {}

    def din(name, shape, dt=F32):
        return nc.dram_tensor(name, list(shape), dt, kind="ExternalInput").ap()

    def dscr(name, shape, dt):
        return nc.dram_tensor(name, list(shape), dt, kind="Internal").ap()

    def dump(name, ap_sb, shape, reads, dt=F32, eng="sp"):
        if name not in debug:
            return
        o = nc.dram_tensor("dbg_" + name, list(shape), dt, kind="ExternalOutput").ap()
        dbg_out[name] = o
        P.dma(eng, o, ap_sb, reads=reads, writes=[R("dbg_" + name)])

    x_all = din("x_all", [S_CTX + S_OWN, D])
    w_in = din("w_in", [D, 4096])
    norm1_g = din("norm1_g", [1, D])
    lamre_t = din("lamre_t", [64, 32])
    lamim_t = din("lamim_t", [64, 32])
    log_dt = din("log_dt", [1, 32])
    b_re_t = din("b_re_t", [64, 32, 16])
    b_im_t = din("b_im_t", [64, 32, 16])
    c_re_t = din("c_re_t", [64, 32, 16])
    c_im_t = din("c_im_t", [64, 32, 16])
    ssm_d = din("ssm_d", [1, 512])
    w_glu = din("w_glu", [512, 512])
    b_glu = din("b_glu", [1, 512])
    q_norm_g = din("q_norm_g", [1, 64])
    k_norm_g = din("k_norm_g", [1, 64])
    w_proj_ssm = din("w_proj_ssm", [512, D])
    w_proj_attn = din("w_proj_attn", [512, D])
    w_out = din("w_out", [D, D])
    norm2_g = din("norm2_g", [1, D])
    w_router = din("w_router", [D, 36])
    b_router = din("b_router", [1, 36])
    w_gate = din("w_gate", [32, D, 128])
    w_up = din("w_up", [32, D, 128])
    w_down = din("w_down", [32, 128, D])
    ident_in = din("ident_in", [128, 128])
    cs_tab_in = din("cs_tab", [128, 64 * 16])
    pbrep_in = din("pbrep", [128, 16 * 32])
    onehot_in = din("onehot_k", [32, 8192])
    tri_in = din("tri", [128, 128])
    rconst_in = din("rconst", [128, 32 + 96 + 1])
    y_out = nc.dram_tensor("y_out", [S_OWN, D], F32, kind="ExternalOutput").ap()

    kT_scr = dscr("kT_scr", [8, 64, 8192], BF16)
    qT_scr = dscr("qT_scr", [8, 64, 4096], BF16)
    v_scr = dscr("v_scr", [8, 128, 64 * 65], BF16)
    z_scr = dscr("z_scr", [NST_OWN, 128, 8 * 512], BF16)
    att_scr = dscr("att_scr", [S_OWN, 512], BF16)
    wgu_scr = dscr("wgu_scr", [32, 128, 2, 8, 128], BF16)
    wd_scr = dscr("wd_scr", [32, 128, D], BF16)
    hs_scr = dscr("hs_scr", [96 * 128, D], BF16)
    h2_scr = dscr("h2_scr", [S_OWN, D], BF16)
    ys_scr = dscr("ys_scr", [96 * 128, D], F32)
    r_kT_scr = [R(f"kTs{h}") for h in range(8)]
    r_qT_scr = [R(f"qTs{h}") for h in range(8)]
    r_v_scr = [R(f"vs{h}") for h in range(8)]
    r_z_scr = [R(f"zs{s}") for s in range(NST_OWN)]
    r_att_scr = R("atts")
    _rw = [R(f"wscr{i}") for i in range(4)]
    r_wgu = [_rw[e % 4] for e in range(32)]
    r_wd = [_rw[e % 4] for e in range(32)]

    pd = [P.stack.enter_context(nc.psum_tensor(f"pd{i}", [128, 1024], F32)) for i in range(4)]
    pr = [[R(f"pd{i}a"), R(f"pd{i}b")] for i in range(4)]

    def pbank(i):
        return pd[i // 2][:, (i % 2) * 512:(i % 2) * 512 + 512], pr[i // 2][i % 2]

    def pbank_bf(i):
        return pd[i // 2][:, (i % 2) * 512:(i % 2) * 512 + 512].bitcast(BF16), pr[i // 2][i % 2]

    identf = T([128], F32); r_identf = R("identf")
    identb = T([128], BF16); r_identb = R("identb")
    P.dma("sp", identf, ident_in, writes=[r_identf])
    P.dma("pool", identb, ident_in, writes=[r_identb])
    g1rep = T([D], F32); r_g1 = R("g1rep")
    P.dma("sp", g1rep, norm1_g[0:1, :].partition_broadcast(128), writes=[r_g1])
    ksum = T([8, 64], F32); r_ksum = R("ksum")
    m_ssm = P.mark()
    gqk = T([2, 64], F32); r_gqk = R("gqk")
    P.dma("sp", gqk[:, 0, :], q_norm_g[0:1, :].partition_broadcast(128), writes=[r_gqk])
    P.dma("sp", gqk[:, 1, :], k_norm_g[0:1, :].partition_broadcast(128), writes=[r_gqk])
    cs_tab = T([64, 16], F32); r_cs = R("cs_tab")
    P.dma("sp", cs_tab, cs_tab_in.rearrange("p (t f) -> p t f", f=16), writes=[r_cs])
    drep = T([512], F32); r_drep = R("drep")
    P.dma("sp", drep, ssm_d[0:1, :].partition_broadcast(128), writes=[r_drep])


    W_e = T([32, 2, 64], BF16); r_We = R("W_e")
    Tz = T([32, 128], BF16); r_Tz = R("Tz")
    W_cr = T([16, 8, 16], BF16); r_Wc = R("W_c")
    W_ci = T([16, 8, 16], BF16)
    ER = T([16, 129], F32); r_E = R("E")
    EI = T([16, 129], F32)
    rho = T([16], F32); r_rho = R("rho")
    SR0 = T([16], F32); r_S0 = R("S0")
    SI0 = T([16], F32)
    b.memset("dve", SR0, 0.0, [r_S0])
    b.memset("dve", SI0, 0.0, [r_S0])

    m0 = P.mark()
    P.arena_off = P0_BASE
    P.recording = []
    r_p = R("prm")

    def hn_load(dst, src, extra=""):
        for gh in range(2):
            P.dma("sp", dst[64 * gh:64 * gh + 64], src[:, 16 * gh:16 * gh + 16], writes=[r_p])

    LR = T([16], F32); LI = T([16], F32); DT = T([16], F32)
    hn_load(LR, lamre_t); hn_load(LI, lamim_t)
    for gh in range(2):
        P.dma("sp", DT[64 * gh:64 * gh + 64], log_dt[0:1, 16 * gh:16 * gh + 16].partition_broadcast(64), writes=[r_p])
    BR = T([16, 16], F32); BI = T([16, 16], F32); CR = T([16, 16], F32); CI = T([16, 16], F32)
    hn_load(BR, b_re_t); hn_load(BI, b_im_t); hn_load(CR, c_re_t); hn_load(CI, c_im_t)
    rp = [r_p]
    ey = T([16], F32); ep = T([16], F32)

    def exp_acc(dst, src, extra_w=()):
        b.ts("dve", ey, src, 0.125, None, ALU.mult, None, rp, rp)
        b.ts("dve", ep, ey, 1.0 / 10, 1.0, ALU.mult, ALU.add, rp, rp)
        for n in range(9, 0, -1):
            b.tt("dve", ep, ep, ey, ALU.mult, rp, rp)
            b.ts("dve", ep, ep, 1.0 / n, 1.0, ALU.mult, ALU.add, rp, rp)
        b.tt("dve", ep, ep, ep, ALU.mult, rp, rp)
        b.tt("dve", ep, ep, ep, ALU.mult, rp, rp)
        b.tt("dve", dst, ep, ep, ALU.mult, rp, rp + list(extra_w))

    exp_acc(DT, DT)
    b.ts("dve", LR, LR, -1e-4, None, ALU.min, None, rp, rp)
    LRDT = T([16], F32); ANG = T([16], F32)
    b.tt("dve", LRDT, LR, DT, ALU.mult, rp, rp)
    b.tt("dve", ANG, LI, DT, ALU.mult, rp, rp)
    MAG = T([16], F32)
    exp_acc(MAG, LRDT)
    b.tt("dve", rho, MAG, MAG, ALU.mult, rp, [r_p, r_rho])
    b.tt("dve", rho, rho, rho, ALU.mult, [r_p, r_rho], [r_p, r_rho])
    b.tt("dve", rho, rho, rho, ALU.mult, [r_p, r_rho], [r_p, r_rho])
    SN = T([16], F32); CS = T([16], F32)
    tq = T([16], F32); ki = T([16], I32); kf = T([16], F32); rr = T([16], F32)
    for shift, outt in ((0.0, SN), (math.pi / 2, CS)):
        b.ts("dve", tq, ANG, 1.0 / TWO_PI, shift / TWO_PI, ALU.mult, ALU.add, rp, rp)
        b.cp("dve", ki, tq, rp, rp)
        b.cp("dve", kf, ki, rp, rp)
        b.stt("dve", rr, kf, -6.28125, ANG, ALU.mult, ALU.add, rp, rp)
        b.stt("dve", rr, kf, -(TWO_PI - 6.28125), rr, ALU.mult, ALU.add, rp, rp)
        b.ts("dve", rr, rr, shift, math.pi, ALU.add, ALU.min, rp, rp)
        b.ts("dve", rr, rr, -math.pi, None, ALU.max, None, rp, rp)
        b.act(outt, rr, AF.Sin, rp, rp)
    PR = T([9, 16], F32); PI = T([9, 16], F32)
    b.memset("dve", PR[:, 0, :], 1.0, rp)
    b.memset("dve", PI[:, 0, :], 0.0, rp)
    b.tt("dve", PR[:, 1, :], MAG, CS, ALU.mult, rp, rp)
    b.tt("dve", PI[:, 1, :], MAG, SN, ALU.mult, rp, rp)
    t1 = T([4, 16], F32); t2 = T([4, 16], F32)

    def cmul_pow(dst0, n, src0, m):
        a_r = PR[:, src0:src0 + n, :]; a_i = PI[:, src0:src0 + n, :]
        b_r = bc(PR[:, m:m + 1, :], [128, n, 16]); b_i = bc(PI[:, m:m + 1, :], [128, n, 16])
        b.tt("dve", t1[:, 0:n, :], a_r, b_r, ALU.mult, rp, rp)
        b.tt("dve", t2[:, 0:n, :], a_i, b_i, ALU.mult, rp, rp)
        b.tt("dve", PR[:, dst0:dst0 + n, :], t1[:, 0:n, :], t2[:, 0:n, :], ALU.subtract, rp, rp)
        b.tt("dve", t1[:, 0:n, :], a_r, b_i, ALU.mult, rp, rp)
        b.tt("dve", t2[:, 0:n, :], a_i, b_r, ALU.mult, rp, rp)
        b.tt("dve", PI[:, dst0:dst0 + n, :], t1[:, 0:n, :], t2[:, 0:n, :], ALU.add, rp, rp)

    cmul_pow(2, 1, 1, 1)
    cmul_pow(3, 2, 1, 2)
    cmul_pow(5, 4, 1, 4)
    DEN = T([16], F32); NR = T([16], F32); CRc = T([16], F32); CIc = T([16], F32); tmp = T([16], F32)
    b.tt("dve", DEN, LR, LR, ALU.mult, rp, rp)
    b.tt("dve", tmp, LI, LI, ALU.mult, rp, rp)
    b.tt("dve", DEN, DEN, tmp, ALU.add, rp, rp)
    b.recip(DEN, DEN, rp, rp)
    b.ts("dve", NR, PR[:, 1, :], -1.0, None, ALU.add, None, rp, rp)
    AIc = PI[:, 1, :]
    b.tt("dve", CRc, NR, LR, ALU.mult, rp, rp)
    b.tt("dve", tmp, AIc, LI, ALU.mult, rp, rp)
    b.tt("dve", CRc, CRc, tmp, ALU.add, rp, rp)
    b.tt("dve", CRc, CRc, DEN, ALU.mult, rp, rp)
    b.tt("dve", CIc, AIc, LR, ALU.mult, rp, rp)
    b.tt("dve", tmp, NR, LI, ALU.mult, rp, rp)
    b.tt("dve", CIc, CIc, tmp, ALU.subtract, rp, rp)
    b.tt("dve", CIc, CIc, DEN, ALU.mult, rp, rp)
    BBR = T([16, 16], F32); BBI = T([16, 16], F32); tb1 = T([16, 16], F32); tb2 = T([16, 16], F32)
    crb = bc(CRc.unsqueeze(2), [128, 16, 16]); cib = bc(CIc.unsqueeze(2), [128, 16, 16])
    b.tt("dve", tb1, BR, crb, ALU.mult, rp, rp)
    b.tt("dve", tb2, BI, cib, ALU.mult, rp, rp)
    b.tt("dve", BBR, tb1, tb2, ALU.subtract, rp, rp)
    b.tt("dve", tb1, BI, crb, ALU.mult, rp, rp)
    b.tt("dve", tb2, BR, cib, ALU.mult, rp, rp)
    b.tt("dve", BBI, tb1, tb2, ALU.add, rp, rp)
    mpad_off = P.mark()
    Mpad = T([16, 2, 240], F32)
    b.memset("pool", Mpad, 0.0, rp)
    for s in range(8):
        tau = 7 - s
        prb = bc(PR[:, tau, :].unsqueeze(2), [128, 16, 16]); pib = bc(PI[:, tau, :].unsqueeze(2), [128, 16, 16])
        b.tt("dve", tb1, BBR, prb, ALU.mult, rp, rp)
        b.tt("dve", tb2, BBI, pib, ALU.mult, rp, rp)
        b.tt("dve", Mpad[:, :, 0, 16 * s:16 * s + 16], tb1, tb2, ALU.subtract, rp, rp)
        b.tt("dve", tb1, BBI, prb, ALU.mult, rp, rp)
        b.tt("dve", tb2, BBR, pib, ALU.mult, rp, rp)
        b.tt("dve", Mpad[:, :, 1, 16 * s:16 * s + 16], tb1, tb2, ALU.add, rp, rp)
    NCI = T([16, 16], F32)
    b.ts("dve", NCI, CI, -1.0, None, ALU.mult, None, rp, rp)
    for g0 in range(0, 32, 4):
        pv, prs = pbank(6 + g0 // 4 % 2)
        for gg in range(4):
            g = g0 + gg
            gh, gl = g // 16, g % 16
            for ri in range(2):
                b.tr(pv[:, (gg * 2 + ri) * 64:(gg * 2 + ri) * 64 + 64], Mpad[64 * gh:64 * gh + 64, gl, ri, 0:128],
                     identf[64 * gh:64 * gh + 64, 64 * gh:64 * gh + 64], [r_p, r_identf], [prs])
        b.cp("act", W_e[:, g0:g0 + 4, :, :], pv.rearrange("p (g r n) -> p g r n", g=4, r=2), [prs], [r_We])
    MpadB = T([16, 2, 240], BF16); CRb = T([16, 16], BF16); NCIb = T([16, 16], BF16)
    b.cp("pool", MpadB, Mpad, rp, rp)
    b.cp("pool", CRb, CR, rp, rp)
    b.cp("pool", NCIb, NCI, rp, rp)
    for g0 in range(0, 32, 4):
        pv, prs = pbank(6 + g0 // 4 % 2)
        for gg in range(4):
            g = g0 + gg
            gh, gl = g // 16, g % 16
            sl = slice(64 * gh, 64 * gh + 64)
            for i in range(8):
                o = pv[:, gg * 128 + i * 16:gg * 128 + i * 16 + 16]
                b.mm(o, MpadB[sl, gl, 0, 16 * (7 - i):16 * (7 - i) + 128], CRb[sl, gl, :], True, False, [r_p], [prs])
                b.mm(o, MpadB[sl, gl, 1, 16 * (7 - i):16 * (7 - i) + 128], NCIb[sl, gl, :], False, True, [r_p], [prs])
        b.cp("act", Tz[:, g0:g0 + 4, :], pv.rearrange("p (g x) -> p g x", g=4), [prs], [r_Tz])
    _cur = P.mark()
    P.arena_off = mpad_off
    tc1 = T([16, 8, 16], F32); tc2 = T([16, 8, 16], F32)
    crB = bc(CR.unsqueeze(2), [128, 16, 8, 16]); ciB = bc(CI.unsqueeze(2), [128, 16, 8, 16]); nciB = bc(NCI.unsqueeze(2), [128, 16, 8, 16])
    PRi = bc(PR[:, 1:9, :].rearrange("p i g -> p g i").unsqueeze(3), [128, 16, 8, 16])
    PIi = bc(PI[:, 1:9, :].rearrange("p i g -> p g i").unsqueeze(3), [128, 16, 8, 16])
    b.tt("dve", tc1, crB, PRi, ALU.mult, rp, rp)
    b.tt("dve", tc2, ciB, PIi, ALU.mult, rp, rp)
    b.tt("dve", W_cr, tc1, tc2, ALU.subtract, rp, [r_p, r_Wc])
    b.tt("dve", tc1, nciB, PRi, ALU.mult, rp, rp)
    b.tt("dve", tc2, crB, PIi, ALU.mult, rp, rp)
    b.tt("dve", W_ci, tc1, tc2, ALU.subtract, rp, [r_p, r_Wc])
    RINV = T([16], F32)
    b.recip(RINV, rho, [r_rho], rp)
    rE = [r_p, r_E]
    b.memset("dve", ER[:, :, 0:1], 1.0, rE)
    b.memset("dve", EI[:, :, 0:1], 0.0, rE)
    b.tt("dve", ER[:, :, 1], PR[:, 8, :], RINV, ALU.mult, rp, rE)
    b.tt("dve", EI[:, :, 1], PI[:, 8, :], RINV, ALU.mult, rp, rE)
    te1 = T([16, 64], F32); te2 = T([16, 64], F32)
    assert P.arena_off <= mpad_off + 16 * 2 * 240 * 2
    P.arena_off = _cur
    m = 1
    while m < 128:
        n = m
        a_r = ER[:, :, 1:1 + n]; a_i = EI[:, :, 1:1 + n]
        b_r = bc(ER[:, :, m:m + 1], [128, 16, n]); b_i = bc(EI[:, :, m:m + 1], [128, 16, n])
        b.tt("dve", te1[:, :, 0:n], a_r, b_r, ALU.mult, rE, rp)
        b.tt("pool", te2[:, :, 0:n], a_i, b_i, ALU.mult, rE, rp)
        b.tt("dve", ER[:, :, m + 1:m + 1 + n], te1[:, :, 0:n], te2[:, :, 0:n], ALU.subtract, rp, rE)
        b.tt("dve", te1[:, :, 0:n], a_r, b_i, ALU.mult, rE, rp)
        b.tt("pool", te2[:, :, 0:n], a_i, b_r, ALU.mult, rE, rp)
        b.tt("dve", EI[:, :, m + 1:m + 1 + n], te1[:, :, 0:n], te2[:, :, 0:n], ALU.add, rp, rE)
        m *= 2
    dump("W_e", W_e, [128, 32, 2, 64], [r_We], BF16)
    dump("Tz", Tz, [128, 32, 128], [r_Tz], BF16)
    dump("W_cr", W_cr, [128, 16, 8, 16], [r_Wc], BF16)
    dump("W_ci", W_ci, [128, 16, 8, 16], [r_Wc], BF16)
    dump("ER", ER, [128, 16, 129], [r_E])
    dump("EI", EI, [128, 16, 129], [r_E])
    dump("rho", rho, [128, 16], [r_rho])
    dump("PR", PR, [128, 9, 16], rp)
    dump("PI", PI, [128, 9, 16], rp)
    print("phase 0 temporaries end", P.arena_peak)
    p0_ops = P.recording
    P.recording = None
    P.release(m0)
    ctx = dict(nc=nc, P=P, b=b, T=T, R=R, dump=dump, dbg_out=dbg_out, pbank=pbank, pbank_bf=pbank_bf, pd=pd, pr=pr)
    ctx.update(locals())
    if stop_after == "0":
        return finish(ctx)
    phase_A(ctx)
    if stop_after == "A":
        return finish(ctx)
    phase_B(ctx)
    if stop_after == "B":
        return finish(ctx)
    phase_CD(ctx)
    return finish(ctx)


def finish(ctx):
    P = ctx["P"]
    P.finalize()
    return ctx["nc"], ctx["dbg_out"]


def run_interleaved(*gens, weights=None):
    gens = list(gens)
    w = {id(g): (weights[i] if weights else 1) for i, g in enumerate(gens)}
    while gens:
        for g in list(gens):
            for _ in range(w[id(g)]):
                try:
                    next(g)
                except StopIteration:
                    gens.remove(g)
                    break


def phase_A(c):
    nc, P, b, T, R, dump = c["nc"], c["P"], c["b"], c["T"], c["R"], c["dump"]
    pbank, pbank_bf = c["pbank"], c["pbank_bf"]
    identb, r_identb, g1rep, r_g1, gqk, r_gqk, cs_tab, r_cs = (c[k] for k in
        ("identb", "r_identb", "g1rep", "r_g1", "gqk", "r_gqk", "cs_tab", "r_cs"))
    W_e, r_We, Tz, r_Tz, W_cr, W_ci, r_Wc, ER, EI, r_E, rho, r_rho, SR0, SI0, r_S0 = (c[k] for k in
        ("W_e", "r_We", "Tz", "r_Tz", "W_cr", "W_ci", "r_Wc", "ER", "EI", "r_E", "rho", "r_rho", "SR0", "SI0", "r_S0"))
    drep, r_drep = c["drep"], c["r_drep"]
    x_all, w_in = c["x_all"], c["w_in"]
    kT_scr, qT_scr, v_scr, z_scr = c["kT_scr"], c["qT_scr"], c["v_scr"], c["z_scr"]
    r_kT_scr, r_qT_scr, r_v_scr, r_z_scr = c["r_kT_scr"], c["r_qT_scr"], c["r_v_scr"], c["r_z_scr"]
    pd, pr = c["pd"], c["pr"]
    ksum, r_ksum = c["ksum"], c["r_ksum"]
    mA = P.mark()
    c["mA"] = mA

    WinA = T([8, 2048], BF16); r_WinA = R("WinA")
    for kc in range(8):
        P.dma("pool", WinA[:, kc, :], w_in[kc * 128:(kc + 1) * 128, 0:2048], writes=[r_WinA])

    xt = [T([D], F32) for _ in range(2)]; r_xt = [R("xt0"), R("xt1")]
    hb = [T([D], BF16) for _ in range(2)]; r_hb = [R("hb0"), R("hb1")]
    ss = T([16], F32); rs = T([16], F32); r_ss = [R(f"ss{i}") for i in range(16)]
    hT = T([8, 1024], BF16); r_hT = [R(f"hT{i}") for i in range(8)]
    pad = [None, [T([8, 64], BF16) for _ in range(2)]]
    r_pad = [[R(f"pad{w}{i}") for i in range(2)] for w in range(2)]
    ktt = [None, [T([8, 128], BF16) for _ in range(2)]]
    r_ktt = [[R(f"ktt{w}{i}") for i in range(2)] for w in range(2)]
    v_st = [T([8, 8, 65], BF16) for _ in range(2)]; r_vst = [R("v_st0"), R("v_st1")]
    for i in range(2):
        b.memset("pool", v_st[i], 1.0, [r_vst[i]])
    sqt = [T([8, 64], F32) for _ in range(2)]; r_sqt = [R("sqt0"), R("sqt1")]
    qn = [T([8, 64], F32) for _ in range(2)]; r_qn = [R("qn0"), R("qn1")]
    ssq = [T([8], F32) for _ in range(2)]; rq = [T([8], F32) for _ in range(2)]
    ra = [T([8, 8], F32) for _ in range(2)]; rb = [T([8, 8], F32) for _ in range(2)]
    assert P.arena_off <= P0_BASE, P.arena_off
    P.arena_off = P0_BASE
    pad[0] = [T([8, 64], BF16) for _ in range(2)]
    ktt[0] = [T([8, 128], BF16) for _ in range(2)]
    Ukj = T([32, 8, 16], BF16); r_Ukj = R("Ukj")
    U8 = T([32, 128], BF16); r_U8 = R("U8")
    NQ = 4
    SRf = T([NQ, 129], F32); SIf = T([NQ, 129], F32); r_Sf = R("Sf")
    zt_r = SRf[:, :, 0:128]; zt_i = SIf[:, :, 0:128]; r_zt = r_Sf
    srm = [T([NQ, 129], F32) for _ in range(2)]; r_srm = [R("srm0"), R("srm1")]
    ztm = [srm[0][:, :, 0:128], srm[1][:, :, 0:128]]; r_ztm = r_srm
    ZR = T([NQ, 129], F32); ZI = T([NQ, 129], F32); r_Z = R("Z")
    S_re = T([NQ, 128], BF16); S_im = T([NQ, 128], BF16); r_S = R("Sbf")
    du = [T([4, 128], F32) for _ in range(2)]; r_du = [R("du0"), R("du1")]
    ys = [T([4, 128], F32) for _ in range(2)]; y2 = [T([4, 128], F32) for _ in range(2)]
    r_ys = [R("ys0"), R("ys1")]; r_y2 = [R("y20"), R("y21")]
    Zst = T([8, 32, 16], BF16); r_Zst = R("Zst")
    print("phase A arena", P.arena_off)

    cnt = {"qkv": 0, "qk": 0}
    outs = {}

    def load_x(s, tt, par):
        row0 = s * 1024 + tt * 128
        P.dma("sp", xt[par], x_all[row0:row0 + 128, :], writes=[r_xt[par]])

    busy = {}

    def qkv_mm(tt, col0):
        i = 1 + cnt["qkv"] % 4
        cnt["qkv"] += 1
        assert not busy.get(i, False)
        psv, prs = pbank(i)
        for kc in range(8):
            b.mm(psv, hT[:, kc, tt * 128:(tt + 1) * 128], WinA[:, kc, col0:col0 + 512], kc == 0, kc == 7,
                 [r_hT[tt], r_WinA], [prs])
        return psv, prs

    def qkv_mm_nb(tt, col0):
        i = 1 + cnt["qkv"] % 4
        cnt["qkv"] += 1
        psv, prs = pbank(i)
        for kc in range(8):
            b.mm(psv, hT[:, kc, tt * 128:(tt + 1) * 128], WinA[:, kc, col0:col0 + 512], kc == 0, kc == 7,
                 [r_hT[tt], r_WinA], [prs])
        return psv, prs

    BACK_LAG = 3
    fstate = {"tick": 0, "done": -1}

    def gen_front(s):
        for _ in gen_front_(s):
            fstate["tick"] += 1
            yield
        fstate["done"] = s

    def gen_front_(s):
        own = is_own(s)
        for tt in range(8):
            par = tt % 2
            if tt < 7:
                load_x(s, tt + 1, 1 - par)
            elif s + 1 < NST:
                load_x(s + 1, 0, 1 - par)
            si = (s % 2) * 8 + tt
            b.act(hb[par], xt[par], AF.Square, [r_xt[par]], [r_hb[par], r_ss[si]], accum=ss[:, si:si + 1])
            b.act(rs[:, si:si + 1], ss[:, si:si + 1], AF.Sqrt, [r_ss[si]], [r_ss[si]], bias=EPS, scale=1.0 / D)
            b.recip(rs[:, si:si + 1], rs[:, si:si + 1], [r_ss[si]], [r_ss[si]])
            b.stt("dve", hb[par], xt[par], rs[:, si:si + 1], g1rep, ALU.mult, ALU.mult,
                  [r_xt[par], r_ss[si], r_g1], [r_hb[par]])
            yield
            ptv, ptr_ = pbank_bf(0)
            for kc in range(8):
                b.tr(ptv[:, kc * 128:(kc + 1) * 128], hb[par][:, kc * 128:(kc + 1) * 128], identb,
                     [r_hb[par], r_identb], [ptr_])
            b.cp("act", hT[:, :, tt * 128:(tt + 1) * 128], ptv.rearrange("p (k t) -> p k t", t=128),
                 [ptr_], [r_hT[tt]])
            yield
            while busy.get(1 + cnt["qkv"] % 4, False):
                yield
            busy[1 + cnt["qkv"] % 4] = True
            outs[(s, tt, 1)] = (1 + cnt["qkv"] % 4,) + tuple(qkv_mm_nb(tt, 1024)) + (fstate["tick"],)
            yield
            while busy.get(1 + cnt["qkv"] % 4, False):
                yield
            psv, prs = qkv_mm(tt, 1536)
            b.cp("act", v_st[s % 2][:, :, tt, 0:64], psv.rearrange("p (h d) -> p h d", d=64), [prs], [r_vst[s % 2]])
            yield
            if own:
                while busy.get(1 + cnt["qkv"] % 4, False):
                    yield
                busy[1 + cnt["qkv"] % 4] = True
                outs[(s, tt, 0)] = (1 + cnt["qkv"] % 4,) + tuple(qkv_mm_nb(tt, 512)) + (fstate["tick"],)
                yield
        P.dma("sp", v_scr[:, :, s * 8 * 65:(s + 1) * 8 * 65].rearrange("h p x -> p h x"),
              v_st[s % 2].rearrange("p h t d -> p h (t d)"), reads=[r_vst[s % 2]], writes=r_v_scr)

    def gen_back(s, which):
        own = is_own(s)
        for tt in range(8):
            ti = s * 8 + tt
            if True:
                while (s, tt, which) not in outs:
                    yield
                while fstate["done"] < s and fstate["tick"] < outs[(s, tt, which)][3] + BACK_LAG:
                    yield
                bank_i, psv, prs, _tk = outs.pop((s, tt, which))
                k_ = which if own else tt % 2
                ps3 = psv.rearrange("p (h d) -> p h d", d=64)
                b.act(sqt[k_], ps3, AF.Square, [prs], [r_sqt[k_]])
                b.reduce(ssq[k_], sqt[k_], ALU.add, [r_sqt[k_]], [r_sqt[k_]])
                b.act(rq[k_], ssq[k_], AF.Sqrt, [r_sqt[k_]], [r_sqt[k_]], bias=EPS, scale=1.0 / 64)
                b.recip(rq[k_], rq[k_], [r_sqt[k_]], [r_sqt[k_]])
                b.tt("dve", qn[k_], ps3, bc(rq[k_].unsqueeze(2), [128, 8, 64]), ALU.mult, [prs, r_sqt[k_]], [r_qn[k_]])
                busy[bank_i] = False
                yield
                b.tt("pool", qn[k_], qn[k_], bc(gqk[:, which, :].unsqueeze(1), [128, 8, 64]), ALU.mult,
                     [r_qn[k_], r_gqk], [r_qn[k_]])
                cosb = bc(cs_tab[:, ti, 0:8].unsqueeze(1), [128, 8, 8])
                sinb = bc(cs_tab[:, ti, 8:16].unsqueeze(1), [128, 8, 8])
                x1 = qn[k_][:, :, 0:8]; x2 = qn[k_][:, :, 8:16]
                pd_ = pad[which][k_]; rpd = r_pad[which][k_]
                rd = [r_qn[k_], r_cs]
                b.tt("pool", ra[k_], x1, cosb, ALU.mult, rd, [r_qn[k_]])
                b.tt("pool", rb[k_], x2, sinb, ALU.mult, rd, [r_qn[k_]])
                b.tt("pool", pd_[:, :, 0:8], ra[k_], rb[k_], ALU.subtract, [r_qn[k_]], [rpd])
                yield
                b.tt("pool", ra[k_], x2, cosb, ALU.mult, rd, [r_qn[k_]])
                b.tt("pool", rb[k_], x1, sinb, ALU.mult, rd, [r_qn[k_]])
                b.tt("pool", pd_[:, :, 8:16], ra[k_], rb[k_], ALU.add, [r_qn[k_]], [rpd])
                b.cp("act", pd_[:, :, 16:64], qn[k_][:, :, 16:64], [r_qn[k_]], [rpd])
                yield
                ptv, ptr_ = pbank_bf(5)
                for h in range(8):
                    b.tr(ptv[0:64, h * 128:(h + 1) * 128], pd_[:, h, :], identb, [rpd, r_identb], [ptr_])
                kt = ktt[which][k_]; rkt = r_ktt[which][k_]
                b.cp("dve", kt[0:64], ptv[0:64].rearrange("p (h t) -> p h t", t=128), [ptr_], [rkt])
                yield
                if which == 1:
                    b.reduce(ksum[0:64, :, ti], kt[0:64], ALU.add, [rkt], [r_ksum])
                    P.dma("sp", kT_scr[:, :, ti * 128:(ti + 1) * 128].rearrange("h d t -> d h t"), kt[0:64],
                          reads=[rkt], writes=r_kT_scr)
                else:
                    oti = (s // 2) * 8 + tt
                    P.dma("sp", qT_scr[:, :, oti * 128:(oti + 1) * 128].rearrange("h d t -> d h t"), kt[0:64],
                          reads=[rkt], writes=r_qT_scr)
                yield

    def gen_ssm_u(s):
        hT8 = hT.rearrange("p k (c j) -> p k j c", j=8)
        for j in range(8):
            psv, prs = pbank(6 + j % 2)
            for kc in range(8):
                b.mm(psv, hT8[:, kc, j, :], WinA[:, kc, 0:512], kc == 0, kc == 7, r_hT + [r_WinA], [prs])
            b.cp("act" if j % 2 else "dve", Ukj[:, :, j, :], psv.rearrange("p (g c) -> p g c", c=16), [prs], [r_Ukj])
            yield
        if "Ukj" in c["debug"] and s == 1:
            dump("Ukj", Ukj, [128, 32, 8, 16], [r_Ukj], BF16)

    def gen_ssm(s):
        own = is_own(s)
        for g0 in range(0, 32, 8):
            ptv, ptr_ = pbank_bf(6 + (g0 // 8) % 2)
            for gg in range(8):
                b.tr(ptv[:, gg * 128:(gg + 1) * 128], Ukj[:, g0 + gg, :, :].rearrange("p j c -> p (j c)"), identb,
                     [r_Ukj, r_identb], [ptr_])
            b.cp("dve", U8[:, g0:g0 + 8, :], ptv.rearrange("p (g k) -> p g k", k=128), [ptr_], [r_U8])
            yield
        for glq in range(16 // NQ):
            e_re, pre = pbank(6)
            e_im, pim = pbank(7)
            e_re = e_re.rearrange("p (g k) -> p g k", k=128)
            e_im = e_im.rearrange("p (g k) -> p g k", k=128)
            for gi in range(NQ):
                for gh in range(2):
                    g = gh * 16 + glq * NQ + gi
                    sl = slice(64 * gh, 64 * gh + 64)
                    b.mm(e_re[sl, gi, :], W_e[:, g, 0, :], U8[:, g, :], True, True, [r_We, r_U8], [pre])
                    b.mm(e_im[sl, gi, :], W_e[:, g, 1, :], U8[:, g, :], True, True, [r_We, r_U8], [pim])
            yield
            gsl = slice(glq * NQ, glq * NQ + NQ)
            E1r = ER[:, gsl, 1:129]; E1i = EI[:, gsl, 1:129]
            b.tt("dve", ztm[0], e_re, E1r, ALU.mult, [pre, r_E], [r_ztm[0]])
            b.tt("dve", ztm[1], e_im, E1i, ALU.mult, [pim, r_E], [r_ztm[1]])
            b.tt("pool", zt_r, ztm[0], ztm[1], ALU.add, r_ztm, [r_zt])
            yield
            b.tt("dve", ztm[0], e_im, E1r, ALU.mult, [pim, r_E], [r_ztm[0]])
            b.tt("dve", ztm[1], e_re, E1i, ALU.mult, [pre, r_E], [r_ztm[1]])
            b.tt("pool", zt_i, ztm[0], ztm[1], ALU.subtract, r_ztm, [r_zt])
            b.cp("pool", ZR[:, :, 0], SR0[:, gsl], [r_S0], [r_Z])
            b.cp("pool", ZI[:, :, 0], SI0[:, gsl], [r_S0], [r_Z])
            yield
            for gi in range(NQ):
                gl = glq * NQ + gi
                for (Zx, S0x, ztx) in ((ZR, SR0, zt_r), (ZI, SI0, zt_i)):
                    P.op("dve", (lambda Zx=Zx, S0x=S0x, ztx=ztx, gi=gi, gl=gl: (lambda e: e.tensor_tensor_scan(
                        out=Zx[:, gi, 1:129], data0=bc(rho[:, gl:gl + 1], [128, 128]), data1=ztx[:, gi, :],
                        initial=S0x[:, gl:gl + 1], op0=ALU.mult, op1=ALU.add)))(),
                        [r_zt, r_rho, r_S0], [r_Z])
                yield
            E0r = ER[:, gsl, :]; E0i = EI[:, gsl, :]
            b.tt("dve", srm[0], ZR, E0r, ALU.mult, [r_Z, r_E], [r_srm[0]])
            b.tt("pool", srm[1], ZI, E0i, ALU.mult, [r_Z, r_E], [r_srm[1]])
            b.tt("dve", SRf, srm[0], srm[1], ALU.subtract, r_srm, [r_Sf])
            yield
            b.tt("dve", srm[0], ZR, E0i, ALU.mult, [r_Z, r_E], [r_srm[0]])
            b.tt("pool", srm[1], ZI, E0r, ALU.mult, [r_Z, r_E], [r_srm[1]])
            b.tt("dve", SIf, srm[0], srm[1], ALU.add, r_srm, [r_Sf])
            b.cp("act", SR0[:, gsl], SRf[:, :, 128], [r_Sf], [r_S0])
            b.cp("act", SI0[:, gsl], SIf[:, :, 128], [r_Sf], [r_S0])
            yield
            if not own:
                continue
            b.cp("act", S_re, SRf[:, :, 0:128], [r_Sf], [r_S])
            b.cp("act", S_im, SIf[:, :, 0:128], [r_Sf], [r_S])
            for gh in range(2):
                psv, prs = pbank(6 + gh)
                sl = slice(64 * gh, 64 * gh + 64)
                gbase = gh * 16 + glq * NQ
                k_ = gh
                for gi in range(NQ):
                    g = gbase + gi
                    o = psv[:, gi * 128:(gi + 1) * 128]
                    b.mm(o, U8[:, g, :], Tz[:, g, :], True, False, [r_U8, r_Tz], [prs])
                    b.mm(o, S_re[sl, gi, :], W_cr[sl, glq * NQ + gi, :, :].rearrange("p i c -> p (i c)"),
                         False, False, [r_S, r_Wc], [prs])
                    b.mm(o, S_im[sl, gi, :], W_ci[sl, glq * NQ + gi, :, :].rearrange("p i c -> p (i c)"),
                         False, True, [r_S, r_Wc], [prs])
                b.tt("pool", du[k_].rearrange("p g (j c) -> p g j c", c=16), Ukj[:, gbase:gbase + 4, :, :],
                     bc(drep[:, gbase * 16:gbase * 16 + 64].rearrange("p (g c) -> p g c", c=16).unsqueeze(2),
                        [128, 4, 8, 16]), ALU.mult, [r_Ukj, r_drep], [r_du[k_]])
                yield
                b.tt("dve", ys[k_], psv.rearrange("p (g x) -> p g x", x=128), du[k_], ALU.add, [prs, r_du[k_]], [r_ys[k_]])
                b.tt("pool", y2[k_], ys[k_], ys[k_], ALU.mult, [r_ys[k_]], [r_y2[k_]])
                b.ts("pool", y2[k_], y2[k_], 0.044715, 1.0, ALU.mult, ALU.add, [r_y2[k_]], [r_y2[k_]])
                b.tt("pool", y2[k_], y2[k_], ys[k_], ALU.mult, [r_y2[k_], r_ys[k_]], [r_y2[k_]])
                yield
                b.act(y2[k_], y2[k_], AF.Sigmoid, [r_y2[k_]], [r_y2[k_]], scale=1.5957691216057308)
                b.tt("dve", Zst[:, :, gbase:gbase + 4, :].rearrange("p i g c -> p g i c"),
                     ys[k_].rearrange("p g (i c) -> p g i c", c=16), y2[k_].rearrange("p g (i c) -> p g i c", c=16),
                     ALU.mult, [r_ys[k_], r_y2[k_]], [r_Zst])
                yield
        if own:
            so = s // 2
            P.dma("sp", z_scr[so], Zst.rearrange("p i g c -> p (i g c)"), reads=[r_Zst], writes=[r_z_scr[so]])
            if "Zst" in c["debug"] and so == 0:
                dump("Zst", Zst, [128, 8, 32, 16], [r_Zst], BF16)

    def gen_p0():
        for k, th in enumerate(c["p0_ops"]):
            th()
            if k % 4 == 3:
                yield

    load_x(0, 0, 0)
    for s in range(NST):
        gens = [gen_front(s), gen_back(s, 1)]
        wts = [1, 1]
        if is_own(s):
            gens.append(gen_back(s, 0))
            wts.append(1)
        if s > 0:
            gens.append(gen_ssm(s - 1))
            wts.append(2 if is_own(s - 1) else 1)
        else:
            gens.append(gen_p0())
            wts.append(3)
        run_interleaved(*gens, weights=wts)
        if s == 0:
            P.barrier()
        run_interleaved(gen_ssm_u(s))
    run_interleaved(gen_ssm(NST - 1))
    dump("kT", kT_scr, [8, 64, 8192], r_kT_scr, BF16)
    dump("qT", qT_scr, [8, 64, 4096], r_qT_scr, BF16)
    dump("v", v_scr, [8, 128, 64 * 65], r_v_scr, BF16)
    dump("z", z_scr, [NST_OWN, 128, 4096], r_z_scr, BF16)
    dump("ksum", ksum, [128, 8, 64], [r_ksum])
    P.barrier()
    P.release(mA)


def prefetch_C(c):
    P, b, T, R = c["P"], c["b"], c["T"], c["R"]
    w_in, hs_scr = c["w_in"], c["hs_scr"]
    cw = {}
    zrow = T([D], BF16); r_zrow = R("czrow")
    b.memset("dve", zrow, 0.0, [r_zrow])
    hz_stream = P.stream("hz")
    r_hz = []
    for j in range(96):
        rj = R(f"chz{j}")
        rj.stream = hz_stream
        r_hz.append(rj)
        P.dma("sp", hs_scr[j * 128:(j + 1) * 128, :], zrow, reads=[r_zrow], writes=[rj])
    WinC = T([8, 2048], BF16); r_WinC = R("WinC")
    for kc in range(8):
        P.dma("pool", WinC[:, kc, :], w_in[kc * 128:(kc + 1) * 128, 2048:4096], writes=[r_WinC])
    Wglu = T([4, 512], BF16); Wps = T([4, 1024], BF16); Wpa = T([4, 1024], BF16); Wo = T([8, 1024], BF16)
    Wr = T([8, 36], BF16); r_W = R("Wsmall")
    P.dma("pool", Wglu, c["w_glu"].rearrange("(kc p) n -> p kc n", p=128), writes=[r_W])
    P.dma("pool", Wps, c["w_proj_ssm"].rearrange("(kc p) n -> p kc n", p=128), writes=[r_W])
    P.dma("pool", Wpa, c["w_proj_attn"].rearrange("(kc p) n -> p kc n", p=128), writes=[r_W])
    P.dma("pool", Wo, c["w_out"].rearrange("(kc p) n -> p kc n", p=128), writes=[r_W])
    P.dma("pool", Wr, c["w_router"].rearrange("(kc p) n -> p kc n", p=128), writes=[r_W])
    brow = T([512 + 36], BF16)
    P.dma("pool", brow[0:1, 0:512], c["b_glu"], writes=[r_W])
    P.dma("pool", brow[0:1, 512:548], c["b_router"], writes=[r_W])
    g2rep = T([D], F32)
    P.dma("pool", g2rep, c["norm2_g"][0:1, :].partition_broadcast(128), writes=[r_W])
    for k in ("r_hz", "WinC", "r_WinC", "Wglu", "Wps", "Wpa", "Wo", "Wr", "r_W", "brow", "g2rep"):
        cw[k] = locals()[k]
    c["cw"] = cw
    print("prefetch_C arena", P.arena_off)


def phase_B(c):
    nc, P, b, T, R, dump = c["nc"], c["P"], c["b"], c["T"], c["R"], c["dump"]
    pbank, pbank_bf = c["pbank"], c["pbank_bf"]
    identb, r_identb = c["identb"], c["r_identb"]
    ksum, r_ksum = c["ksum"], c["r_ksum"]
    kT_scr, qT_scr, v_scr, att_scr = c["kT_scr"], c["qT_scr"], c["v_scr"], c["att_scr"]
    r_kT_scr, r_qT_scr, r_v_scr, r_att_scr = c["r_kT_scr"], c["r_qT_scr"], c["r_v_scr"], c["r_att_scr"]
    P.release(c["m_ssm"])
    mB = P.mark()
    cvf = [T([1024], F32) for _ in range(3)]; r_cvf = [R(f"cvf{i}") for i in range(3)]
    cvb = [T([1024], BF16) for _ in range(3)]; r_cvb = [R(f"cvb{i}") for i in range(3)]
    r_wgu = c["r_wgu"]; r_wd = c["r_wd"]

    def conv_steps():
        jobs = []
        for e in range(32):
            jobs.append((c["w_gate"][e].rearrange("(kc p) f -> p kc f", p=128), c["wgu_scr"][e, :, 0], r_wgu[e], True))
            jobs.append((c["w_up"][e].rearrange("(kc p) f -> p kc f", p=128), c["wgu_scr"][e, :, 1], r_wgu[e], True))
            jobs.append((c["w_down"][e], c["wd_scr"][e], r_wd[e], False))
        n = len(jobs)
        for k in range(n + 2):
            if k < n:
                src, dst, rr_, three = jobs[k]
                o = cvf[k % 3].rearrange("p (kc f) -> p kc f", f=128) if three else cvf[k % 3]
                P.dma("sp", o, src, writes=[r_cvf[k % 3]])
            if 0 <= k - 1 < n:
                j = k - 1
                b.cp("pool", cvb[j % 3], cvf[j % 3], [r_cvf[j % 3]], [r_cvb[j % 3]])
            if 0 <= k - 2 < n:
                j = k - 2
                src, dst, rr_, three = jobs[j]
                i_ = cvb[j % 3].rearrange("p (kc f) -> p kc f", f=128) if three else cvb[j % 3]
                P.dma("sp", dst, i_, reads=[r_cvb[j % 3]], writes=[rr_])
            yield

    conv = conv_steps()

    kaug = [T([8192], BF16) for _ in range(2)]; r_kaug = [R("kaug0"), R("kaug1")]
    r_k1h = [R("k1h0"), R("k1h1")]
    qaug = [T([4096], BF16) for _ in range(2)]
    r_qd = [R("qd0"), R("qd1")]
    r_qb = [[R(f"qb{i}_{t}") for t in range(32)] for i in range(2)]
    vb = [T([64, 65], BF16) for _ in range(2)]; r_vb = [R("vb0"), R("vb1")]
    att_tok = T([32, 512], BF16); r_att = R("att_tok")
    kmeanT = T([8, 32], BF16); r_km = R("kmeanT")
    kmf = T([8, 32], F32)
    pbrep = T([16, 32], F32); pbm = T([16, 32], F32); r_pb = R("pb")
    tri = T([128], BF16); r_tri = R("tri")
    biaspad = [T([96], BF16) for _ in range(2)]; r_bp = [R("bp0"), R("bp1")]
    sm = [T([32], F32) for _ in range(2)]; mx8 = [T([8], F32) for _ in range(2)]; sel = [T([32], F32) for _ in range(2)]
    r_sm = [R("sm0"), R("sm1")]
    PTb = [T([512], BF16) for _ in range(4)]; r_PT = [R(f"PT{i}") for i in range(4)]
    rinv = [T([2], F32) for _ in range(2)]; r_rinv = [R("rinv0"), R("rinv1")]
    print("phase B arena", P.arena_off)
    c["cw_base"] = P.arena_off

    P.dma("sp", pbrep, c["pbrep_in"].rearrange("p (a b) -> p a b", b=32), writes=[r_pb])
    b.ts("dve", pbm, pbrep, NEG, None, ALU.add, None, [r_pb], [r_pb])
    P.dma("pool", tri, c["tri_in"], writes=[r_tri])
    for i in range(2):
        P.dma("pool", kaug[i][64:96, :], c["onehot_in"], writes=[r_k1h[i]])
        b.memset("dve", biaspad[i], 0.0, [r_bp[i]])
    b.reduce(kmf[0:64], ksum[0:64].rearrange("p h (n two) -> p h n two", two=2), ALU.add, [r_ksum], [r_km])
    b.ts("dve", kmeanT[0:64], kmf[0:64], 1.0 / 256, None, ALU.mult, None, [r_km], [r_km])

    def load_head(h):
        hb_ = h % 2
        P.dma("sp", kaug[hb_][0:64, :], kT_scr[h], reads=[r_kT_scr[h]], writes=[r_kaug[hb_]])
        P.dma("sp", qaug[hb_][0:64, :], qT_scr[h], reads=[r_qT_scr[h]], writes=[r_qd[hb_]])
        P.dma("sp", vb[hb_], v_scr[h].rearrange("p (t d) -> p t d", d=65), reads=[r_v_scr[h]], writes=[r_vb[hb_]])

    rt_cnt = {"n": 0}

    def route_stage1(h, t):
        hb_ = h % 2
        k_ = rt_cnt["n"] % 2
        rt_cnt["n"] += 1
        ob = t // 2
        psv, prs = pbank(0)
        sc = psv[:, (t % 8) * 32:(t % 8) * 32 + 32]
        b.mm(sc, qaug[hb_][0:64, t * 128:(t + 1) * 128], kmeanT[0:64, h, :], True, True, [r_qd[hb_], r_km], [prs])
        b.tt("dve", sm[k_], sc, pbrep[:, ob, :], ALU.add, [prs, r_pb], [r_sm[k_]])
        P.op("dve", lambda e: e.max(out=mx8[k_], in_=sm[k_]), [r_sm[k_]], [r_sm[k_]])
        b.ts("dve", sel[k_], sm[k_], mx8[k_][:, 2:3], None, ALU.is_ge, None, [r_sm[k_]], [r_sm[k_]])
        b.stt("dve", biaspad[k_][:, 64:96], sel[k_], -NEG, pbm[:, ob, :], ALU.mult, ALU.add, [r_sm[k_], r_pb], [r_bp[k_]])
        return k_

    def route_stage2(h, t, k_):
        hb_ = h % 2
        ptv, ptr_ = pbank_bf(0)
        o = ptv[0:96, 512 + (t % 4) * 128:512 + (t % 4) * 128 + 128]
        b.tr(o, biaspad[k_], identb, [r_bp[k_], r_identb], [ptr_])
        b.cp("dve", qaug[hb_][64:96, t * 128:(t + 1) * 128], o[64:96], [ptr_], [r_qb[hb_][t]])

    NSTB = 5
    LA = 4

    def emitS(h, ob, n, diag, slot):
        hb_ = h % 2
        psv, prs = pbank(1 + slot % NSTB)
        qc = slice(256 * ob, 256 * ob + 256)
        for half in range(2):
            kc = slice(n * 256 + half * 128, n * 256 + half * 128 + 128)
            if diag:
                b.mm(psv[:, half * 256:half * 256 + 256], kaug[hb_][0:64, kc], qaug[hb_][0:64, qc], True, True,
                     [r_kaug[hb_], r_qd[hb_]], [prs])
            else:
                b.mm(psv[:, half * 256:half * 256 + 256], kaug[hb_][0:96, kc], qaug[hb_][0:96, qc], True, True,
                     [r_kaug[hb_], r_k1h[hb_], r_qd[hb_], r_qb[hb_][2 * ob], r_qb[hb_][2 * ob + 1]], [prs])

    def emitPV(h, ob, n, diag, slot, first):
        hb_ = h % 2
        psv, prs = pbank(1 + slot % NSTB)
        pt = PTb[slot % 4]; rpt = r_PT[slot % 4]
        b.act(pt, psv, AF.Exp, [prs], [rpt], scale=0.125)
        pov, pors = pbank(6 + ob % 2)
        if diag:
            b.tt("pool", pt[:, 0:128], pt[:, 0:128], tri, ALU.mult, [rpt, r_tri], [rpt])
            b.tt("pool", pt[:, 384:512], pt[:, 384:512], tri, ALU.mult, [rpt, r_tri], [rpt])
        for j in range(2):
            for half in range(2):
                if diag and j == 0 and half == 1:
                    continue
                last = diag and (half == 1 or j == 0)
                b.mm(pov[:, j * 128:j * 128 + 65], pt[:, half * 256 + j * 128:half * 256 + j * 128 + 128],
                     vb[hb_][:, n * 2 + half, :], first and half == 0 and j == 0, last, [rpt, r_vb[hb_]], [pors])
        if diag:
            k_ = ob % 2
            po3 = pov[:, 0:256].rearrange("p (j x) -> p j x", x=128)
            b.recip(rinv[k_], po3[:, :, 64], [pors], [r_rinv[k_]])
            for j in range(2):
                b.ts("dve", att_tok[:, 2 * ob + j, h * 64:(h + 1) * 64], pov[:, j * 128:j * 128 + 64],
                     rinv[k_][:, j:j + 1], None, ALU.mult, None, [pors, r_rinv[k_]], [r_att])

    load_head(0)
    for t in range(32):
        k_ = route_stage1(0, t)
        route_stage2(0, t, k_)
    for h in range(8):
        if h + 1 < 8:
            load_head(h + 1)
        if h == 0:
            prefetch_C(c)
        items = []
        for ob in range(16):
            for n in range(q_block(ob)):
                items.append((ob, n, False, n == 0))
            items.append((ob, q_block(ob), True, False))
        pend = {}
        nxt_t = 0
        for i in range(len(items) + LA):
            if i < len(items):
                ob, n, diag, first = items[i]
                emitS(h, ob, n, diag, i)
            if i - LA >= 0:
                ob, n, diag, first = items[i - LA]
                emitPV(h, ob, n, diag, i - LA, first)
            if i % 12 == 6:
                next(conv, None)
            if h + 1 < 8:
                if i % 9 == 0 and nxt_t < 32:
                    pend[i + 5] = (nxt_t, route_stage1(h + 1, nxt_t))
                    nxt_t += 1
                if i in pend:
                    t_, k_ = pend.pop(i)
                    route_stage2(h + 1, t_, k_)
        for i in sorted(pend):
            t_, k_ = pend[i]
            route_stage2(h + 1, t_, k_)
        assert nxt_t == 32 or h == 7
    for _ in conv:
        pass
    P.dma("sp", att_scr.rearrange("(t p) c -> p t c", p=128), att_tok, reads=[r_att], writes=[r_att_scr])
    dump("att", att_tok, [128, 32, 512], [r_att], BF16)
    dump("qaug7", qaug[1], [128, 4096], [r_qd[1]] + r_qb[1], BF16)
    dump("kmeanT", kmeanT, [128, 8, 32], [r_km], BF16)
    P.barrier()
    P.release(mB)


def phase_CD(c):
    nc, P, b, T, R, dump = c["nc"], c["P"], c["b"], c["T"], c["R"], c["dump"]
    pbank, pbank_bf = c["pbank"], c["pbank_bf"]
    identb, r_identb, g1rep, r_g1 = c["identb"], c["r_identb"], c["g1rep"], c["r_g1"]
    x_all, w_in, y_out = c["x_all"], c["w_in"], c["y_out"]
    z_scr, att_scr, r_z_scr, r_att_scr = c["z_scr"], c["att_scr"], c["r_z_scr"], c["r_att_scr"]
    wgu_scr, wd_scr, r_wgu, r_wd = c["wgu_scr"], c["wd_scr"], c["r_wgu"], c["r_wd"]
    hs_scr, ys_scr = c["hs_scr"], c["ys_scr"]
    P.release(c["m_ssm"])
    NT = 32
    NSL = 96

    M1all = T([NT, 32], F32); M2all = T([NT, 32], F32); r_M = R("cMall")
    w12 = T([2, NT], F32); r_w12 = R("cw12")
    h2_scr = c["h2_scr"]
    h2s_stream = P.stream("h2s")
    r_h2s = []
    for t in range(NT):
        rj = R(f"ch2s{t}")
        rj.stream = h2s_stream
        r_h2s.append(rj)
    slot_i = T([2, NT], I32); r_slot = R("cslot")
    widx_i = T([NSL], I32); r_widx = R("cwidx")
    rconst = T([32 + NSL + 1], F32); r_rc = R("crconst")
    P.dma("sp", rconst, c["rconst_in"], writes=[r_rc])
    tri2 = T([128], BF16); r_tri2 = R("ctri2")
    P.dma("pool", tri2, c["tri_in"], writes=[r_tri2])
    ones = T([128], BF16); r_ones = R("cones")
    b.memset("dve", ones, 1.0, [r_ones])
    cw = c["cw"]
    r_hz, WinC, r_WinC, Wglu, Wps, Wpa, Wo, Wr, r_W, brow, g2rep = (cw[k] for k in
        ("r_hz", "WinC", "r_WinC", "Wglu", "Wps", "Wpa", "Wo", "Wr", "r_W", "brow", "g2rep"))
    mC = P.mark()

    xt = [T([D], F32) for _ in range(2)]; r_xt = [R("cxt0"), R("cxt1")]
    zt = [T([512], BF16) for _ in range(2)]; r_zt = [R("czt0"), R("czt1")]
    at = [T([512], BF16) for _ in range(2)]; r_at = [R("cat0"), R("cat1")]
    x1 = [T([D], F32) for _ in range(2)]; r_x1 = [R("cx1a"), R("cx1b")]
    r_y = [R(f"y_out{i}") for i in range(32)]
    y_stream = P.stream("ypark")
    for r_ in r_y:
        r_.stream = y_stream

    class PB:
        pass
    pbs = []
    for q in range(2):
        o = PB()
        o.hb = T([D], BF16); o.r_hb = R(f"chb{q}")
        o.st = T([8], F32); o.r_st = R(f"cst{q}")
        o.hTt = T([8, 128], BF16); o.r_hTt = R(f"chTt{q}")
        o.sgt = T([2048], BF16); o.r_sgt = R(f"csg{q}")
        o.zT = T([4, 128], BF16); o.r_zT = R(f"czT{q}")
        o.sgl = T([512], F32); o.r_sgl = R(f"csgl{q}")
        o.glu = T([512], BF16); o.r_glu = R(f"cglu{q}")
        o.gluT = T([4, 128], BF16); o.r_gluT = R(f"cgluT{q}")
        o.attT = T([4, 128], BF16); o.r_attT = R(f"cattT{q}")
        o.mg = T([D], BF16); o.r_mg = R(f"cmg{q}")
        o.mT = T([8, 128], BF16); o.r_mT = R(f"cmT{q}")
        o.h2 = T([D], BF16); o.r_h2 = R(f"ch2{q}")
        o.h2Tt = T([8, 128], BF16); o.r_h2Tt = R(f"ch2Tt{q}")
        o.lg = T([36], F32); o.rw = T([16], F32); o.gm = T([4], F32); o.em = T([4, 8], F32); o.mx = T([8], F32)
        o.r_rt = R(f"crt{q}")
        o.mm1 = T([512], F32); o.mm2 = T([512], F32); o.r_m1 = R(f"cm1{q}"); o.r_m2 = R(f"cm2{q}")
        o.bT, o.bM0, o.bM1, o.bX = (4 * q, 4 * q + 1, 4 * q + 2, 4 * q + 3)
        pbs.append(o)
    print("phase C arena", P.arena_off)
    assert P.arena_off <= c["cw_base"]

    def tile_src(base_ap, S, i):
        return base_ap[1024 * S:1024 * S + 1024].rearrange("(k j) d -> j k d", j=8)[i]

    def load_tile(tidx):
        S, i = tiles[tidx]
        par = tidx % 2
        P.dma("sp", xt[par], tile_src(x_all, 2 * S + 1, i), writes=[r_xt[par]])
        P.dma("sp", zt[par], z_scr[S][:, i * 512:(i + 1) * 512], reads=[r_z_scr[S]], writes=[r_zt[par]])
        P.dma("sp", at[par], tile_src(att_scr, S, i), reads=[r_att_scr], writes=[r_at[par]])

    def transposes(dst, r_dst, src, r_src, n, bank):
        ptv, ptr_ = pbank_bf(bank)
        for kc in range(n):
            b.tr(ptv[:, kc * 128:(kc + 1) * 128], src[:, kc * 128:(kc + 1) * 128], identb, [r_src, r_identb], [ptr_])
        b.cp("act", dst, ptv[:, 0:n * 128].rearrange("p (k t) -> p k t", t=128), [ptr_], [r_dst])

    tiles = [(S, i) for S in range(NST_OWN) for i in range(8)]

    def gen_C(par):
        o = pbs[par]
        for tidx in range(par, NT, 2):
            S, i = tiles[tidx]
            b.act(o.hb, xt[par], AF.Square, [r_xt[par]], [o.r_hb, o.r_st], accum=o.st[:, 0:1])
            b.act(o.st[:, 1:2], o.st[:, 0:1], AF.Sqrt, [o.r_st], [o.r_st], bias=EPS, scale=1.0 / D)
            b.recip(o.st[:, 1:2], o.st[:, 1:2], [o.r_st], [o.r_st])
            b.stt("dve", o.hb, xt[par], o.st[:, 1:2], g1rep, ALU.mult, ALU.mult, [r_xt[par], o.r_st, r_g1], [o.r_hb])
            yield
            transposes(o.hTt, o.r_hTt, o.hb, o.r_hb, 8, o.bT)
            yield
            for cc in range(4):
                psv, prs = pbank(o.bM0 if cc % 2 == 0 else o.bM1)
                for kc in range(8):
                    b.mm(psv, o.hTt[:, kc, :], WinC[:, kc, cc * 512:(cc + 1) * 512], kc == 0, kc == 7, [o.r_hTt, r_WinC], [prs])
                b.act(o.sgt[:, cc * 512:(cc + 1) * 512], psv, AF.Sigmoid, [prs], [o.r_sgt])
                yield
            transposes(o.zT, o.r_zT, zt[par], r_zt[par], 4, o.bX)
            yield
            psv, prs = pbank(o.bM0)
            for kc in range(4):
                b.mm(psv, o.zT[:, kc, :], Wglu[:, kc, :], kc == 0, False, [o.r_zT, r_W], [prs])
            b.mm(psv, ones[0:1, :], brow[0:1, 0:512], False, True, [r_W, r_ones], [prs])
            b.act(o.sgl, psv, AF.Sigmoid, [prs], [o.r_sgl])
            b.tt("pool", o.glu, zt[par], o.sgl, ALU.mult, [r_zt[par], o.r_sgl], [o.r_glu])
            transposes(o.attT, o.r_attT, at[par], r_at[par], 4, o.bT)
            yield
            transposes(o.gluT, o.r_gluT, o.glu, o.r_glu, 4, o.bX)
            yield
            for cc in range(2):
                ps1, pr1 = pbank(o.bM0)
                ps2, pr2 = pbank(o.bM1)
                for kc in range(4):
                    b.mm(ps2, o.attT[:, kc, :], Wpa[:, kc, cc * 512:(cc + 1) * 512], kc == 0, kc == 3, [o.r_attT, r_W], [pr2])
                for kc in range(4):
                    b.mm(ps1, o.gluT[:, kc, :], Wps[:, kc, cc * 512:(cc + 1) * 512], kc == 0, kc == 3, [o.r_gluT, r_W], [pr1])
                b.tt("dve", o.mm2, ps2, o.sgt[:, 1024 + cc * 512:1024 + (cc + 1) * 512], ALU.mult, [pr2, o.r_sgt], [o.r_m2])
                b.tt("dve", o.mm1, ps1, o.sgt[:, cc * 512:(cc + 1) * 512], ALU.mult, [pr1, o.r_sgt], [o.r_m1])
                b.tt("pool", o.mg[:, cc * 512:(cc + 1) * 512], o.mm1, o.mm2, ALU.add, [o.r_m1, o.r_m2], [o.r_mg])
                yield
            transposes(o.mT, o.r_mT, o.mg, o.r_mg, 8, o.bT)
            yield
            for cc in range(2):
                psv, prs = pbank(o.bM0 if cc == 0 else o.bM1)
                for kc in range(8):
                    b.mm(psv, o.mT[:, kc, :], Wo[:, kc, cc * 512:(cc + 1) * 512], kc == 0, kc == 7, [o.r_mT, r_W], [prs])
                b.tt("dve", x1[par][:, cc * 512:(cc + 1) * 512], psv, xt[par][:, cc * 512:(cc + 1) * 512], ALU.add,
                     [prs, r_xt[par]], [r_x1[par]])
            P.dma("sp", tile_src(y_out, S, i), x1[par], reads=[r_x1[par]], writes=[r_y[tidx]])
            if tidx + 2 < NT:
                load_tile(tidx + 2)
            yield
            h2 = o.h2; r_h2 = o.r_h2
            b.act(h2, x1[par], AF.Square, [r_x1[par]], [r_h2, o.r_st], accum=o.st[:, 2:3])
            b.act(o.st[:, 3:4], o.st[:, 2:3], AF.Sqrt, [o.r_st], [o.r_st], bias=EPS, scale=1.0 / D)
            b.recip(o.st[:, 3:4], o.st[:, 3:4], [o.r_st], [o.r_st])
            b.stt("dve", h2, x1[par], o.st[:, 3:4], g2rep, ALU.mult, ALU.mult, [r_x1[par], o.r_st, r_W], [r_h2])
            P.dma("sp", h2_scr[tidx * 128:(tidx + 1) * 128, :], h2, reads=[r_h2], writes=[r_h2s[tidx]])
            yield
            transposes(o.h2Tt, o.r_h2Tt, h2, r_h2, 8, o.bX)
            yield
            psv, prs = pbank(o.bM0)
            for kc in range(8):
                b.mm(psv[:, 0:36], o.h2Tt[:, kc, :], Wr[:, kc, :], kc == 0, False, [o.r_h2Tt, r_W], [prs])
            b.mm(psv[:, 0:36], ones[0:1, :], brow[0:1, 512:548], False, True, [r_W, r_ones], [prs])
            rt = [o.r_rt]
            lg, rw, gm, em, mx = o.lg, o.rw, o.gm, o.em, o.mx
            b.cp("dve", lg, psv[:, 0:36], [prs], rt)
            b.reduce(rw[:, 0:1], lg[:, 0:4], ALU.max, rt, rt)
            b.ts("dve", rw[:, 1:2], rw[:, 0:1], -1.0, None, ALU.mult, None, rt, rt)
            b.act(gm, lg[:, 0:4], AF.Exp, rt, rt, bias=rw[:, 1:2], scale=1.0, accum=rw[:, 2:3])
            b.recip(rw[:, 3:4], rw[:, 2:3], rt, rt)
            yield
            b.ts("dve", gm, lg[:, 0:4], rw[:, 0:1], None, ALU.is_ge, None, rt, rt)
            b.ts("dve", gm, gm, 10000.0, -10000.0, ALU.mult, ALU.add, rt, rt)
            b.tt("dve", em, lg[:, 4:36].rearrange("p (g e) -> p g e", e=8), bc(gm.unsqueeze(2), [128, 4, 8]), ALU.add, rt, rt)
            P.op("dve", (lambda mx=mx, em=em: (lambda e: e.max(out=mx, in_=em.rearrange("p g e -> p (g e)"))))(), rt, rt)
            b.tt("dve", rw[:, 4:5], mx[:, 0:1], mx[:, 1:2], ALU.subtract, rt, rt)
            b.act(rw[:, 5:6], rw[:, 4:5], AF.Sigmoid, rt, rt)
            yield
            b.tt("dve", w12[:, 0, tidx:tidx + 1], rw[:, 5:6], rw[:, 3:4], ALU.mult, rt, [r_w12])
            b.tt("dve", w12[:, 1, tidx:tidx + 1], rw[:, 3:4], w12[:, 0, tidx:tidx + 1], ALU.subtract, rt + [r_w12], [r_w12])
            emf = em.rearrange("p g e -> p (g e)")
            b.ts("dve", M1all[:, tidx, :], emf, mx[:, 0:1], None, ALU.is_equal, None, rt, [r_M])
            b.ts("dve", M2all[:, tidx, :], emf, mx[:, 1:2], None, ALU.is_equal, None, rt, [r_M])
            yield

    load_tile(0)
    load_tile(1)
    g0 = gen_C(0)
    g1 = gen_C(1)
    for _ in range(8):
        next(g0)
    run_interleaved(g0, g1)
    P.barrier()
    P.release(mC)

    rr = R("cR"); rt = [rr]
    M1f = M1all.rearrange("p t e -> p (t e)"); M2f = M2all.rearrange("p t e -> p (t e)")
    Mf = T([1024], F32); Mb = T([1024], BF16); r_Mb = R("cMb")
    cumS = T([1024], F32); totS = T([1024], F32); r_cum = R("ccum"); r_tot = R("ctot")
    b.tt("dve", Mf, M1f, M2f, ALU.add, [r_M], rt)
    b.cp("dve", Mb, Mf, rt, [r_Mb])
    for hf in range(2):
        sl = slice(hf * 512, hf * 512 + 512)
        pv, prs = pbank(hf)
        b.mm(pv, tri2, Mb[:, sl], True, True, [r_tri2, r_Mb], [prs])
        b.cp("act", cumS[:, sl], pv, [prs], [r_cum])
        pv2, prs2 = pbank(2 + hf)
        b.mm(pv2, ones, Mb[:, sl], True, True, [r_ones, r_Mb], [prs2])
        b.cp("dve", totS[:, sl], pv2, [prs2], [r_tot])
    thr = rconst[:, 0:32]; jv = rconst[:, 32:32 + NSL]; pidx = rconst[:, 32 + NSL:33 + NSL]
    cnt_e = T([32], F32); ntile = T([32], F32); base = T([32], F32)
    cmp3 = T([NSL, 32], F32)
    pbuf = [T([1024], F32) for _ in range(2)]
    sbuf_ = [T([32], F32) for _ in range(2)]

    def prefix(src, n, unit, bufs):
        cur = src; s = 1; k = 0
        L = n * unit
        while s < n:
            dst = bufs[k % 2]; k += 1
            sh = s * unit
            b.cp("dve", dst[:, 0:sh], cur[:, 0:sh], rt + [r_tot], rt)
            b.tt("dve", dst[:, sh:L], cur[:, sh:L], cur[:, 0:L - sh], ALU.add, rt + [r_tot], rt)
            cur = dst; s *= 2
        return cur

    b.reduce(cnt_e, totS.rearrange("p (t e) -> p e t", e=32), ALU.add, [r_tot], rt)
    c3 = cmp3[:, 0:32, :]
    b.tt("dve", c3, bc(cnt_e.unsqueeze(2), [128, 32, 32]), bc(thr.unsqueeze(1), [128, 32, 32]), ALU.is_gt, rt + [r_rc], rt)
    b.reduce(ntile, c3, ALU.add, rt, rt)
    cti = prefix(ntile, 32, 1, sbuf_)
    b.tt("dve", base, cti, ntile, ALU.subtract, rt, rt)
    b.ts("dve", base, base, 128.0, None, ALU.mult, None, rt, rt)
    inc_t = prefix(totS, 32, 32, pbuf)
    G = T([1024], F32)
    b.tt("dve", G, inc_t, totS, ALU.subtract, rt + [r_tot], rt)
    G3 = G.rearrange("p (t e) -> p t e", e=32)
    b.tt("dve", G3, G3, bc(base.unsqueeze(1), [128, 32, 32]), ALU.add, rt, rt)
    b.tt("dve", G, G, cumS, ALU.add, rt + [r_cum], rt)
    sl_f = T([2, NT], F32)
    q1 = T([1024], F32)
    for k, Mk in ((0, M1f), (1, M2f)):
        b.tt("dve", q1, G, Mk, ALU.mult, rt + [r_M], rt)
        b.reduce(sl_f[:, k, :], q1.rearrange("p (t e) -> p t e", e=32), ALU.add, rt, rt)
    b.ts("dve", sl_f, sl_f, -1.0, float(NSL * 128 - 1), ALU.add, ALU.min, rt, rt)
    b.ts("dve", sl_f, sl_f, 0.0, None, ALU.max, None, rt, rt)
    b.cp("dve", slot_i, sl_f, rt, [r_slot])
    b.tt("dve", cmp3, bc(cti.unsqueeze(1), [128, NSL, 32]), bc(jv.unsqueeze(2), [128, NSL, 32]), ALU.is_le, rt + [r_rc], rt)
    eid = T([NSL], F32)
    b.reduce(eid, cmp3, ALU.add, rt, rt)
    b.ts("dve", eid, eid, 31.0, 128.0, ALU.min, ALU.mult, rt, rt)
    b.tt("dve", eid, eid, bc(pidx, [128, NSL]), ALU.add, rt + [r_rc], rt)
    b.cp("dve", widx_i, eid, rt, [r_widx])
    dump("slot_i", slot_i, [128, 2, NT], [r_slot], I32)
    dump("widx_i", widx_i, [128, NSL], [r_widx], I32)
    dump("w12", w12, [128, 2, NT], [r_w12])
    dump("M1all", M1all, [128, NT, 32], [r_M])
    dump("M2all", M2all, [128, NT, 32], [r_M])

    sc_stream = P.stream("sc")
    r_sc = []
    h2l = [T([D], BF16) for _ in range(8)]; r_h2l = [R(f"ch2l{i}") for i in range(8)]
    for t in range(NT):
        P.dma("sp", h2l[t % 8], h2_scr[t * 128:(t + 1) * 128, :], reads=r_h2s, writes=[r_h2l[t % 8]])
        for k in range(2):
            rj = R(f"csc{t}_{k}")
            rj.stream = sc_stream
            r_sc.append(rj)
            P.idma(hs_scr[:, :], h2l[t % 8], slot_i[:, k, t:t + 1], False,
                   reads=[r_h2l[t % 8], r_slot] + r_hz, writes=[rj])

    NB = 6
    PF = 4
    hs = [T([D], BF16) for _ in range(NB)]; r_hs = [R(f"dhs{i}") for i in range(NB)]
    wgu = [T([2, 8, 128], BF16) for _ in range(NB)]; r_wgs = [R(f"dwgu{i}") for i in range(NB)]
    wdn = [T([D], BF16) for _ in range(NB)]; r_wds = [R(f"dwdn{i}") for i in range(NB)]
    hsT = [T([8, 128], BF16) for _ in range(2)]; r_hsT = [R("dhsT0"), R("dhsT1")]
    sg = [T([128], F32) for _ in range(2)]; r_sg = [R("dsg0"), R("dsg1")]
    actb = [T([128], BF16) for _ in range(2)]; r_actb = [R("dact0"), R("dact1")]
    yt = [T([D], F32) for _ in range(2)]; r_yt = [R("dyt0"), R("dyt1")]
    ys_stream = P.stream("ys")
    r_ys = []
    wgu2d = wgu_scr.rearrange("e p g k f -> (e p) (g k f)")
    wd2d = wd_scr.rearrange("e p d -> (e p) d")
    print("phase D arena", P.arena_off)

    def d_load(j):
        s4 = j % NB
        P.dma("sp", hs[s4], hs_scr[j * 128:(j + 1) * 128, :], reads=r_sc, writes=[r_hs[s4]])
        P.idma(wgu[s4].rearrange("p g k f -> p (g k f)"), wgu2d, widx_i[:, j:j + 1], True,
               reads=[r_widx] + list(set(r_wgu)), writes=[r_wgs[s4]])
        P.idma(wdn[s4], wd2d, widx_i[:, j:j + 1], True, reads=[r_widx] + list(set(r_wd)), writes=[r_wds[s4]])

    def d_T(j):
        s4 = j % NB; s2 = j % 2
        ptv, ptr_ = pbank_bf(s2)
        for kc in range(8):
            b.tr(ptv[:, kc * 128:(kc + 1) * 128], hs[s4][:, kc * 128:(kc + 1) * 128], identb, [r_hs[s4], r_identb], [ptr_])
        b.cp("act", hsT[s2], ptv.rearrange("p (k t) -> p k t", t=128), [ptr_], [r_hsT[s2]])

    def d_GU(j):
        s4 = j % NB; s2 = j % 2
        psv, prs = pbank(2 + s2)
        for g in range(2):
            for kc in range(8):
                b.mm(psv[:, g * 128:(g + 1) * 128], wgu[s4][:, g, kc, :], hsT[s2][:, kc, :], kc == 0, kc == 7,
                     [r_wgs[s4], r_hsT[s2]], [prs])
        b.act(sg[s2], psv[:, 0:128], AF.Silu, [prs], [r_sg[s2]])
        b.tt("dve", actb[s2], psv[:, 128:256], sg[s2], ALU.mult, [prs, r_sg[s2]], [r_actb[s2]])

    def d_DN(j):
        s4 = j % NB; s2 = j % 2
        for cc in range(2):
            psv, prs = pbank(4 + 2 * s2 + cc)
            b.mm(psv, actb[s2], wdn[s4][:, cc * 512:(cc + 1) * 512], True, True, [r_actb[s2], r_wds[s4]], [prs])
            if cc == 0:
                b.cp("act", yt[s2][:, 0:512], psv, [prs], [r_yt[s2]])
            else:
                b.cp("dve", yt[s2][:, 512:1024], psv, [prs], [r_yt[s2]])
        rj = R(f"dys{j}")
        rj.stream = ys_stream
        r_ys.append(rj)
        P.dma("sp", ys_scr[j * 128:(j + 1) * 128, :], yt[s2], reads=[r_yt[s2]], writes=[rj])

    for j in range(PF):
        d_load(j)
    for it in range(NSL + 2):
        if it < NSL:
            d_T(it)
        if 0 <= it - 1 < NSL:
            d_GU(it - 1)
        if 0 <= it - 2 < NSL:
            d_DN(it - 2)
        if it + PF < NSL:
            d_load(it + PF)

    xb = [T([D], F32) for _ in range(3)]; r_xb = [R(f"exb{i}") for i in range(3)]
    ya = [T([D], F32) for _ in range(3)]; r_ya = [R(f"eya{i}") for i in range(3)]
    yb = [T([D], F32) for _ in range(3)]; r_yb = [R(f"eyb{i}") for i in range(3)]
    print("phase E arena", P.arena_off)

    def e_load(t):
        S, i = tiles[t]
        s2 = t % 3
        P.dma("sp", xb[s2], tile_src(y_out, S, i), reads=[r_y[t]], writes=[r_xb[s2]])
        P.idma(ya[s2], ys_scr[:, :], slot_i[:, 0, t:t + 1], True, reads=[r_slot] + r_ys, writes=[r_ya[s2]])
        P.idma(yb[s2], ys_scr[:, :], slot_i[:, 1, t:t + 1], True, reads=[r_slot] + r_ys, writes=[r_yb[s2]])

    e_load(0)
    e_load(1)
    for t in range(NT):
        S, i = tiles[t]
        s2 = t % 3
        if t + 2 < NT:
            e_load(t + 2)
        b.stt("dve", ya[s2], ya[s2], w12[:, 0, t:t + 1], xb[s2], ALU.mult, ALU.add, [r_ya[s2], r_xb[s2], r_w12], [r_ya[s2]])
        b.stt("dve", ya[s2], yb[s2], w12[:, 1, t:t + 1], ya[s2], ALU.mult, ALU.add, [r_ya[s2], r_yb[s2], r_w12], [r_ya[s2]])
        P.dma("sp", tile_src(y_out, S, i), ya[s2], reads=[r_ya[s2]], writes=[r_y[t]])
    P.barrier()

def _const_tables(p):
    f32 = np.float32
    half = 8
    inv_freq = (np.float32(500000.0) ** (-(np.arange(half, dtype=f32) * f32(2.0) / f32(16.0)))).astype(f32)
    pos = (np.arange(8192) - (0 if p == 1 else 1024)).astype(f32)
    ang = (pos[:, None] * inv_freq[None, :]).astype(f32)
    cs = np.concatenate([np.cos(ang), np.sin(ang)], axis=1).astype(f32)
    cs_tab = cs.reshape(64, 128, 16).transpose(1, 0, 2).reshape(128, 64 * 16)
    pb = np.zeros((16, 32), f32)
    for ob in range(16):
        pb[ob, q_block(ob):] = NEG
        if p == 0:
            pb[ob, :4] = NEG
    pbrep = np.broadcast_to(pb.reshape(1, 512), (128, 512)).copy()
    onehot = np.zeros((32, 8192), f32)
    for n in range(32):
        onehot[n, n * 256:(n + 1) * 256] = 1.0
    tri = (np.arange(128)[:, None] <= np.arange(128)[None, :]).astype(f32)
    rconst = np.zeros((128, 32 + 96 + 1), f32)
    rconst[:, 0:32] = 128.0 * np.arange(32, dtype=f32)[None, :]
    rconst[:, 32:128] = np.arange(96, dtype=f32)[None, :]
    rconst[:, 128] = np.arange(128, dtype=f32)
    return dict(cs_tab=np.ascontiguousarray(cs_tab), pbrep=pbrep, onehot_k=onehot, tri=tri,
                rconst=rconst, ident_in=np.eye(128, dtype=f32))


def make_in_maps(inputs):
    f32 = np.float32
    x = np.asarray(inputs["x"], f32)
    g = lambda k: np.ascontiguousarray(np.asarray(inputs[k], f32)[0])
    shared = dict(
        w_in=g("w_in"), norm1_g=g("norm1_g").reshape(1, D),
        lamre_t=np.ascontiguousarray(g("lam_re").T), lamim_t=np.ascontiguousarray(g("lam_im").T),
        log_dt=g("log_dt").reshape(1, 32),
        b_re_t=np.ascontiguousarray(g("ssm_b_re").transpose(1, 0, 2)),
        b_im_t=np.ascontiguousarray(g("ssm_b_im").transpose(1, 0, 2)),
        c_re_t=np.ascontiguousarray(g("ssm_c_re").transpose(2, 0, 1)),
        c_im_t=np.ascontiguousarray(g("ssm_c_im").transpose(2, 0, 1)),
        ssm_d=g("ssm_d").reshape(1, 512), w_glu=g("w_glu"), b_glu=g("b_glu").reshape(1, 512),
        q_norm_g=g("q_norm_g").reshape(1, 64), k_norm_g=g("k_norm_g").reshape(1, 64),
        w_proj_ssm=g("w_proj_ssm"), w_proj_attn=g("w_proj_attn"), w_out=g("w_out"),
        norm2_g=g("norm2_g").reshape(1, D),
        w_router=np.ascontiguousarray(np.concatenate([g("w_router_group"), g("w_router_expert")], axis=1)),
        b_router=np.concatenate([g("b_router_group"), g("b_router_expert")]).reshape(1, 36),
        w_gate=g("w_gate"), w_up=g("w_up"), w_down=g("w_down"),
    )
    tabs = [_const_tables(0), _const_tables(1)]
    zeros = np.zeros((1024, D), f32)
    in_maps = []
    for c in range(NCORES):
        bi, p = c // 2, c % 2
        m = dict(shared)
        m.update(tabs[p])
        m["x_all"] = np.ascontiguousarray(x[bi]) if p == 1 else np.concatenate([zeros, x[bi, 0:7168]], axis=0)
        in_maps.append(m)
    return in_maps


_CACHE = {}


def kernel(**inputs):
    in_maps = make_in_maps(inputs)
    if "nc" not in _CACHE:
        _CACHE["nc"] = build()[0]
    nc = _CACHE["nc"]
    res = run_bass_kernel_spmd(nc, in_maps, core_ids=list(range(NCORES)))
    x = np.asarray(inputs["x"])
    out = np.empty(x.shape, np.float32)
    for c in range(NCORES):
        bi, p = c // 2, c % 2
        y = res.results[c]["y_out"]
        for S in range(NST_OWN):
            g0 = 1024 * (2 * S + p)
            out[bi, g0:g0 + 1024] = y[1024 * S:1024 * S + 1024]
    return out
```

```python
import math
import os
from contextlib import ExitStack

import numpy as np
import ml_dtypes

import concourse.bass as bass
import concourse.mybir as mybir
from concourse.bass_utils import run_bass_kernel_spmd

F32 = mybir.dt.float32
BF16 = mybir.dt.bfloat16
I32 = mybir.dt.int32
AF = mybir.ActivationFunctionType
ALU = mybir.AluOpType
AX = mybir.AxisListType

NCORES = 8
D = 1024
S_OWN = 4096
S_CTX = 4096
NST_CTX = 4
NST_OWN = 4
NST = NST_CTX + NST_OWN
NEG = -30000.0
EPS = 1e-6
TWO_PI = 2.0 * math.pi


class Res:
    __slots__ = ("name", "last_w", "readers", "stream")

    def __init__(self, name):
        self.name = name
        self.last_w = None
        self.readers = []
        self.stream = None


class Ins:
    __slots__ = ("idx", "eng", "fn", "deps", "marked", "count", "dma_sem", "dma_val", "is_dma",
                 "bar_streams")


class Prog:
    ENGS = ("pe", "act", "dve", "pool", "sp")

    def __init__(self, nc, sbuf_elems):
        self.nc = nc
        self.ins = []
        self.per_eng = {e: [] for e in self.ENGS}
        self.stack = ExitStack()
        self.dma_streams = []
        self.nres = 0
        self.bar = None
        self.recording = None
        self.arena = self.stack.enter_context(nc.sbuf_tensor("arena", [128, sbuf_elems], BF16))
        self.arena_off = 0
        self.arena_size = sbuf_elems
        self.arena_peak = 0

    def tile(self, shape, dtype):
        n = 1
        for s in shape:
            n *= s
        nb = n * (2 if dtype == F32 or dtype == I32 else 1)
        nb = (nb + 15) // 16 * 16
        off = self.arena_off
        self.arena_off += nb
        self.arena_peak = max(self.arena_peak, self.arena_off)
        assert self.arena_off <= self.arena_size, f"SBUF arena overflow {self.arena_off}"
        ap = self.arena[:, off:off + nb]
        if dtype != BF16:
            ap = ap.bitcast(dtype)
            nb //= 2
        ap = ap[:, 0:n]
        if len(shape) == 2:
            ap = ap.rearrange("p (a b) -> p a b", b=shape[1])
        elif len(shape) == 3:
            ap = ap.rearrange("p (a b c) -> p a b c", b=shape[1], c=shape[2])
        elif len(shape) == 4:
            ap = ap.rearrange("p (a b c d) -> p a b c d", b=shape[1], c=shape[2], d=shape[3])
        return ap

    def mark(self):
        return self.arena_off

    def release(self, m):
        self.arena_off = m

    def sem(self, name):
        return self.stack.enter_context(self.nc.semaphore(name))

    def res(self, name=None):
        self.nres += 1
        return Res(name or f"r{self.nres}")

    def stream(self, name):
        st = {"sem": self.sem(f"dq{len(self.dma_streams)}"), "val": 0}
        self.dma_streams.append(st)
        return st

    def barrier(self):
        last = []
        for e in ("pe", "act", "dve", "pool"):
            for I in reversed(self.per_eng[e]):
                if not I.is_dma:
                    last.append(I)
                    break
        snap = [(st, st["val"]) for st in self.dma_streams if st["val"] > 0]
        self.bar = {e: (list(last), snap) for e in self.ENGS}

    def _add(self, eng, fn, reads, writes, is_dma=False, stream=None):
        I = Ins()
        I.idx = len(self.ins)
        I.eng = eng
        I.fn = fn
        I.marked = False
        I.count = None
        I.is_dma = is_dma
        I.dma_sem = None
        I.dma_val = None
        I.bar_streams = None
        if is_dma:
            stream["val"] += 16
            I.dma_sem = stream["sem"]
            I.dma_val = stream["val"]
        deps = {}

        def add_dep(d, raw):
            if d is None:
                return
            if (not d.is_dma) and (not is_dma) and d.eng == eng:
                if eng == "pe" or (not raw and not SYNC_SAME_ENGINE_WAR):
                    return
            deps[d.idx] = d

        for r in reads:
            add_dep(r.last_w, True)
        for w in writes:
            add_dep(w.last_w, False)
            for rd in w.readers:
                add_dep(rd, False)
        for r in reads:
            r.readers.append(I)
        for w in writes:
            w.last_w = I
            w.readers = []
        if self.bar is not None and eng in self.bar:
            last, snap = self.bar.pop(eng)
            for d in last:
                if d.eng != eng or is_dma:
                    deps[d.idx] = d
            I.bar_streams = snap
        I.deps = list(deps.values())
        self.ins.append(I)
        self.per_eng[eng].append(I)
        return I

    def op(self, eng, fn, reads=(), writes=()):
        if self.recording is not None:
            reads, writes = list(reads), list(writes)
            self.recording.append(lambda: self._add(eng, fn, reads, writes))
            return None
        return self._add(eng, fn, reads, writes)

    def dma(self, eng, out, in_, reads=(), writes=(), slow=False):
        if self.recording is not None:
            reads, writes = list(reads), list(writes)
            rec = self.recording
            self.recording = None
            rec.append(lambda: self.dma(eng, out, in_, reads, writes, slow))
            self.recording = rec
            return None
        w = writes[0]
        if w.stream is None:
            w.stream = self.stream(w.name)
        if slow:
            f = lambda e: e.dma_start(out=out, in_=in_, allow_slow_non_contiguous=True)
        else:
            f = lambda e: e.dma_start(out=out, in_=in_)
        return self._add(eng, f, reads, writes, is_dma=True, stream=w.stream)

    def idma(self, out, in_, idx, gather, reads=(), writes=()):
        w = writes[0]
        if w.stream is None:
            w.stream = self.stream(w.name)
        if gather:
            f = lambda e: e.indirect_dma_start(out=out, out_offset=None, in_=in_,
                                               in_offset=bass.IndirectOffsetOnAxis(ap=idx, axis=0))
        else:
            f = lambda e: e.indirect_dma_start(out=out, out_offset=bass.IndirectOffsetOnAxis(ap=idx, axis=0),
                                               in_=in_, in_offset=None)
        return self._add("pool", f, reads, writes, is_dma=True, stream=w.stream)

    def finalize(self):
        nc = self.nc
        final_waits = [(st["sem"], st["val"]) for st in self.dma_streams if st["val"] > 0]
        for I in self.ins:
            for d in I.deps:
                if not d.is_dma:
                    d.marked = True
        engsem = {}
        for e in ("pe", "act", "dve", "pool"):
            engsem[e] = self.sem("es_" + e)
            c = 0
            for I in self.per_eng[e]:
                if I.is_dma:
                    continue
                if I.marked:
                    c += 1
                    I.count = c
        nwaits = 0
        with nc.Block() as block:
            def emit(ename):
                def body(e):
                    nonlocal nwaits
                    waited = {}
                    for I in self.per_eng[ename]:
                        need = {}
                        for d in I.deps:
                            if d.is_dma:
                                key = ("d", id(d.dma_sem))
                                sem, val = d.dma_sem, d.dma_val
                            else:
                                key = ("e", d.eng)
                                sem, val = engsem[d.eng], d.count
                            if key not in need or need[key][1] < val:
                                need[key] = (sem, val)
                        if I.bar_streams:
                            for st, val in I.bar_streams:
                                key = ("d", id(st["sem"]))
                                if key not in need or need[key][1] < val:
                                    need[key] = (st["sem"], val)
                        for key, (sem, val) in need.items():
                            if waited.get(key, 0) >= val:
                                continue
                            e.wait_ge(sem, val)
                            nwaits += 1
                            waited[key] = val
                        bi = I.fn(e)
                        if I.is_dma:
                            bi.then_inc(I.dma_sem, 16)
                        elif I.marked:
                            bi.then_inc(engsem[ename], 1)
                    if ename == "sp":
                        for (sem, val) in final_waits:
                            e.wait_ge(sem, val)
                return body
            block.tensor(emit("pe"))
            block.scalar(emit("act"))
            block.vector(emit("dve"))
            block.gpsimd(emit("pool"))
            block.sync(emit("sp"))
        self.nwaits = nwaits
        self.stack.close()


class B:
    def __init__(self, P):
        self.P = P

    def mm(self, out, lhsT, rhs, start, stop, reads, writes):
        self.P.op("pe", lambda e: e.matmul(out, lhsT, rhs, start=start, stop=stop), reads, writes)

    def tr(self, out, in_, ident, reads, writes):
        self.P.op("pe", lambda e: e.transpose(out, in_, ident), reads, writes)

    def act(self, out, in_, func, reads, writes, bias=0.0, scale=1.0, accum=None):
        if accum is None:
            self.P.op("act", lambda e: e.activation(out=out, in_=in_, func=func, bias=bias, scale=scale),
                      reads, writes)
        else:
            self.P.op("act", lambda e: e.activation(out=out, in_=in_, func=func, bias=bias, scale=scale,
                                                    accum_out=accum), reads, writes)

    def tt(self, eng, out, in0, in1, op, reads, writes):
        self.P.op(eng, lambda e: e.tensor_tensor(out=out, in0=in0, in1=in1, op=op), reads, writes)

    def ts(self, eng, out, in0, s1, s2, op0, op1, reads, writes):
        if s2 is None:
            self.P.op(eng, lambda e: e.tensor_scalar(out=out, in0=in0, scalar1=s1, scalar2=None, op0=op0),
                      reads, writes)
        else:
            self.P.op(eng, lambda e: e.tensor_scalar(out=out, in0=in0, scalar1=s1, scalar2=s2, op0=op0, op1=op1),
                      reads, writes)

    def stt(self, eng, out, in0, scalar, in1, op0, op1, reads, writes):
        self.P.op(eng, lambda e: e.scalar_tensor_tensor(out=out, in0=in0, scalar=scalar, in1=in1,
                                                         op0=op0, op1=op1), reads, writes)

    def cp(self, eng, out, in_, reads, writes):
        if eng == "act":
            self.P.op("act", lambda e: e.copy(out=out, in_=in_), reads, writes)
        else:
            self.P.op(eng, lambda e: e.tensor_copy(out=out, in_=in_), reads, writes)

    def memset(self, eng, out, val, writes):
        self.P.op(eng, lambda e: e.memset(out, val), (), writes)

    def recip(self, out, in_, reads, writes):
        self.P.op("dve", lambda e: e.reciprocal(out=out, in_=in_), reads, writes)

    def reduce(self, out, in_, op, reads, writes, eng="dve"):
        self.P.op(eng, lambda e: e.tensor_reduce(out=out, in_=in_, axis=AX.X, op=op), reads, writes)


def is_own(s):
    return s % 2 == 1


def q_block(ob):
    return 4 * (2 * (ob // 4) + 1) + ob % 4


def bc(ap, shape):
    return ap.to_broadcast(list(shape))


SYNC_SAME_ENGINE_WAR = True
ARENA_ELEMS = 106400
P0_BASE = 74272


def build(debug=(), stop_after="D"):
    nc = bass.Bass("TRN2", target_bir_lowering=False)
    P = Prog(nc, ARENA_ELEMS)
    b = B(P)
    T = P.tile
    R = P.res
    dbg_out = {}

    def din(name, shape, dt=F32):
        return nc.dram_tensor(name, list(shape), dt, kind="ExternalInput").ap()

    def dscr(name, shape, dt):
        return nc.dram_tensor(name, list(shape), dt, kind="Internal").ap()

    def dump(name, ap_sb, shape, reads, dt=F32, eng="sp"):
        if name not in debug:
            return
        o = nc.dram_tensor("dbg_" + name, list(shape), dt, kind="ExternalOutput").ap()
        dbg_out[name] = o
        P.dma(eng, o, ap_sb, reads=reads, writes=[R("dbg_" + name)])

    x_all = din("x_all", [S_CTX + S_OWN, D])
    w_in = din("w_in", [D, 4096])
    norm1_g = din("norm1_g", [1, D])
    lamre_t = din("lamre_t", [64, 32])
    lamim_t = din("lamim_t", [64, 32])
    log_dt = din("log_dt", [1, 32])
    b_re_t = din("b_re_t", [64, 32, 16])
    b_im_t = din("b_im_t", [64, 32, 16])
    c_re_t = din("c_re_t", [64, 32, 16])
    c_im_t = din("c_im_t", [64, 32, 16])
    ssm_d = din("ssm_d", [1, 512])
    w_glu = din("w_glu", [512, 512])
    b_glu = din("b_glu", [1, 512])
    q_norm_g = din("q_norm_g", [1, 64])
    k_norm_g = din("k_norm_g", [1, 64])
    w_proj_ssm = din("w_proj_ssm", [512, D])
    w_proj_attn = din("w_proj_attn", [512, D])
    w_out = din("w_out", [D, D])
    norm2_g = din("norm2_g", [1, D])
    w_router = din("w_router", [D, 36])
    b_router = din("b_router", [1, 36])
    w_gate = din("w_gate", [32, D, 128])
    w_up = din("w_up", [32, D, 128])
    w_down = din("w_down", [32, 128, D])
    ident_in = din("ident_in", [128, 128])
    cs_tab_in = din("cs_tab", [128, 64 * 16])
    pbrep_in = din("pbrep", [128, 16 * 32])
    onehot_in = din("onehot_k", [32, 8192])
    tri_in = din("tri", [128, 128])
    rconst_in = din("rconst", [128, 32 + 96 + 1])
    y_out = nc.dram_tensor("y_out", [S_OWN, D], F32, kind="ExternalOutput").ap()

    kT_scr = dscr("kT_scr", [8, 64, 8192], BF16)
    qT_scr = dscr("qT_scr", [8, 64, 4096], BF16)
    v_scr = dscr("v_scr", [8, 128, 64 * 65], BF16)
    z_scr = dscr("z_scr", [NST_OWN, 128, 8 * 512], BF16)
    att_scr = dscr("att_scr", [S_OWN, 512], BF16)
    wgu_scr = dscr("wgu_scr", [32, 128, 2, 8, 128], BF16)
    wd_scr = dscr("wd_scr", [32, 128, D], BF16)
    hs_scr = dscr("hs_scr", [96 * 128, D], BF16)
    h2_scr = dscr("h2_scr", [S_OWN, D], BF16)
    ys_scr = dscr("ys_scr", [96 * 128, D], F32)
    r_kT_scr = [R(f"kTs{h}") for h in range(8)]
    r_qT_scr = [R(f"qTs{h}") for h in range(8)]
    r_v_scr = [R(f"vs{h}") for h in range(8)]
    r_z_scr = [R(f"zs{s}") for s in range(NST_OWN)]
    r_att_scr = R("atts")
    _rw = [R(f"wscr{i}") for i in range(4)]
    r_wgu = [_rw[e % 4] for e in range(32)]
    r_wd = [_rw[e % 4] for e in range(32)]

    pd = [P.stack.enter_context(nc.psum_tensor(f"pd{i}", [128, 1024], F32)) for i in range(4)]
    pr = [[R(f"pd{i}a"), R(f"pd{i}b")] for i in range(4)]

    def pbank(i):
        return pd[i // 2][:, (i % 2) * 512:(i % 2) * 512 + 512], pr[i // 2][i % 2]

    def pbank_bf(i):
        return pd[i // 2][:, (i % 2) * 512:(i % 2) * 512 + 512].bitcast(BF16), pr[i // 2][i % 2]

    identf = T([128], F32); r_identf = R("identf")
    identb = T([128], BF16); r_identb = R("identb")
    P.dma("sp", identf, ident_in, writes=[r_identf])
    P.dma("pool", identb, ident_in, writes=[r_identb])
    g1rep = T([D], F32); r_g1 = R("g1rep")
    P.dma("sp", g1rep, norm1_g[0:1, :].partition_broadcast(128), writes=[r_g1])
    ksum = T([8, 64], F32); r_ksum = R("ksum")
    m_ssm = P.mark()
    gqk = T([2, 64], F32); r_gqk = R("gqk")
    P.dma("sp", gqk[:, 0, :], q_norm_g[0:1, :].partition_broadcast(128), writes=[r_gqk])
    P.dma("sp", gqk[:, 1, :], k_norm_g[0:1, :].partition_broadcast(128), writes=[r_gqk])
    cs_tab = T([64, 16], F32); r_cs = R("cs_tab")
    P.dma("sp", cs_tab, cs_tab_in.rearrange("p (t f) -> p t f", f=16), writes=[r_cs])
    drep = T([512], F32); r_drep = R("drep")
    P.dma("sp", drep, ssm_d[0:1, :].partition_broadcast(128), writes=[r_drep])


    W_e = T([32, 2, 64], BF16); r_We = R("W_e")
    Tz = T([32, 128], BF16); r_Tz = R("Tz")
    W_cr = T([16, 8, 16], BF16); r_Wc = R("W_c")
    W_ci = T([16, 8, 16], BF16)
    ER = T([16, 129], F32); r_E = R("E")
    EI = T([16, 129], F32)
    rho = T([16], F32); r_rho = R("rho")
    SR0 = T([16], F32); r_S0 = R("S0")
    SI0 = T([16], F32)
    b.memset("dve", SR0, 0.0, [r_S0])
    b.memset("dve", SI0, 0.0, [r_S0])

    m0 = P.mark()
    P.arena_off = P0_BASE
    P.recording = []
    r_p = R("prm")

    def hn_load(dst, src, extra=""):
        for gh in range(2):
            P.dma("sp", dst[64 * gh:64 * gh + 64], src[:, 16 * gh:16 * gh + 16], writes=[r_p])

    LR = T([16], F32); LI = T([16], F32); DT = T([16], F32)
    hn_load(LR, lamre_t); hn_load(LI, lamim_t)
    for gh in range(2):
        P.dma("sp", DT[64 * gh:64 * gh + 64], log_dt[0:1, 16 * gh:16 * gh + 16].partition_broadcast(64), writes=[r_p])
    BR = T([16, 16], F32); BI = T([16, 16], F32); CR = T([16, 16], F32); CI = T([16, 16], F32)
    hn_load(BR, b_re_t); hn_load(BI, b_im_t); hn_load(CR, c_re_t); hn_load(CI, c_im_t)
    rp = [r_p]
    ey = T([16], F32); ep = T([16], F32)

    def exp_acc(dst, src, extra_w=()):
        b.ts("dve", ey, src, 0.125, None, ALU.mult, None, rp, rp)
        b.ts("dve", ep, ey, 1.0 / 10, 1.0, ALU.mult, ALU.add, rp, rp)
        for n in range(9, 0, -1):
            b.tt("dve", ep, ep, ey, ALU.mult, rp, rp)
            b.ts("dve", ep, ep, 1.0 / n, 1.0, ALU.mult, ALU.add, rp, rp)
        b.tt("dve", ep, ep, ep, ALU.mult, rp, rp)
        b.tt("dve", ep, ep, ep, ALU.mult, rp, rp)
        b.tt("dve", dst, ep, ep, ALU.mult, rp, rp + list(extra_w))

    exp_acc(DT, DT)
    b.ts("dve", LR, LR, -1e-4, None, ALU.min, None, rp, rp)
    LRDT = T([16], F32); ANG = T([16], F32)
    b.tt("dve", LRDT, LR, DT, ALU.mult, rp, rp)
    b.tt("dve", ANG, LI, DT, ALU.mult, rp, rp)
    MAG = T([16], F32)
    exp_acc(MAG, LRDT)
    b.tt("dve", rho, MAG, MAG, ALU.mult, rp, [r_p, r_rho])
    b.tt("dve", rho, rho, rho, ALU.mult, [r_p, r_rho], [r_p, r_rho])
    b.tt("dve", rho, rho, rho, ALU.mult, [r_p, r_rho], [r_p, r_rho])
    SN = T([16], F32); CS = T([16], F32)
    tq = T([16], F32); ki = T([16], I32); kf = T([16], F32); rr = T([16], F32)
    for shift, outt in ((0.0, SN), (math.pi / 2, CS)):
        b.ts("dve", tq, ANG, 1.0 / TWO_PI, shift / TWO_PI, ALU.mult, ALU.add, rp, rp)
        b.cp("dve", ki, tq, rp, rp)
        b.cp("dve", kf, ki, rp, rp)
        b.stt("dve", rr, kf, -6.28125, ANG, ALU.mult, ALU.add, rp, rp)
        b.stt("dve", rr, kf, -(TWO_PI - 6.28125), rr, ALU.mult, ALU.add, rp, rp)
        b.ts("dve", rr, rr, shift, math.pi, ALU.add, ALU.min, rp, rp)
        b.ts("dve", rr, rr, -math.pi, None, ALU.max, None, rp, rp)
        b.act(outt, rr, AF.Sin, rp, rp)
    PR = T([9, 16], F32); PI = T([9, 16], F32)
    b.memset("dve", PR[:, 0, :], 1.0, rp)
    b.memset("dve", PI[:, 0, :], 0.0, rp)
    b.tt("dve", PR[:, 1, :], MAG, CS, ALU.mult, rp, rp)
    b.tt("dve", PI[:, 1, :], MAG, SN, ALU.mult, rp, rp)
    t1 = T([4, 16], F32); t2 = T([4, 16], F32)

    def cmul_pow(dst0, n, src0, m):
        a_r = PR[:, src0:src0 + n, :]; a_i = PI[:, src0:src0 + n, :]
        b_r = bc(PR[:, m:m + 1, :], [128, n, 16]); b_i = bc(PI[:, m:m + 1, :], [128, n, 16])
        b.tt("dve", t1[:, 0:n, :], a_r, b_r, ALU.mult, rp, rp)
        b.tt("dve", t2[:, 0:n, :], a_i, b_i, ALU.mult, rp, rp)
        b.tt("dve", PR[:, dst0:dst0 + n, :], t1[:, 0:n, :], t2[:, 0:n, :], ALU.subtract, rp, rp)
        b.tt("dve", t1[:, 0:n, :], a_r, b_i, ALU.mult, rp, rp)
        b.tt("dve", t2[:, 0:n, :], a_i, b_r, ALU.mult, rp, rp)
        b.tt("dve", PI[:, dst0:dst0 + n, :], t1[:, 0:n, :], t2[:, 0:n, :], ALU.add, rp, rp)

    cmul_pow(2, 1, 1, 1)
    cmul_pow(3, 2, 1, 2)
    cmul_pow(5, 4, 1, 4)
    DEN = T([16], F32); NR = T([16], F32); CRc = T([16], F32); CIc = T([16], F32); tmp = T([16], F32)
    b.tt("dve", DEN, LR, LR, ALU.mult, rp, rp)
    b.tt("dve", tmp, LI, LI, ALU.mult, rp, rp)
    b.tt("dve", DEN, DEN, tmp, ALU.add, rp, rp)
    b.recip(DEN, DEN, rp, rp)
    b.ts("dve", NR, PR[:, 1, :], -1.0, None, ALU.add, None, rp, rp)
    AIc = PI[:, 1, :]
    b.tt("dve", CRc, NR, LR, ALU.mult, rp, rp)
    b.tt("dve", tmp, AIc, LI, ALU.mult, rp, rp)
    b.tt("dve", CRc, CRc, tmp, ALU.add, rp, rp)
    b.tt("dve", CRc, CRc, DEN, ALU.mult, rp, rp)
    b.tt("dve", CIc, AIc, LR, ALU.mult, rp, rp)
    b.tt("dve", tmp, NR, LI, ALU.mult, rp, rp)
    b.tt("dve", CIc, CIc, tmp, ALU.subtract, rp, rp)
    b.tt("dve", CIc, CIc, DEN, ALU.mult, rp, rp)
    BBR = T([16, 16], F32); BBI = T([16, 16], F32); tb1 = T([16, 16], F32); tb2 = T([16, 16], F32)
    crb = bc(CRc.unsqueeze(2), [128, 16, 16]); cib = bc(CIc.unsqueeze(2), [128, 16, 16])
    b.tt("dve", tb1, BR, crb, ALU.mult, rp, rp)
    b.tt("dve", tb2, BI, cib, ALU.mult, rp, rp)
    b.tt("dve", BBR, tb1, tb2, ALU.subtract, rp, rp)
    b.tt("dve", tb1, BI, crb, ALU.mult, rp, rp)
    b.tt("dve", tb2, BR, cib, ALU.mult, rp, rp)
    b.tt("dve", BBI, tb1, tb2, ALU.add, rp, rp)
    mpad_off = P.mark()
    Mpad = T([16, 2, 240], F32)
    b.memset("pool", Mpad, 0.0, rp)
    for s in range(8):
        tau = 7 - s
        prb = bc(PR[:, tau, :].unsqueeze(2), [128, 16, 16]); pib = bc(PI[:, tau, :].unsqueeze(2), [128, 16, 16])
        b.tt("dve", tb1, BBR, prb, ALU.mult, rp, rp)
        b.tt("dve", tb2, BBI, pib, ALU.mult, rp, rp)
        b.tt("dve", Mpad[:, :, 0, 16 * s:16 * s + 16], tb1, tb2, ALU.subtract, rp, rp)
        b.tt("dve", tb1, BBI, prb, ALU.mult, rp, rp)
        b.tt("dve", tb2, BBR, pib, ALU.mult, rp, rp)
        b.tt("dve", Mpad[:, :, 1, 16 * s:16 * s + 16], tb1, tb2, ALU.add, rp, rp)
    NCI = T([16, 16], F32)
    b.ts("dve", NCI, CI, -1.0, None, ALU.mult, None, rp, rp)
    for g0 in range(0, 32, 4):
        pv, prs = pbank(6 + g0 // 4 % 2)
        for gg in range(4):
            g = g0 + gg
            gh, gl = g // 16, g % 16
            for ri in range(2):
                b.tr(pv[:, (gg * 2 + ri) * 64:(gg * 2 + ri) * 64 + 64], Mpad[64 * gh:64 * gh + 64, gl, ri, 0:128],
                     identf[64 * gh:64 * gh + 64, 64 * gh:64 * gh + 64], [r_p, r_identf], [prs])
        b.cp("act", W_e[:, g0:g0 + 4, :, :], pv.rearrange("p (g r n) -> p g r n", g=4, r=2), [prs], [r_We])
    MpadB = T([16, 2, 240], BF16); CRb = T([16, 16], BF16); NCIb = T([16, 16], BF16)
    b.cp("pool", MpadB, Mpad, rp, rp)
    b.cp("pool", CRb, CR, rp, rp)
    b.cp("pool", NCIb, NCI, rp, rp)
    for g0 in range(0, 32, 4):
        pv, prs = pbank(6 + g0 // 4 % 2)
        for gg in range(4):
            g = g0 + gg
            gh, gl = g // 16, g % 16
            sl = slice(64 * gh, 64 * gh + 64)
            for i in range(8):
                o = pv[:, gg * 128 + i * 16:gg * 128 + i * 16 + 16]
                b.mm(o, MpadB[sl, gl, 0, 16 * (7 - i):16 * (7 - i) + 128], CRb[sl, gl, :], True, False, [r_p], [prs])
                b.mm(o, MpadB[sl, gl, 1, 16 * (7 - i):16 * (7 - i) + 128], NCIb[sl, gl, :], False, True, [r_p], [prs])
        b.cp("act", Tz[:, g0:g0 + 4, :], pv.rearrange("p (g x) -> p g x", g=4), [prs], [r_Tz])
    _cur = P.mark()
    P.arena_off = mpad_off
    tc1 = T([16, 8, 16], F32); tc2 = T([16, 8, 16], F32)
    crB = bc(CR.unsqueeze(2), [128, 16, 8, 16]); ciB = bc(CI.unsqueeze(2), [128, 16, 8, 16]); nciB = bc(NCI.unsqueeze(2), [128, 16, 8, 16])
    PRi = bc(PR[:, 1:9, :].rearrange("p i g -> p g i").unsqueeze(3), [128, 16, 8, 16])
    PIi = bc(PI[:, 1:9, :].rearrange("p i g -> p g i").unsqueeze(3), [128, 16, 8, 16])
    b.tt("dve", tc1, crB, PRi, ALU.mult, rp, rp)
    b.tt("dve", tc2, ciB, PIi, ALU.mult, rp, rp)
    b.tt("dve", W_cr, tc1, tc2, ALU.subtract, rp, [r_p, r_Wc])
    b.tt("dve", tc1, nciB, PRi, ALU.mult, rp, rp)
    b.tt("dve", tc2, crB, PIi, ALU.mult, rp, rp)
    b.tt("dve", W_ci, tc1, tc2, ALU.subtract, rp, [r_p, r_Wc])
    RINV = T([16], F32)
    b.recip(RINV, rho, [r_rho], rp)
    rE = [r_p, r_E]
    b.memset("dve", ER[:, :, 0:1], 1.0, rE)
    b.memset("dve", EI[:, :, 0:1], 0.0, rE)
    b.tt("dve", ER[:, :, 1], PR[:, 8, :], RINV, ALU.mult, rp, rE)
    b.tt("dve", EI[:, :, 1], PI[:, 8, :], RINV, ALU.mult, rp, rE)
    te1 = T([16, 64], F32); te2 = T([16, 64], F32)
    assert P.arena_off <= mpad_off + 16 * 2 * 240 * 2
    P.arena_off = _cur
    m = 1
    while m < 128:
        n = m
        a_r = ER[:, :, 1:1 + n]; a_i = EI[:, :, 1:1 + n]
        b_r = bc(ER[:, :, m:m + 1], [128, 16, n]); b_i = bc(EI[:, :, m:m + 1], [128, 16, n])
        b.tt("dve", te1[:, :, 0:n], a_r, b_r, ALU.mult, rE, rp)
        b.tt("pool", te2[:, :, 0:n], a_i, b_i, ALU.mult, rE, rp)
        b.tt("dve", ER[:, :, m + 1:m + 1 + n], te1[:, :, 0:n], te2[:, :, 0:n], ALU.subtract, rp, rE)
        b.tt("dve", te1[:, :, 0:n], a_r, b_i, ALU.mult, rE, rp)
        b.tt("pool", te2[:, :, 0:n], a_i, b_r, ALU.mult, rE, rp)
        b.tt("dve", EI[:, :, m + 1:m + 1 + n], te1[:, :, 0:n], te2[:, :, 0:n], ALU.add, rp, rE)
        m *= 2
    dump("W_e", W_e, [128, 32, 2, 64], [r_We], BF16)
    dump("Tz", Tz, [128, 32, 128], [r_Tz], BF16)
    dump("W_cr", W_cr, [128, 16, 8, 16], [r_Wc], BF16)
    dump("W_ci", W_ci, [128, 16, 8, 16], [r_Wc], BF16)
    dump("ER", ER, [128, 16, 129], [r_E])
    dump("EI", EI, [128, 16, 129], [r_E])
    dump("rho", rho, [128, 16], [r_rho])
    dump("PR", PR, [128, 9, 16], rp)
    dump("PI", PI, [128, 9, 16], rp)
    print("phase 0 temporaries end", P.arena_peak)
    p0_ops = P.recording
    P.recording = None
    P.release(m0)
    ctx = dict(nc=nc, P=P, b=b, T=T, R=R, dump=dump, dbg_out=dbg_out, pbank=pbank, pbank_bf=pbank_bf, pd=pd, pr=pr)
    ctx.update(locals())
    if stop_after == "0":
        return finish(ctx)
    phase_A(ctx)
    if stop_after == "A":
        return finish(ctx)
    phase_B(ctx)
    if stop_after == "B":
        return finish(ctx)
    phase_CD(ctx)
    return finish(ctx)


def finish(ctx):
    P = ctx["P"]
    P.finalize()
    return ctx["nc"], ctx["dbg_out"]


def run_interleaved(*gens, weights=None):
    gens = list(gens)
    w = {id(g): (weights[i] if weights else 1) for i, g in enumerate(gens)}
    while gens:
        for g in list(gens):
            for _ in range(w[id(g)]):
                try:
                    next(g)
                except StopIteration:
                    gens.remove(g)
                    break


def phase_A(c):
    nc, P, b, T, R, dump = c["nc"], c["P"], c["b"], c["T"], c["R"], c["dump"]
    pbank, pbank_bf = c["pbank"], c["pbank_bf"]
    identb, r_identb, g1rep, r_g1, gqk, r_gqk, cs_tab, r_cs = (c[k] for k in
        ("identb", "r_identb", "g1rep", "r_g1", "gqk", "r_gqk", "cs_tab", "r_cs"))
    W_e, r_We, Tz, r_Tz, W_cr, W_ci, r_Wc, ER, EI, r_E, rho, r_rho, SR0, SI0, r_S0 = (c[k] for k in
        ("W_e", "r_We", "Tz", "r_Tz", "W_cr", "W_ci", "r_Wc", "ER", "EI", "r_E", "rho", "r_rho", "SR0", "SI0", "r_S0"))
    drep, r_drep = c["drep"], c["r_drep"]
    x_all, w_in = c["x_all"], c["w_in"]
    kT_scr, qT_scr, v_scr, z_scr = c["kT_scr"], c["qT_scr"], c["v_scr"], c["z_scr"]
    r_kT_scr, r_qT_scr, r_v_scr, r_z_scr = c["r_kT_scr"], c["r_qT_scr"], c["r_v_scr"], c["r_z_scr"]
    pd, pr = c["pd"], c["pr"]
    ksum, r_ksum = c["ksum"], c["r_ksum"]
    mA = P.mark()
    c["mA"] = mA

    WinA = T([8, 2048], BF16); r_WinA = R("WinA")
    for kc in range(8):
        P.dma("pool", WinA[:, kc, :], w_in[kc * 128:(kc + 1) * 128, 0:2048], writes=[r_WinA])

    xt = [T([D], F32) for _ in range(2)]; r_xt = [R("xt0"), R("xt1")]
    hb = [T([D], BF16) for _ in range(2)]; r_hb = [R("hb0"), R("hb1")]
    ss = T([16], F32); rs = T([16], F32); r_ss = [R(f"ss{i}") for i in range(16)]
    hT = T([8, 1024], BF16); r_hT = [R(f"hT{i}") for i in range(8)]
    pad = [None, [T([8, 64], BF16) for _ in range(2)]]
    r_pad = [[R(f"pad{w}{i}") for i in range(2)] for w in range(2)]
    ktt = [None, [T([8, 128], BF16) for _ in range(2)]]
    r_ktt = [[R(f"ktt{w}{i}") for i in range(2)] for w in range(2)]
    v_st = [T([8, 8, 65], BF16) for _ in range(2)]; r_vst = [R("v_st0"), R("v_st1")]
    for i in range(2):
        b.memset("pool", v_st[i], 1.0, [r_vst[i]])
    sqt = [T([8, 64], F32) for _ in range(2)]; r_sqt = [R("sqt0"), R("sqt1")]
    qn = [T([8, 64], F32) for _ in range(2)]; r_qn = [R("qn0"), R("qn1")]
    ssq = [T([8], F32) for _ in range(2)]; rq = [T([8], F32) for _ in range(2)]
    ra = [T([8, 8], F32) for _ in range(2)]; rb = [T([8, 8], F32) for _ in range(2)]
    assert P.arena_off <= P0_BASE, P.arena_off
    P.arena_off = P0_BASE
    pad[0] = [T([8, 64], BF16) for _ in range(2)]
    ktt[0] = [T([8, 128], BF16) for _ in range(2)]
    Ukj = T([32, 8, 16], BF16); r_Ukj = R("Ukj")
    U8 = T([32, 128], BF16); r_U8 = R("U8")
    NQ = 4
    SRf = T([NQ, 129], F32); SIf = T([NQ, 129], F32); r_Sf = R("Sf")
    zt_r = SRf[:, :, 0:128]; zt_i = SIf[:, :, 0:128]; r_zt = r_Sf
    srm = [T([NQ, 129], F32) for _ in range(2)]; r_srm = [R("srm0"), R("srm1")]
    ztm = [srm[0][:, :, 0:128], srm[1][:, :, 0:128]]; r_ztm = r_srm
    ZR = T([NQ, 129], F32); ZI = T([NQ, 129], F32); r_Z = R("Z")
    S_re = T([NQ, 128], BF16); S_im = T([NQ, 128], BF16); r_S = R("Sbf")
    du = [T([4, 128], F32) for _ in range(2)]; r_du = [R("du0"), R("du1")]
    ys = [T([4, 128], F32) for _ in range(2)]; y2 = [T([4, 128], F32) for _ in range(2)]
    r_ys = [R("ys0"), R("ys1")]; r_y2 = [R("y20"), R("y21")]
    Zst = T([8, 32, 16], BF16); r_Zst = R("Zst")
    print("phase A arena", P.arena_off)

    cnt = {"qkv": 0, "qk": 0}
    outs = {}

    def load_x(s, tt, par):
        row0 = s * 1024 + tt * 128
        P.dma("sp", xt[par], x_all[row0:row0 + 128, :], writes=[r_xt[par]])

    busy = {}

    def qkv_mm(tt, col0):
        i = 1 + cnt["qkv"] % 4
        cnt["qkv"] += 1
        assert not busy.get(i, False)
        psv, prs = pbank(i)
        for kc in range(8):
            b.mm(psv, hT[:, kc, tt * 128:(tt + 1) * 128], WinA[:, kc, col0:col0 + 512], kc == 0, kc == 7,
                 [r_hT[tt], r_WinA], [prs])
        return psv, prs

    def qkv_mm_nb(tt, col0):
        i = 1 + cnt["qkv"] % 4
        cnt["qkv"] += 1
        psv, prs = pbank(i)
        for kc in range(8):
            b.mm(psv, hT[:, kc, tt * 128:(tt + 1) * 128], WinA[:, kc, col0:col0 + 512], kc == 0, kc == 7,
                 [r_hT[tt], r_WinA], [prs])
        return psv, prs

    BACK_LAG = 3
    fstate = {"tick": 0, "done": -1}

    def gen_front(s):
        for _ in gen_front_(s):
            fstate["tick"] += 1
            yield
        fstate["done"] = s

    def gen_front_(s):
        own = is_own(s)
        for tt in range(8):
            par = tt % 2
            if tt < 7:
                load_x(s, tt + 1, 1 - par)
            elif s + 1 < NST:
                load_x(s + 1, 0, 1 - par)
            si = (s % 2) * 8 + tt
            b.act(hb[par], xt[par], AF.Square, [r_xt[par]], [r_hb[par], r_ss[si]], accum=ss[:, si:si + 1])
            b.act(rs[:, si:si + 1], ss[:, si:si + 1], AF.Sqrt, [r_ss[si]], [r_ss[si]], bias=EPS, scale=1.0 / D)
            b.recip(rs[:, si:si + 1], rs[:, si:si + 1], [r_ss[si]], [r_ss[si]])
            b.stt("dve", hb[par], xt[par], rs[:, si:si + 1], g1rep, ALU.mult, ALU.mult,
                  [r_xt[par], r_ss[si], r_g1], [r_hb[par]])
            yield
            ptv, ptr_ = pbank_bf(0)
            for kc in range(8):
                b.tr(ptv[:, kc * 128:(kc + 1) * 128], hb[par][:, kc * 128:(kc + 1) * 128], identb,
                     [r_hb[par], r_identb], [ptr_])
            b.cp("act", hT[:, :, tt * 128:(tt + 1) * 128], ptv.rearrange("p (k t) -> p k t", t=128),
                 [ptr_], [r_hT[tt]])
            yield
            while busy.get(1 + cnt["qkv"] % 4, False):
                yield
            busy[1 + cnt["qkv"] % 4] = True
            outs[(s, tt, 1)] = (1 + cnt["qkv"] % 4,) + tuple(qkv_mm_nb(tt, 1024)) + (fstate["tick"],)
            yield
            while busy.get(1 + cnt["qkv"] % 4, False):
                yield
            psv, prs = qkv_mm(tt, 1536)
            b.cp("act", v_st[s % 2][:, :, tt, 0:64], psv.rearrange("p (h d) -> p h d", d=64), [prs], [r_vst[s % 2]])
            yield
            if own:
                while busy.get(1 + cnt["qkv"] % 4, False):
                    yield
                busy[1 + cnt["qkv"] % 4] = True
                outs[(s, tt, 0)] = (1 + cnt["qkv"] % 4,) + tuple(qkv_mm_nb(tt, 512)) + (fstate["tick"],)
                yield
        P.dma("sp", v_scr[:, :, s * 8 * 65:(s + 1) * 8 * 65].rearrange("h p x -> p h x"),
              v_st[s % 2].rearrange("p h t d -> p h (t d)"), reads=[r_vst[s % 2]], writes=r_v_scr)

    def gen_back(s, which):
        own = is_own(s)
        for tt in range(8):
            ti = s * 8 + tt
            if True:
                while (s, tt, which) not in outs:
                    yield
                while fstate["done"] < s and fstate["tick"] < outs[(s, tt, which)][3] + BACK_LAG:
                    yield
                bank_i, psv, prs, _tk = outs.pop((s, tt, which))
                k_ = which if own else tt % 2
                ps3 = psv.rearrange("p (h d) -> p h d", d=64)
                b.act(sqt[k_], ps3, AF.Square, [prs], [r_sqt[k_]])
                b.reduce(ssq[k_], sqt[k_], ALU.add, [r_sqt[k_]], [r_sqt[k_]])
                b.act(rq[k_], ssq[k_], AF.Sqrt, [r_sqt[k_]], [r_sqt[k_]], bias=EPS, scale=1.0 / 64)
                b.recip(rq[k_], rq[k_], [r_sqt[k_]], [r_sqt[k_]])
                b.tt("dve", qn[k_], ps3, bc(rq[k_].unsqueeze(2), [128, 8, 64]), ALU.mult, [prs, r_sqt[k_]], [r_qn[k_]])
                busy[bank_i] = False
                yield
                b.tt("pool", qn[k_], qn[k_], bc(gqk[:, which, :].unsqueeze(1), [128, 8, 64]), ALU.mult,
                     [r_qn[k_], r_gqk], [r_qn[k_]])
                cosb = bc(cs_tab[:, ti, 0:8].unsqueeze(1), [128, 8, 8])
                sinb = bc(cs_tab[:, ti, 8:16].unsqueeze(1), [128, 8, 8])
                x1 = qn[k_][:, :, 0:8]; x2 = qn[k_][:, :, 8:16]
                pd_ = pad[which][k_]; rpd = r_pad[which][k_]
                rd = [r_qn[k_], r_cs]
                b.tt("pool", ra[k_], x1, cosb, ALU.mult, rd, [r_qn[k_]])
                b.tt("pool", rb[k_], x2, sinb, ALU.mult, rd, [r_qn[k_]])
                b.tt("pool", pd_[:, :, 0:8], ra[k_], rb[k_], ALU.subtract, [r_qn[k_]], [rpd])
                yield
                b.tt("pool", ra[k_], x2, cosb, ALU.mult, rd, [r_qn[k_]])
                b.tt("pool", rb[k_], x1, sinb, ALU.mult, rd, [r_qn[k_]])
                b.tt("pool", pd_[:, :, 8:16], ra[k_], rb[k_], ALU.add, [r_qn[k_]], [rpd])
                b.cp("act", pd_[:, :, 16:64], qn[k_][:, :, 16:64], [r_qn[k_]], [rpd])
                yield
                ptv, ptr_ = pbank_bf(5)
                for h in range(8):
                    b.tr(ptv[0:64, h * 128:(h + 1) * 128], pd_[:, h, :], identb, [rpd, r_identb], [ptr_])
                kt = ktt[which][k_]; rkt = r_ktt[which][k_]
                b.cp("dve", kt[0:64], ptv[0:64].rearrange("p (h t) -> p h t", t=128), [ptr_], [rkt])
                yield
                if which == 1:
                    b.reduce(ksum[0:64, :, ti], kt[0:64], ALU.add, [rkt], [r_ksum])
                    P.dma("sp", kT_scr[:, :, ti * 128:(ti + 1) * 128].rearrange("h d t -> d h t"), kt[0:64],
                          reads=[rkt], writes=r_kT_scr)
                else:
                    oti = (s // 2) * 8 + tt
                    P.dma("sp", qT_scr[:, :, oti * 128:(oti + 1) * 128].rearrange("h d t -> d h t"), kt[0:64],
                          reads=[rkt], writes=r_qT_scr)
                yield

    def gen_ssm_u(s):
        hT8 = hT.rearrange("p k (c j) -> p k j c", j=8)
        for j in range(8):
            psv, prs = pbank(6 + j % 2)
            for kc in range(8):
                b.mm(psv, hT8[:, kc, j, :], WinA[:, kc, 0:512], kc == 0, kc == 7, r_hT + [r_WinA], [prs])
            b.cp("act" if j % 2 else "dve", Ukj[:, :, j, :], psv.rearrange("p (g c) -> p g c", c=16), [prs], [r_Ukj])
            yield
        if "Ukj" in c["debug"] and s == 1:
            dump("Ukj", Ukj, [128, 32, 8, 16], [r_Ukj], BF16)

    def gen_ssm(s):
        own = is_own(s)
        for g0 in range(0, 32, 8):
            ptv, ptr_ = pbank_bf(6 + (g0 // 8) % 2)
            for gg in range(8):
                b.tr(ptv[:, gg * 128:(gg + 1) * 128], Ukj[:, g0 + gg, :, :].rearrange("p j c -> p (j c)"), identb,
                     [r_Ukj, r_identb], [ptr_])
            b.cp("dve", U8[:, g0:g0 + 8, :], ptv.rearrange("p (g k) -> p g k", k=128), [ptr_], [r_U8])
            yield
        for glq in range(16 // NQ):
            e_re, pre = pbank(6)
            e_im, pim = pbank(7)
            e_re = e_re.rearrange("p (g k) -> p g k", k=128)
            e_im = e_im.rearrange("p (g k) -> p g k", k=128)
            for gi in range(NQ):
                for gh in range(2):
                    g = gh * 16 + glq * NQ + gi
                    sl = slice(64 * gh, 64 * gh + 64)
                    b.mm(e_re[sl, gi, :], W_e[:, g, 0, :], U8[:, g, :], True, True, [r_We, r_U8], [pre])
                    b.mm(e_im[sl, gi, :], W_e[:, g, 1, :], U8[:, g, :], True, True, [r_We, r_U8], [pim])
            yield
            gsl = slice(glq * NQ, glq * NQ + NQ)
            E1r = ER[:, gsl, 1:129]; E1i = EI[:, gsl, 1:129]
            b.tt("dve", ztm[0], e_re, E1r, ALU.mult, [pre, r_E], [r_ztm[0]])
            b.tt("dve", ztm[1], e_im, E1i, ALU.mult, [pim, r_E], [r_ztm[1]])
            b.tt("pool", zt_r, ztm[0], ztm[1], ALU.add, r_ztm, [r_zt])
            yield
            b.tt("dve", ztm[0], e_im, E1r, ALU.mult, [pim, r_E], [r_ztm[0]])
            b.tt("dve", ztm[1], e_re, E1i, ALU.mult, [pre, r_E], [r_ztm[1]])
            b.tt("pool", zt_i, ztm[0], ztm[1], ALU.subtract, r_ztm, [r_zt])
            b.cp("pool", ZR[:, :, 0], SR0[:, gsl], [r_S0], [r_Z])
            b.cp("pool", ZI[:, :, 0], SI0[:, gsl], [r_S0], [r_Z])
            yield
            for gi in range(NQ):
                gl = glq * NQ + gi
                for (Zx, S0x, ztx) in ((ZR, SR0, zt_r), (ZI, SI0, zt_i)):
                    P.op("dve", (lambda Zx=Zx, S0x=S0x, ztx=ztx, gi=gi, gl=gl: (lambda e: e.tensor_tensor_scan(
                        out=Zx[:, gi, 1:129], data0=bc(rho[:, gl:gl + 1], [128, 128]), data1=ztx[:, gi, :],
                        initial=S0x[:, gl:gl + 1], op0=ALU.mult, op1=ALU.add)))(),
                        [r_zt, r_rho, r_S0], [r_Z])
                yield
            E0r = ER[:, gsl, :]; E0i = EI[:, gsl, :]
            b.tt("dve", srm[0], ZR, E0r, ALU.mult, [r_Z, r_E], [r_srm[0]])
            b.tt("pool", srm[1], ZI, E0i, ALU.mult, [r_Z, r_E], [r_srm[1]])
            b.tt("dve", SRf, srm[0], srm[1], ALU.subtract, r_srm, [r_Sf])
            yield
            b.tt("dve", srm[0], ZR, E0i, ALU.mult, [r_Z, r_E], [r_srm[0]])
            b.tt("pool", srm[1], ZI, E0r, ALU.mult, [r_Z, r_E], [r_srm[1]])
            b.tt("dve", SIf, srm[0], srm[1], ALU.add, r_srm, [r_Sf])
            b.cp("act", SR0[:, gsl], SRf[:, :, 128], [r_Sf], [r_S0])
            b.cp("act", SI0[:, gsl], SIf[:, :, 128], [r_Sf], [r_S0])
            yield
            if not own:
                continue
            b.cp("act", S_re, SRf[:, :, 0:128], [r_Sf], [r_S])
            b.cp("act", S_im, SIf[:, :, 0:128], [r_Sf], [r_S])
            for gh in range(2):
                psv, prs = pbank(6 + gh)
                sl = slice(64 * gh, 64 * gh + 64)
                gbase = gh * 16 + glq * NQ
                k_ = gh
                for gi in range(NQ):
                    g = gbase + gi
                    o = psv[:, gi * 128:(gi + 1) * 128]
                    b.mm(o, U8[:, g, :], Tz[:, g, :], True, False, [r_U8, r_Tz], [prs])
                    b.mm(o, S_re[sl, gi, :], W_cr[sl, glq * NQ + gi, :, :].rearrange("p i c -> p (i c)"),
                         False, False, [r_S, r_Wc], [prs])
                    b.mm(o, S_im[sl, gi, :], W_ci[sl, glq * NQ + gi, :, :].rearrange("p i c -> p (i c)"),
                         False, True, [r_S, r_Wc], [prs])
                b.tt("pool", du[k_].rearrange("p g (j c) -> p g j c", c=16), Ukj[:, gbase:gbase + 4, :, :],
                     bc(drep[:, gbase * 16:gbase * 16 + 64].rearrange("p (g c) -> p g c", c=16).unsqueeze(2),
                        [128, 4, 8, 16]), ALU.mult, [r_Ukj, r_drep], [r_du[k_]])
                yield
                b.tt("dve", ys[k_], psv.rearrange("p (g x) -> p g x", x=128), du[k_], ALU.add, [prs, r_du[k_]], [r_ys[k_]])
                b.tt("pool", y2[k_], ys[k_], ys[k_], ALU.mult, [r_ys[k_]], [r_y2[k_]])
                b.ts("pool", y2[k_], y2[k_], 0.044715, 1.0, ALU.mult, ALU.add, [r_y2[k_]], [r_y2[k_]])
                b.tt("pool", y2[k_], y2[k_], ys[k_], ALU.mult, [r_y2[k_], r_ys[k_]], [r_y2[k_]])
                yield
                b.act(y2[k_], y2[k_], AF.Sigmoid, [r_y2[k_]], [r_y2[k_]], scale=1.5957691216057308)
                b.tt("dve", Zst[:, :, gbase:gbase + 4, :].rearrange("p i g c -> p g i c"),
                     ys[k_].rearrange("p g (i c) -> p g i c", c=16), y2[k_].rearrange("p g (i c) -> p g i c", c=16),
                     ALU.mult, [r_ys[k_], r_y2[k_]], [r_Zst])
                yield
        if own:
            so = s // 2
            P.dma("sp", z_scr[so], Zst.rearrange("p i g c -> p (i g c)"), reads=[r_Zst], writes=[r_z_scr[so]])
            if "Zst" in c["debug"] and so == 0:
                dump("Zst", Zst, [128, 8, 32, 16], [r_Zst], BF16)

    def gen_p0():
        for k, th in enumerate(c["p0_ops"]):
            th()
            if k % 4 == 3:
                yield

    load_x(0, 0, 0)
    for s in range(NST):
        gens = [gen_front(s), gen_back(s, 1)]
        wts = [1, 1]
        if is_own(s):
            gens.append(gen_back(s, 0))
            wts.append(1)
        if s > 0:
            gens.append(gen_ssm(s - 1))
            wts.append(2 if is_own(s - 1) else 1)
        else:
            gens.append(gen_p0())
            wts.append(3)
        run_interleaved(*gens, weights=wts)
        if s == 0:
            P.barrier()
        run_interleaved(gen_ssm_u(s))
    run_interleaved(gen_ssm(NST - 1))
    dump("kT", kT_scr, [8, 64, 8192], r_kT_scr, BF16)
    dump("qT", qT_scr, [8, 64, 4096], r_qT_scr, BF16)
    dump("v", v_scr, [8, 128, 64 * 65], r_v_scr, BF16)
    dump("z", z_scr, [NST_OWN, 128, 4096], r_z_scr, BF16)
    dump("ksum", ksum, [128, 8, 64], [r_ksum])
    P.barrier()
    P.release(mA)


def prefetch_C(c):
    P, b, T, R = c["P"], c["b"], c["T"], c["R"]
    w_in, hs_scr = c["w_in"], c["hs_scr"]
    cw = {}
    zrow = T([D], BF16); r_zrow = R("czrow")
    b.memset("dve", zrow, 0.0, [r_zrow])
    hz_stream = P.stream("hz")
    r_hz = []
    for j in range(96):
        rj = R(f"chz{j}")
        rj.stream = hz_stream
        r_hz.append(rj)
        P.dma("sp", hs_scr[j * 128:(j + 1) * 128, :], zrow, reads=[r_zrow], writes=[rj])
    WinC = T([8, 2048], BF16); r_WinC = R("WinC")
    for kc in range(8):
        P.dma("pool", WinC[:, kc, :], w_in[kc * 128:(kc + 1) * 128, 2048:4096], writes=[r_WinC])
    Wglu = T([4, 512], BF16); Wps = T([4, 1024], BF16); Wpa = T([4, 1024], BF16); Wo = T([8, 1024], BF16)
    Wr = T([8, 36], BF16); r_W = R("Wsmall")
    P.dma("pool", Wglu, c["w_glu"].rearrange("(kc p) n -> p kc n", p=128), writes=[r_W])
    P.dma("pool", Wps, c["w_proj_ssm"].rearrange("(kc p) n -> p kc n", p=128), writes=[r_W])
    P.dma("pool", Wpa, c["w_proj_attn"].rearrange("(kc p) n -> p kc n", p=128), writes=[r_W])
    P.dma("pool", Wo, c["w_out"].rearrange("(kc p) n -> p kc n", p=128), writes=[r_W])
    P.dma("pool", Wr, c["w_router"].rearrange("(kc p) n -> p kc n", p=128), writes=[r_W])
    brow = T([512 + 36], BF16)
    P.dma("pool", brow[0:1, 0:512], c["b_glu"], writes=[r_W])
    P.dma("pool", brow[0:1, 512:548], c["b_router"], writes=[r_W])
    g2rep = T([D], F32)
    P.dma("pool", g2rep, c["norm2_g"][0:1, :].partition_broadcast(128), writes=[r_W])
    for k in ("r_hz", "WinC", "r_WinC", "Wglu", "Wps", "Wpa", "Wo", "Wr", "r_W", "brow", "g2rep"):
        cw[k] = locals()[k]
    c["cw"] = cw
    print("prefetch_C arena", P.arena_off)


def phase_B(c):
    nc, P, b, T, R, dump = c["nc"], c["P"], c["b"], c["T"], c["R"], c["dump"]
    pbank, pbank_bf = c["pbank"], c["pbank_bf"]
    identb, r_identb = c["identb"], c["r_identb"]
    ksum, r_ksum = c["ksum"], c["r_ksum"]
    kT_scr, qT_scr, v_scr, att_scr = c["kT_scr"], c["qT_scr"], c["v_scr"], c["att_scr"]
    r_kT_scr, r_qT_scr, r_v_scr, r_att_scr = c["r_kT_scr"], c["r_qT_scr"], c["r_v_scr"], c["r_att_scr"]
    P.release(c["m_ssm"])
    mB = P.mark()
    cvf = [T([1024], F32) for _ in range(3)]; r_cvf = [R(f"cvf{i}") for i in range(3)]
    cvb = [T([1024], BF16) for _ in range(3)]; r_cvb = [R(f"cvb{i}") for i in range(3)]
    r_wgu = c["r_wgu"]; r_wd = c["r_wd"]

    def conv_steps():
        jobs = []
        for e in range(32):
            jobs.append((c["w_gate"][e].rearrange("(kc p) f -> p kc f", p=128), c["wgu_scr"][e, :, 0], r_wgu[e], True))
            jobs.append((c["w_up"][e].rearrange("(kc p) f -> p kc f", p=128), c["wgu_scr"][e, :, 1], r_wgu[e], True))
            jobs.append((c["w_down"][e], c["wd_scr"][e], r_wd[e], False))
        n = len(jobs)
        for k in range(n + 2):
            if k < n:
                src, dst, rr_, three = jobs[k]
                o = cvf[k % 3].rearrange("p (kc f) -> p kc f", f=128) if three else cvf[k % 3]
                P.dma("sp", o, src, writes=[r_cvf[k % 3]])
            if 0 <= k - 1 < n:
                j = k - 1
                b.cp("pool", cvb[j % 3], cvf[j % 3], [r_cvf[j % 3]], [r_cvb[j % 3]])
            if 0 <= k - 2 < n:
                j = k - 2
                src, dst, rr_, three = jobs[j]
                i_ = cvb[j % 3].rearrange("p (kc f) -> p kc f", f=128) if three else cvb[j % 3]
                P.dma("sp", dst, i_, reads=[r_cvb[j % 3]], writes=[rr_])
            yield

    conv = conv_steps()

    kaug = [T([8192], BF16) for _ in range(2)]; r_kaug = [R("kaug0"), R("kaug1")]
    r_k1h = [R("k1h0"), R("k1h1")]
    qaug = [T([4096], BF16) for _ in range(2)]
    r_qd = [R("qd0"), R("qd1")]
    r_qb = [[R(f"qb{i}_{t}") for t in range(32)] for i in range(2)]
    vb = [T([64, 65], BF16) for _ in range(2)]; r_vb = [R("vb0"), R("vb1")]
    att_tok = T([32, 512], BF16); r_att = R("att_tok")
    kmeanT = T([8, 32], BF16); r_km = R("kmeanT")
    kmf = T([8, 32], F32)
    pbrep = T([16, 32], F32); pbm = T([16, 32], F32); r_pb = R("pb")
    tri = T([128], BF16); r_tri = R("tri")
    biaspad = [T([96], BF16) for _ in range(2)]; r_bp = [R("bp0"), R("bp1")]
    sm = [T([32], F32) for _ in range(2)]; mx8 = [T([8], F32) for _ in range(2)]; sel = [T([32], F32) for _ in range(2)]
    r_sm = [R("sm0"), R("sm1")]
    PTb = [T([512], BF16) for _ in range(4)]; r_PT = [R(f"PT{i}") for i in range(4)]
    rinv = [T([2], F32) for _ in range(2)]; r_rinv = [R("rinv0"), R("rinv1")]
    print("phase B arena", P.arena_off)
    c["cw_base"] = P.arena_off

    P.dma("sp", pbrep, c["pbrep_in"].rearrange("p (a b) -> p a b", b=32), writes=[r_pb])
    b.ts("dve", pbm, pbrep, NEG, None, ALU.add, None, [r_pb], [r_pb])
    P.dma("pool", tri, c["tri_in"], writes=[r_tri])
    for i in range(2):
        P.dma("pool", kaug[i][64:96, :], c["onehot_in"], writes=[r_k1h[i]])
        b.memset("dve", biaspad[i], 0.0, [r_bp[i]])
    b.reduce(kmf[0:64], ksum[0:64].rearrange("p h (n two) -> p h n two", two=2), ALU.add, [r_ksum], [r_km])
    b.ts("dve", kmeanT[0:64], kmf[0:64], 1.0 / 256, None, ALU.mult, None, [r_km], [r_km])

    def load_head(h):
        hb_ = h % 2
        P.dma("sp", kaug[hb_][0:64, :], kT_scr[h], reads=[r_kT_scr[h]], writes=[r_kaug[hb_]])
        P.dma("sp", qaug[hb_][0:64, :], qT_scr[h], reads=[r_qT_scr[h]], writes=[r_qd[hb_]])
        P.dma("sp", vb[hb_], v_scr[h].rearrange("p (t d) -> p t d", d=65), reads=[r_v_scr[h]], writes=[r_vb[hb_]])

    rt_cnt = {"n": 0}

    def route_stage1(h, t):
        hb_ = h % 2
        k_ = rt_cnt["n"] % 2
        rt_cnt["n"] += 1
        ob = t // 2
        psv, prs = pbank(0)
        sc = psv[:, (t % 8) * 32:(t % 8) * 32 + 32]
        b.mm(sc, qaug[hb_][0:64, t * 128:(t + 1) * 128], kmeanT[0:64, h, :], True, True, [r_qd[hb_], r_km], [prs])
        b.tt("dve", sm[k_], sc, pbrep[:, ob, :], ALU.add, [prs, r_pb], [r_sm[k_]])
        P.op("dve", lambda e: e.max(out=mx8[k_], in_=sm[k_]), [r_sm[k_]], [r_sm[k_]])
        b.ts("dve", sel[k_], sm[k_], mx8[k_][:, 2:3], None, ALU.is_ge, None, [r_sm[k_]], [r_sm[k_]])
        b.stt("dve", biaspad[k_][:, 64:96], sel[k_], -NEG, pbm[:, ob, :], ALU.mult, ALU.add, [r_sm[k_], r_pb], [r_bp[k_]])
        return k_

    def route_stage2(h, t, k_):
        hb_ = h % 2
        ptv, ptr_ = pbank_bf(0)
        o = ptv[0:96, 512 + (t % 4) * 128:512 + (t % 4) * 128 + 128]
        b.tr(o, biaspad[k_], identb, [r_bp[k_], r_identb], [ptr_])
        b.cp("dve", qaug[hb_][64:96, t * 128:(t + 1) * 128], o[64:96], [ptr_], [r_qb[hb_][t]])

    NSTB = 5
    LA = 4

    def emitS(h, ob, n, diag, slot):
        hb_ = h % 2
        psv, prs = pbank(1 + slot % NSTB)
        qc = slice(256 * ob, 256 * ob + 256)
        for half in range(2):
            kc = slice(n * 256 + half * 128, n * 256 + half * 128 + 128)
            if diag:
                b.mm(psv[:, half * 256:half * 256 + 256], kaug[hb_][0:64, kc], qaug[hb_][0:64, qc], True, True,
                     [r_kaug[hb_], r_qd[hb_]], [prs])
            else:
                b.mm(psv[:, half * 256:half * 256 + 256], kaug[hb_][0:96, kc], qaug[hb_][0:96, qc], True, True,
                     [r_kaug[hb_], r_k1h[hb_], r_qd[hb_], r_qb[hb_][2 * ob], r_qb[hb_][2 * ob + 1]], [prs])

    def emitPV(h, ob, n, diag, slot, first):
        hb_ = h % 2
        psv, prs = pbank(1 + slot % NSTB)
        pt = PTb[slot % 4]; rpt = r_PT[slot % 4]
        b.act(pt, psv, AF.Exp, [prs], [rpt], scale=0.125)
        pov, pors = pbank(6 + ob % 2)
        if diag:
            b.tt("pool", pt[:, 0:128], pt[:, 0:128], tri, ALU.mult, [rpt, r_tri], [rpt])
            b.tt("pool", pt[:, 384:512], pt[:, 384:512], tri, ALU.mult, [rpt, r_tri], [rpt])
        for j in range(2):
            for half in range(2):
                if diag and j == 0 and half == 1:
                    continue
                last = diag and (half == 1 or j == 0)
                b.mm(pov[:, j * 128:j * 128 + 65], pt[:, half * 256 + j * 128:half * 256 + j * 128 + 128],
                     vb[hb_][:, n * 2 + half, :], first and half == 0 and j == 0, last, [rpt, r_vb[hb_]], [pors])
        if diag:
            k_ = ob % 2
            po3 = pov[:, 0:256].rearrange("p (j x) -> p j x", x=128)
            b.recip(rinv[k_], po3[:, :, 64], [pors], [r_rinv[k_]])
            for j in range(2):
                b.ts("dve", att_tok[:, 2 * ob + j, h * 64:(h + 1) * 64], pov[:, j * 128:j * 128 + 64],
                     rinv[k_][:, j:j + 1], None, ALU.mult, None, [pors, r_rinv[k_]], [r_att])

    load_head(0)
    for t in range(32):
        k_ = route_stage1(0, t)
        route_stage2(0, t, k_)
    for h in range(8):
        if h + 1 < 8:
            load_head(h + 1)
        if h == 0:
            prefetch_C(c)
        items = []
        for ob in range(16):
            for n in range(q_block(ob)):
                items.append((ob, n, False, n == 0))
            items.append((ob, q_block(ob), True, False))
        pend = {}
        nxt_t = 0
        for i in range(len(items) + LA):
            if i < len(items):
                ob, n, diag, first = items[i]
                emitS(h, ob, n, diag, i)
            if i - LA >= 0:
                ob, n, diag, first = items[i - LA]
                emitPV(h, ob, n, diag, i - LA, first)
            if i % 12 == 6:
                next(conv, None)
            if h + 1 < 8:
                if i % 9 == 0 and nxt_t < 32:
                    pend[i + 5] = (nxt_t, route_stage1(h + 1, nxt_t))
                    nxt_t += 1
                if i in pend:
                    t_, k_ = pend.pop(i)
                    route_stage2(h + 1, t_, k_)
        for i in sorted(pend):
            t_, k_ = pend[i]
            route_stage2(h + 1, t_, k_)
        assert nxt_t == 32 or h == 7
    for _ in conv:
        pass
    P.dma("sp", att_scr.rearrange("(t p) c -> p t c", p=128), att_tok, reads=[r_att], writes=[r_att_scr])
    dump("att", att_tok, [128, 32, 512], [r_att], BF16)
    dump("qaug7", qaug[1], [128, 4096], [r_qd[1]] + r_qb[1], BF16)
    dump("kmeanT", kmeanT, [128, 8, 32], [r_km], BF16)
    P.barrier()
    P.release(mB)


def phase_CD(c):
    nc, P, b, T, R, dump = c["nc"], c["P"], c["b"], c["T"], c["R"], c["dump"]
    pbank, pbank_bf = c["pbank"], c["pbank_bf"]
    identb, r_identb, g1rep, r_g1 = c["identb"], c["r_identb"], c["g1rep"], c["r_g1"]
    x_all, w_in, y_out = c["x_all"], c["w_in"], c["y_out"]
    z_scr, att_scr, r_z_scr, r_att_scr = c["z_scr"], c["att_scr"], c["r_z_scr"], c["r_att_scr"]
    wgu_scr, wd_scr, r_wgu, r_wd = c["wgu_scr"], c["wd_scr"], c["r_wgu"], c["r_wd"]
    hs_scr, ys_scr = c["hs_scr"], c["ys_scr"]
    P.release(c["m_ssm"])
    NT = 32
    NSL = 96

    M1all = T([NT, 32], F32); M2all = T([NT, 32], F32); r_M = R("cMall")
    w12 = T([2, NT], F32); r_w12 = R("cw12")
    h2_scr = c["h2_scr"]
    h2s_stream = P.stream("h2s")
    r_h2s = []
    for t in range(NT):
        rj = R(f"ch2s{t}")
        rj.stream = h2s_stream
        r_h2s.append(rj)
    slot_i = T([2, NT], I32); r_slot = R("cslot")
    widx_i = T([NSL], I32); r_widx = R("cwidx")
    rconst = T([32 + NSL + 1], F32); r_rc = R("crconst")
    P.dma("sp", rconst, c["rconst_in"], writes=[r_rc])
    tri2 = T([128], BF16); r_tri2 = R("ctri2")
    P.dma("pool", tri2, c["tri_in"], writes=[r_tri2])
    ones = T([128], BF16); r_ones = R("cones")
    b.memset("dve", ones, 1.0, [r_ones])
    cw = c["cw"]
    r_hz, WinC, r_WinC, Wglu, Wps, Wpa, Wo, Wr, r_W, brow, g2rep = (cw[k] for k in
        ("r_hz", "WinC", "r_WinC", "Wglu", "Wps", "Wpa", "Wo", "Wr", "r_W", "brow", "g2rep"))
    mC = P.mark()

    xt = [T([D], F32) for _ in range(2)]; r_xt = [R("cxt0"), R("cxt1")]
    zt = [T([512], BF16) for _ in range(2)]; r_zt = [R("czt0"), R("czt1")]
    at = [T([512], BF16) for _ in range(2)]; r_at = [R("cat0"), R("cat1")]
    x1 = [T([D], F32) for _ in range(2)]; r_x1 = [R("cx1a"), R("cx1b")]
    r_y = [R(f"y_out{i}") for i in range(32)]
    y_stream = P.stream("ypark")
    for r_ in r_y:
        r_.stream = y_stream

    class PB:
        pass
    pbs = []
    for q in range(2):
        o = PB()
        o.hb = T([D], BF16); o.r_hb = R(f"chb{q}")
        o.st = T([8], F32); o.r_st = R(f"cst{q}")
        o.hTt = T([8, 128], BF16); o.r_hTt = R(f"chTt{q}")
        o.sgt = T([2048], BF16); o.r_sgt = R(f"csg{q}")
        o.zT = T([4, 128], BF16); o.r_zT = R(f"czT{q}")
        o.sgl = T([512], F32); o.r_sgl = R(f"csgl{q}")
        o.glu = T([512], BF16); o.r_glu = R(f"cglu{q}")
        o.gluT = T([4, 128], BF16); o.r_gluT = R(f"cgluT{q}")
        o.attT = T([4, 128], BF16); o.r_attT = R(f"cattT{q}")
        o.mg = T([D], BF16); o.r_mg = R(f"cmg{q}")
        o.mT = T([8, 128], BF16); o.r_mT = R(f"cmT{q}")
        o.h2 = T([D], BF16); o.r_h2 = R(f"ch2{q}")
        o.h2Tt = T([8, 128], BF16); o.r_h2Tt = R(f"ch2Tt{q}")
        o.lg = T([36], F32); o.rw = T([16], F32); o.gm = T([4], F32); o.em = T([4, 8], F32); o.mx = T([8], F32)
        o.r_rt = R(f"crt{q}")
        o.mm1 = T([512], F32); o.mm2 = T([512], F32); o.r_m1 = R(f"cm1{q}"); o.r_m2 = R(f"cm2{q}")
        o.bT, o.bM0, o.bM1, o.bX = (4 * q, 4 * q + 1, 4 * q + 2, 4 * q + 3)
        pbs.append(o)
    print("phase C arena", P.arena_off)
    assert P.arena_off <= c["cw_base"]

    def tile_src(base_ap, S, i):
        return base_ap[1024 * S:1024 * S + 1024].rearrange("(k j) d -> j k d", j=8)[i]

    def load_tile(tidx):
        S, i = tiles[tidx]
        par = tidx % 2
        P.dma("sp", xt[par], tile_src(x_all, 2 * S + 1, i), writes=[r_xt[par]])
        P.dma("sp", zt[par], z_scr[S][:, i * 512:(i + 1) * 512], reads=[r_z_scr[S]], writes=[r_zt[par]])
        P.dma("sp", at[par], tile_src(att_scr, S, i), reads=[r_att_scr], writes=[r_at[par]])

    def transposes(dst, r_dst, src, r_src, n, bank):
        ptv, ptr_ = pbank_bf(bank)
        for kc in range(n):
            b.tr(ptv[:, kc * 128:(kc + 1) * 128], src[:, kc * 128:(kc + 1) * 128], identb, [r_src, r_identb], [ptr_])
        b.cp("act", dst, ptv[:, 0:n * 128].rearrange("p (k t) -> p k t", t=128), [ptr_], [r_dst])

    tiles = [(S, i) for S in range(NST_OWN) for i in range(8)]

    def gen_C(par):
        o = pbs[par]
        for tidx in range(par, NT, 2):
            S, i = tiles[tidx]
            b.act(o.hb, xt[par], AF.Square, [r_xt[par]], [o.r_hb, o.r_st], accum=o.st[:, 0:1])
            b.act(o.st[:, 1:2], o.st[:, 0:1], AF.Sqrt, [o.r_st], [o.r_st], bias=EPS, scale=1.0 / D)
            b.recip(o.st[:, 1:2], o.st[:, 1:2], [o.r_st], [o.r_st])
            b.stt("dve", o.hb, xt[par], o.st[:, 1:2], g1rep, ALU.mult, ALU.mult, [r_xt[par], o.r_st, r_g1], [o.r_hb])
            yield
            transposes(o.hTt, o.r_hTt, o.hb, o.r_hb, 8, o.bT)
            yield
            for cc in range(4):
                psv, prs = pbank(o.bM0 if cc % 2 == 0 else o.bM1)
                for kc in range(8):
                    b.mm(psv, o.hTt[:, kc, :], WinC[:, kc, cc * 512:(cc + 1) * 512], kc == 0, kc == 7, [o.r_hTt, r_WinC], [prs])
                b.act(o.sgt[:, cc * 512:(cc + 1) * 512], psv, AF.Sigmoid, [prs], [o.r_sgt])
                yield
            transposes(o.zT, o.r_zT, zt[par], r_zt[par], 4, o.bX)
            yield
            psv, prs = pbank(o.bM0)
            for kc in range(4):
                b.mm(psv, o.zT[:, kc, :], Wglu[:, kc, :], kc == 0, False, [o.r_zT, r_W], [prs])
            b.mm(psv, ones[0:1, :], brow[0:1, 0:512], False, True, [r_W, r_ones], [prs])
            b.act(o.sgl, psv, AF.Sigmoid, [prs], [o.r_sgl])
            b.tt("pool", o.glu, zt[par], o.sgl, ALU.mult, [r_zt[par], o.r_sgl], [o.r_glu])
            transposes(o.attT, o.r_attT, at[par], r_at[par], 4, o.bT)
            yield
            transposes(o.gluT, o.r_gluT, o.glu, o.r_glu, 4, o.bX)
            yield
            for cc in range(2):
                ps1, pr1 = pbank(o.bM0)
                ps2, pr2 = pbank(o.bM1)
                for kc in range(4):
                    b.mm(ps2, o.attT[:, kc, :], Wpa[:, kc, cc * 512:(cc + 1) * 512], kc == 0, kc == 3, [o.r_attT, r_W], [pr2])
                for kc in range(4):
                    b.mm(ps1, o.gluT[:, kc, :], Wps[:, kc, cc * 512:(cc + 1) * 512], kc == 0, kc == 3, [o.r_gluT, r_W], [pr1])
                b.tt("dve", o.mm2, ps2, o.sgt[:, 1024 + cc * 512:1024 + (cc + 1) * 512], ALU.mult, [pr2, o.r_sgt], [o.r_m2])
                b.tt("dve", o.mm1, ps1, o.sgt[:, cc * 512:(cc + 1) * 512], ALU.mult, [pr1, o.r_sgt], [o.r_m1])
                b.tt("pool", o.mg[:, cc * 512:(cc + 1) * 512], o.mm1, o.mm2, ALU.add, [o.r_m1, o.r_m2], [o.r_mg])
                yield
            transposes(o.mT, o.r_mT, o.mg, o.r_mg, 8, o.bT)
            yield
            for cc in range(2):
                psv, prs = pbank(o.bM0 if cc == 0 else o.bM1)
                for kc in range(8):
                    b.mm(psv, o.mT[:, kc, :], Wo[:, kc, cc * 512:(cc + 1) * 512], kc == 0, kc == 7, [o.r_mT, r_W], [prs])
                b.tt("dve", x1[par][:, cc * 512:(cc + 1) * 512], psv, xt[par][:, cc * 512:(cc + 1) * 512], ALU.add,
                     [prs, r_xt[par]], [r_x1[par]])
            P.dma("sp", tile_src(y_out, S, i), x1[par], reads=[r_x1[par]], writes=[r_y[tidx]])
            if tidx + 2 < NT:
                load_tile(tidx + 2)
            yield
            h2 = o.h2; r_h2 = o.r_h2
            b.act(h2, x1[par], AF.Square, [r_x1[par]], [r_h2, o.r_st], accum=o.st[:, 2:3])
            b.act(o.st[:, 3:4], o.st[:, 2:3], AF.Sqrt, [o.r_st], [o.r_st], bias=EPS, scale=1.0 / D)
            b.recip(o.st[:, 3:4], o.st[:, 3:4], [o.r_st], [o.r_st])
            b.stt("dve", h2, x1[par], o.st[:, 3:4], g2rep, ALU.mult, ALU.mult, [r_x1[par], o.r_st, r_W], [r_h2])
            P.dma("sp", h2_scr[tidx * 128:(tidx + 1) * 128, :], h2, reads=[r_h2], writes=[r_h2s[tidx]])
            yield
            transposes(o.h2Tt, o.r_h2Tt, h2, r_h2, 8, o.bX)
            yield
            psv, prs = pbank(o.bM0)
            for kc in range(8):
                b.mm(psv[:, 0:36], o.h2Tt[:, kc, :], Wr[:, kc, :], kc == 0, False, [o.r_h2Tt, r_W], [prs])
            b.mm(psv[:, 0:36], ones[0:1, :], brow[0:1, 512:548], False, True, [r_W, r_ones], [prs])
            rt = [o.r_rt]
            lg, rw, gm, em, mx = o.lg, o.rw, o.gm, o.em, o.mx
            b.cp("dve", lg, psv[:, 0:36], [prs], rt)
            b.reduce(rw[:, 0:1], lg[:, 0:4], ALU.max, rt, rt)
            b.ts("dve", rw[:, 1:2], rw[:, 0:1], -1.0, None, ALU.mult, None, rt, rt)
            b.act(gm, lg[:, 0:4], AF.Exp, rt, rt, bias=rw[:, 1:2], scale=1.0, accum=rw[:, 2:3])
            b.recip(rw[:, 3:4], rw[:, 2:3], rt, rt)
            yield
            b.ts("dve", gm, lg[:, 0:4], rw[:, 0:1], None, ALU.is_ge, None, rt, rt)
            b.ts("dve", gm, gm, 10000.0, -10000.0, ALU.mult, ALU.add, rt, rt)
            b.tt("dve", em, lg[:, 4:36].rearrange("p (g e) -> p g e", e=8), bc(gm.unsqueeze(2), [128, 4, 8]), ALU.add, rt, rt)
            P.op("dve", (lambda mx=mx, em=em: (lambda e: e.max(out=mx, in_=em.rearrange("p g e -> p (g e)"))))(), rt, rt)
            b.tt("dve", rw[:, 4:5], mx[:, 0:1], mx[:, 1:2], ALU.subtract, rt, rt)
            b.act(rw[:, 5:6], rw[:, 4:5], AF.Sigmoid, rt, rt)
            yield
            b.tt("dve", w12[:, 0, tidx:tidx + 1], rw[:, 5:6], rw[:, 3:4], ALU.mult, rt, [r_w12])
            b.tt("dve", w12[:, 1, tidx:tidx + 1], rw[:, 3:4], w12[:, 0, tidx:tidx + 1], ALU.subtract, rt + [r_w12], [r_w12])
            emf = em.rearrange("p g e -> p (g e)")
            b.ts("dve", M1all[:, tidx, :], emf, mx[:, 0:1], None, ALU.is_equal, None, rt, [r_M])
            b.ts("dve", M2all[:, tidx, :], emf, mx[:, 1:2], None, ALU.is_equal, None, rt, [r_M])
            yield

    load_tile(0)
    load_tile(1)
    g0 = gen_C(0)
    g1 = gen_C(1)
    for _ in range(8):
        next(g0)
    run_interleaved(g0, g1)
    P.barrier()
    P.release(mC)

    rr = R("cR"); rt = [rr]
    M1f = M1all.rearrange("p t e -> p (t e)"); M2f = M2all.rearrange("p t e -> p (t e)")
    Mf = T([1024], F32); Mb = T([1024], BF16); r_Mb = R("cMb")
    cumS = T([1024], F32); totS = T([1024], F32); r_cum = R("ccum"); r_tot = R("ctot")
    b.tt("dve", Mf, M1f, M2f, ALU.add, [r_M], rt)
    b.cp("dve", Mb, Mf, rt, [r_Mb])
    for hf in range(2):
        sl = slice(hf * 512, hf * 512 + 512)
        pv, prs = pbank(hf)
        b.mm(pv, tri2, Mb[:, sl], True, True, [r_tri2, r_Mb], [prs])
        b.cp("act", cumS[:, sl], pv, [prs], [r_cum])
        pv2, prs2 = pbank(2 + hf)
        b.mm(pv2, ones, Mb[:, sl], True, True, [r_ones, r_Mb], [prs2])
        b.cp("dve", totS[:, sl], pv2, [prs2], [r_tot])
    thr = rconst[:, 0:32]; jv = rconst[:, 32:32 + NSL]; pidx = rconst[:, 32 + NSL:33 + NSL]
    cnt_e = T([32], F32); ntile = T([32], F32); base = T([32], F32)
    cmp3 = T([NSL, 32], F32)
    pbuf = [T([1024], F32) for _ in range(2)]
    sbuf_ = [T([32], F32) for _ in range(2)]

    def prefix(src, n, unit, bufs):
        cur = src; s = 1; k = 0
        L = n * unit
        while s < n:
            dst = bufs[k % 2]; k += 1
            sh = s * unit
            b.cp("dve", dst[:, 0:sh], cur[:, 0:sh], rt + [r_tot], rt)
            b.tt("dve", dst[:, sh:L], cur[:, sh:L], cur[:, 0:L - sh], ALU.add, rt + [r_tot], rt)
            cur = dst; s *= 2
        return cur

    b.reduce(cnt_e, totS.rearrange("p (t e) -> p e t", e=32), ALU.add, [r_tot], rt)
    c3 = cmp3[:, 0:32, :]
    b.tt("dve", c3, bc(cnt_e.unsqueeze(2), [128, 32, 32]), bc(thr.unsqueeze(1), [128, 32, 32]), ALU.is_gt, rt + [r_rc], rt)
    b.reduce(ntile, c3, ALU.add, rt, rt)
    cti = prefix(ntile, 32, 1, sbuf_)
    b.tt("dve", base, cti, ntile, ALU.subtract, rt, rt)
    b.ts("dve", base, base, 128.0, None, ALU.mult, None, rt, rt)
    inc_t = prefix(totS, 32, 32, pbuf)
    G = T([1024], F32)
    b.tt("dve", G, inc_t, totS, ALU.subtract, rt + [r_tot], rt)
    G3 = G.rearrange("p (t e) -> p t e", e=32)
    b.tt("dve", G3, G3, bc(base.unsqueeze(1), [128, 32, 32]), ALU.add, rt, rt)
    b.tt("dve", G, G, cumS, ALU.add, rt + [r_cum], rt)
    sl_f = T([2, NT], F32)
    q1 = T([1024], F32)
    for k, Mk in ((0, M1f), (1, M2f)):
        b.tt("dve", q1, G, Mk, ALU.mult, rt + [r_M], rt)
        b.reduce(sl_f[:, k, :], q1.rearrange("p (t e) -> p t e", e=32), ALU.add, rt, rt)
    b.ts("dve", sl_f, sl_f, -1.0, float(NSL * 128 - 1), ALU.add, ALU.min, rt, rt)
    b.ts("dve", sl_f, sl_f, 0.0, None, ALU.max, None, rt, rt)
    b.cp("dve", slot_i, sl_f, rt, [r_slot])
    b.tt("dve", cmp3, bc(cti.unsqueeze(1), [128, NSL, 32]), bc(jv.unsqueeze(2), [128, NSL, 32]), ALU.is_le, rt + [r_rc], rt)
    eid = T([NSL], F32)
    b.reduce(eid, cmp3, ALU.add, rt, rt)
    b.ts("dve", eid, eid, 31.0, 128.0, ALU.min, ALU.mult, rt, rt)
    b.tt("dve", eid, eid, bc(pidx, [128, NSL]), ALU.add, rt + [r_rc], rt)
    b.cp("dve", widx_i, eid, rt, [r_widx])
    dump("slot_i", slot_i, [128, 2, NT], [r_slot], I32)
    dump("widx_i", widx_i, [128, NSL], [r_widx], I32)
    dump("w12", w12, [128, 2, NT], [r_w12])
    dump("M1all", M1all, [128, NT, 32], [r_M])
    dump("M2all", M2all, [128, NT, 32], [r_M])

    sc_stream = P.stream("sc")
    r_sc = []
    h2l = [T([D], BF16) for _ in range(8)]; r_h2l = [R(f"ch2l{i}") for i in range(8)]
    for t in range(NT):
        P.dma("sp", h2l[t % 8], h2_scr[t * 128:(t + 1) * 128, :], reads=r_h2s, writes=[r_h2l[t % 8]])
        for k in range(2):
            rj = R(f"csc{t}_{k}")
            rj.stream = sc_stream
            r_sc.append(rj)
            P.idma(hs_scr[:, :], h2l[t % 8], slot_i[:, k, t:t + 1], False,
                   reads=[r_h2l[t % 8], r_slot] + r_hz, writes=[rj])

    NB = 6
    PF = 4
    hs = [T([D], BF16) for _ in range(NB)]; r_hs = [R(f"dhs{i}") for i in range(NB)]
    wgu = [T([2, 8, 128], BF16) for _ in range(NB)]; r_wgs = [R(f"dwgu{i}") for i in range(NB)]
    wdn = [T([D], BF16) for _ in range(NB)]; r_wds = [R(f"dwdn{i}") for i in range(NB)]
    hsT = [T([8, 128], BF16) for _ in range(2)]; r_hsT = [R("dhsT0"), R("dhsT1")]
    sg = [T([128], F32) for _ in range(2)]; r_sg = [R("dsg0"), R("dsg1")]
    actb = [T([128], BF16) for _ in range(2)]; r_actb = [R("dact0"), R("dact1")]
    yt = [T([D], F32) for _ in range(2)]; r_yt = [R("dyt0"), R("dyt1")]
    ys_stream = P.stream("ys")
    r_ys = []
    wgu2d = wgu_scr.rearrange("e p g k f -> (e p) (g k f)")
    wd2d = wd_scr.rearrange("e p d -> (e p) d")
    print("phase D arena", P.arena_off)

    def d_load(j):
        s4 = j % NB
        P.dma("sp", hs[s4], hs_scr[j * 128:(j + 1) * 128, :], reads=r_sc, writes=[r_hs[s4]])
        P.idma(wgu[s4].rearrange("p g k f -> p (g k f)"), wgu2d, widx_i[:, j:j + 1], True,
               reads=[r_widx] + list(set(r_wgu)), writes=[r_wgs[s4]])
        P.idma(wdn[s4], wd2d, widx_i[:, j:j + 1], True, reads=[r_widx] + list(set(r_wd)), writes=[r_wds[s4]])

    def d_T(j):
        s4 = j % NB; s2 = j % 2
        ptv, ptr_ = pbank_bf(s2)
        for kc in range(8):
            b.tr(ptv[:, kc * 128:(kc + 1) * 128], hs[s4][:, kc * 128:(kc + 1) * 128], identb, [r_hs[s4], r_identb], [ptr_])
        b.cp("act", hsT[s2], ptv.rearrange("p (k t) -> p k t", t=128), [ptr_], [r_hsT[s2]])

    def d_GU(j):
        s4 = j % NB; s2 = j % 2
        psv, prs = pbank(2 + s2)
        for g in range(2):
            for kc in range(8):
                b.mm(psv[:, g * 128:(g + 1) * 128], wgu[s4][:, g, kc, :], hsT[s2][:, kc, :], kc == 0, kc == 7,
                     [r_wgs[s4], r_hsT[s2]], [prs])
        b.act(sg[s2], psv[:, 0:128], AF.Silu, [prs], [r_sg[s2]])
        b.tt("dve", actb[s2], psv[:, 128:256], sg[s2], ALU.mult, [prs, r_sg[s2]], [r_actb[s2]])

    def d_DN(j):
        s4 = j % NB; s2 = j % 2
        for cc in range(2):
            psv, prs = pbank(4 + 2 * s2 + cc)
            b.mm(psv, actb[s2], wdn[s4][:, cc * 512:(cc + 1) * 512], True, True, [r_actb[s2], r_wds[s4]], [prs])
            if cc == 0:
                b.cp("act", yt[s2][:, 0:512], psv, [prs], [r_yt[s2]])
            else:
                b.cp("dve", yt[s2][:, 512:1024], psv, [prs], [r_yt[s2]])
        rj = R(f"dys{j}")
        rj.stream = ys_stream
        r_ys.append(rj)
        P.dma("sp", ys_scr[j * 128:(j + 1) * 128, :], yt[s2], reads=[r_yt[s2]], writes=[rj])

    for j in range(PF):
        d_load(j)
    for it in range(NSL + 2):
        if it < NSL:
            d_T(it)
        if 0 <= it - 1 < NSL:
            d_GU(it - 1)
        if 0 <= it - 2 < NSL:
            d_DN(it - 2)
        if it + PF < NSL:
            d_load(it + PF)

    xb = [T([D], F32) for _ in range(4)]; r_xb = [R(f"exb{i}") for i in range(4)]
    ya = [T([D], F32) for _ in range(4)]; r_ya = [R(f"eya{i}") for i in range(4)]
    yb = [T([D], F32) for _ in range(4)]; r_yb = [R(f"eyb{i}") for i in range(4)]
    print("phase E arena", P.arena_off)

    def e_load(t):
        S, i = tiles[t]
        s2 = t % 4
        P.dma("sp", xb[s2], tile_src(y_out, S, i), reads=[r_y[t]], writes=[r_xb[s2]])
        P.idma(ya[s2], ys_scr[:, :], slot_i[:, 0, t:t + 1], True, reads=[r_slot] + r_ys, writes=[r_ya[s2]])
        P.idma(yb[s2], ys_scr[:, :], slot_i[:, 1, t:t + 1], True, reads=[r_slot] + r_ys, writes=[r_yb[s2]])

    e_load(0)
    e_load(1)
    e_load(2)
    for t in range(NT):
        S, i = tiles[t]
        s2 = t % 4
        if t + 3 < NT:
            e_load(t + 3)
        b.stt("dve", ya[s2], ya[s2], w12[:, 0, t:t + 1], xb[s2], ALU.mult, ALU.add, [r_ya[s2], r_xb[s2], r_w12], [r_ya[s2]])
        b.stt("dve", ya[s2], yb[s2], w12[:, 1, t:t + 1], ya[s2], ALU.mult, ALU.add, [r_ya[s2], r_yb[s2], r_w12], [r_ya[s2]])
        P.dma("sp", tile_src(y_out, S, i), ya[s2], reads=[r_ya[s2]], writes=[r_y[t]])
    P.barrier()

def _const_tables(p):
    f32 = np.float32
    half = 8
    inv_freq = (np.float32(500000.0) ** (-(np.arange(half, dtype=f32) * f32(2.0) / f32(16.0)))).astype(f32)
    pos = (np.arange(8192) - (0 if p == 1 else 1024)).astype(f32)
    ang = (pos[:, None] * inv_freq[None, :]).astype(f32)
    cs = np.concatenate([np.cos(ang), np.sin(ang)], axis=1).astype(f32)
    cs_tab = cs.reshape(64, 128, 16).transpose(1, 0, 2).reshape(128, 64 * 16)
    pb = np.zeros((16, 32), f32)
    for ob in range(16):
        pb[ob, q_block(ob):] = NEG
        if p == 0:
            pb[ob, :4] = NEG
    pbrep = np.broadcast_to(pb.reshape(1, 512), (128, 512)).copy()
    onehot = np.zeros((32, 8192), f32)
    for n in range(32):
        onehot[n, n * 256:(n + 1) * 256] = 1.0
    tri = (np.arange(128)[:, None] <= np.arange(128)[None, :]).astype(f32)
    rconst = np.zeros((128, 32 + 96 + 1), f32)
    rconst[:, 0:32] = 128.0 * np.arange(32, dtype=f32)[None, :]
    rconst[:, 32:128] = np.arange(96, dtype=f32)[None, :]
    rconst[:, 128] = np.arange(128, dtype=f32)
    return dict(cs_tab=np.ascontiguousarray(cs_tab), pbrep=pbrep, onehot_k=onehot, tri=tri,
                rconst=rconst, ident_in=np.eye(128, dtype=f32))


def make_in_maps(inputs):
    f32 = np.float32
    x = np.asarray(inputs["x"], f32)
    g = lambda k: np.ascontiguousarray(np.asarray(inputs[k], f32)[0])
    shared = dict(
        w_in=g("w_in"), norm1_g=g("norm1_g").reshape(1, D),
        lamre_t=np.ascontiguousarray(g("lam_re").T), lamim_t=np.ascontiguousarray(g("lam_im").T),
        log_dt=g("log_dt").reshape(1, 32),
        b_re_t=np.ascontiguousarray(g("ssm_b_re").transpose(1, 0, 2)),
        b_im_t=np.ascontiguousarray(g("ssm_b_im").transpose(1, 0, 2)),
        c_re_t=np.ascontiguousarray(g("ssm_c_re").transpose(2, 0, 1)),
        c_im_t=np.ascontiguousarray(g("ssm_c_im").transpose(2, 0, 1)),
        ssm_d=g("ssm_d").reshape(1, 512), w_glu=g("w_glu"), b_glu=g("b_glu").reshape(1, 512),
        q_norm_g=g("q_norm_g").reshape(1, 64), k_norm_g=g("k_norm_g").reshape(1, 64),
        w_proj_ssm=g("w_proj_ssm"), w_proj_attn=g("w_proj_attn"), w_out=g("w_out"),
        norm2_g=g("norm2_g").reshape(1, D),
        w_router=np.ascontiguousarray(np.concatenate([g("w_router_group"), g("w_router_expert")], axis=1)),
        b_router=np.concatenate([g("b_router_group"), g("b_router_expert")]).reshape(1, 36),
        w_gate=g("w_gate"), w_up=g("w_up"), w_down=g("w_down"),
    )
    tabs = [_const_tables(0), _const_tables(1)]
    zeros = np.zeros((1024, D), f32)
    in_maps = []
    for c in range(NCORES):
        bi, p = c // 2, c % 2
        m = dict(shared)
        m.update(tabs[p])
        m["x_all"] = np.ascontiguousarray(x[bi]) if p == 1 else np.concatenate([zeros, x[bi, 0:7168]], axis=0)
        in_maps.append(m)
    return in_maps


_CACHE = {}


def kernel(**inputs):
    in_maps = make_in_maps(inputs)
    if "nc" not in _CACHE:
        _CACHE["nc"] = build()[0]
    nc = _CACHE["nc"]
    res = run_bass_kernel_spmd(nc, in_maps, core_ids=list(range(NCORES)))
    x = np.asarray(inputs["x"])
    out = np.empty(x.shape, np.float32)
    for c in range(NCORES):
        bi, p = c // 2, c % 2
        y = res.results[c]["y_out"]
        for S in range(NST_OWN):
            g0 = 1024 * (2 * S + p)
            out[bi, g0:g0 + 1024] = y[1024 * S:1024 * S + 1024]
    return out
```

```python
import math
import os
from contextlib import ExitStack

import numpy as np
import ml_dtypes

import concourse.bass as bass
import concourse.mybir as mybir
from concourse.bass_utils import run_bass_kernel_spmd

F32 = mybir.dt.float32
BF16 = mybir.dt.bfloat16
I32 = mybir.dt.int32
AF = mybir.ActivationFunctionType
ALU = mybir.AluOpType
AX = mybir.AxisListType

NCORES = 8
D = 1024
S_OWN = 4096
S_CTX = 4096
NST_CTX = 4
NST_OWN = 4
NST = NST_CTX + NST_OWN
NEG = -30000.0
EPS = 1e-6
TWO_PI = 2.0 * math.pi


class Res:
    __slots__ = ("name", "last_w", "readers", "stream")

    def __init__(self, name):
        self.name = name
        self.last_w = None
        self.readers = []
        self.stream = None


class Ins:
    __slots__ = ("idx", "eng", "fn", "deps", "marked", "count", "dma_sem", "dma_val", "is_dma",
                 "bar_streams")


class Prog:
    ENGS = ("pe", "act", "dve", "pool", "sp")

    def __init__(self, nc, sbuf_elems):
        self.nc = nc
        self.ins = []
        self.per_eng = {e: [] for e in self.ENGS}
        self.stack = ExitStack()
        self.dma_streams = []
        self.nres = 0
        self.bar = None
        self.recording = None
        self.arena = self.stack.enter_context(nc.sbuf_tensor("arena", [128, sbuf_elems], BF16))
        self.arena_off = 0
        self.arena_size = sbuf_elems
        self.arena_peak = 0

    def tile(self, shape, dtype):
        n = 1
        for s in shape:
            n *= s
        nb = n * (2 if dtype == F32 or dtype == I32 else 1)
        nb = (nb + 15) // 16 * 16
        off = self.arena_off
        self.arena_off += nb
        self.arena_peak = max(self.arena_peak, self.arena_off)
        assert self.arena_off <= self.arena_size, f"SBUF arena overflow {self.arena_off}"
        ap = self.arena[:, off:off + nb]
        if dtype != BF16:
            ap = ap.bitcast(dtype)
            nb //= 2
        ap = ap[:, 0:n]
        if len(shape) == 2:
            ap = ap.rearrange("p (a b) -> p a b", b=shape[1])
        elif len(shape) == 3:
            ap = ap.rearrange("p (a b c) -> p a b c", b=shape[1], c=shape[2])
        elif len(shape) == 4:
            ap = ap.rearrange("p (a b c d) -> p a b c d", b=shape[1], c=shape[2], d=shape[3])
        return ap

    def mark(self):
        return self.arena_off

    def release(self, m):
        self.arena_off = m

    def sem(self, name):
        return self.stack.enter_context(self.nc.semaphore(name))

    def res(self, name=None):
        self.nres += 1
        return Res(name or f"r{self.nres}")

    def stream(self, name):
        st = {"sem": self.sem(f"dq{len(self.dma_streams)}"), "val": 0}
        self.dma_streams.append(st)
        return st

    def barrier(self):
        last = []
        for e in ("pe", "act", "dve", "pool"):
            for I in reversed(self.per_eng[e]):
                if not I.is_dma:
                    last.append(I)
                    break
        snap = [(st, st["val"]) for st in self.dma_streams if st["val"] > 0]
        self.bar = {e: (list(last), snap) for e in self.ENGS}

    def _add(self, eng, fn, reads, writes, is_dma=False, stream=None):
        I = Ins()
        I.idx = len(self.ins)
        I.eng = eng
        I.fn = fn
        I.marked = False
        I.count = None
        I.is_dma = is_dma
        I.dma_sem = None
        I.dma_val = None
        I.bar_streams = None
        if is_dma:
            stream["val"] += 16
            I.dma_sem = stream["sem"]
            I.dma_val = stream["val"]
        deps = {}

        def add_dep(d, raw):
            if d is None:
                return
            if (not d.is_dma) and (not is_dma) and d.eng == eng:
                if eng == "pe" or (not raw and not SYNC_SAME_ENGINE_WAR):
                    return
            deps[d.idx] = d

        for r in reads:
            add_dep(r.last_w, True)
        for w in writes:
            add_dep(w.last_w, False)
            for rd in w.readers:
                add_dep(rd, False)
        for r in reads:
            r.readers.append(I)
        for w in writes:
            w.last_w = I
            w.readers = []
        if self.bar is not None and eng in self.bar:
            last, snap = self.bar.pop(eng)
            for d in last:
                if d.eng != eng or is_dma:
                    deps[d.idx] = d
            I.bar_streams = snap
        I.deps = list(deps.values())
        self.ins.append(I)
        self.per_eng[eng].append(I)
        return I

    def op(self, eng, fn, reads=(), writes=()):
        if self.recording is not None:
            reads, writes = list(reads), list(writes)
            self.recording.append(lambda: self._add(eng, fn, reads, writes))
            return None
        return self._add(eng, fn, reads, writes)

    def dma(self, eng, out, in_, reads=(), writes=(), slow=False):
        if self.recording is not None:
            reads, writes = list(reads), list(writes)
            rec = self.recording
            self.recording = None
            rec.append(lambda: self.dma(eng, out, in_, reads, writes, slow))
            self.recording = rec
            return None
        w = writes[0]
        if w.stream is None:
            w.stream = self.stream(w.name)
        if slow:
            f = lambda e: e.dma_start(out=out, in_=in_, allow_slow_non_contiguous=True)
        else:
            f = lambda e: e.dma_start(out=out, in_=in_)
        return self._add(eng, f, reads, writes, is_dma=True, stream=w.stream)

    def idma(self, out, in_, idx, gather, reads=(), writes=()):
        w = writes[0]
        if w.stream is None:
            w.stream = self.stream(w.name)
        if gather:
            f = lambda e: e.indirect_dma_start(out=out, out_offset=None, in_=in_,
                                               in_offset=bass.IndirectOffsetOnAxis(ap=idx, axis=0))
        else:
            f = lambda e: e.indirect_dma_start(out=out, out_offset=bass.IndirectOffsetOnAxis(ap=idx, axis=0),
                                               in_=in_, in_offset=None)
        return self._add("pool", f, reads, writes, is_dma=True, stream=w.stream)

    def finalize(self):
        nc = self.nc
        final_waits = [(st["sem"], st["val"]) for st in self.dma_streams if st["val"] > 0]
        for I in self.ins:
            for d in I.deps:
                if not d.is_dma:
                    d.marked = True
        engsem = {}
        for e in ("pe", "act", "dve", "pool"):
            engsem[e] = self.sem("es_" + e)
            c = 0
            for I in self.per_eng[e]:
                if I.is_dma:
                    continue
                if I.marked:
                    c += 1
                    I.count = c
        nwaits = 0
        with nc.Block() as block:
            def emit(ename):
                def body(e):
                    nonlocal nwaits
                    waited = {}
                    for I in self.per_eng[ename]:
                        need = {}
                        for d in I.deps:
                            if d.is_dma:
                                key = ("d", id(d.dma_sem))
                                sem, val = d.dma_sem, d.dma_val
                            else:
                                key = ("e", d.eng)
                                sem, val = engsem[d.eng], d.count
                            if key not in need or need[key][1] < val:
                                need[key] = (sem, val)
                        if I.bar_streams:
                            for st, val in I.bar_streams:
                                key = ("d", id(st["sem"]))
                                if key not in need or need[key][1] < val:
                                    need[key] = (st["sem"], val)
                        for key, (sem, val) in need.items():
                            if waited.get(key, 0) >= val:
                                continue
                            e.wait_ge(sem, val)
                            nwaits += 1
                            waited[key] = val
                        bi = I.fn(e)
                        if I.is_dma:
                            bi.then_inc(I.dma_sem, 16)
                        elif I.marked:
                            bi.then_inc(engsem[ename], 1)
                    if ename == "sp":
                        for (sem, val) in final_waits:
                            e.wait_ge(sem, val)
                return body
            block.tensor(emit("pe"))
            block.scalar(emit("act"))
            block.vector(emit("dve"))
            block.gpsimd(emit("pool"))
            block.sync(emit("sp"))
        self.nwaits = nwaits
        self.stack.close()


class B:
    def __init__(self, P):
        self.P = P

    def mm(self, out, lhsT, rhs, start, stop, reads, writes):
        self.P.op("pe", lambda e: e.matmul(out, lhsT, rhs, start=start, stop=stop), reads, writes)

    def tr(self, out, in_, ident, reads, writes):
        self.P.op("pe", lambda e: e.transpose(out, in_, ident), reads, writes)

    def act(self, out, in_, func, reads, writes, bias=0.0, scale=1.0, accum=None):
        if accum is None:
            self.P.op("act", lambda e: e.activation(out=out, in_=in_, func=func, bias=bias, scale=scale),
                      reads, writes)
        else:
            self.P.op("act", lambda e: e.activation(out=out, in_=in_, func=func, bias=bias, scale=scale,
                                                    accum_out=accum), reads, writes)

    def tt(self, eng, out, in0, in1, op, reads, writes):
        self.P.op(eng, lambda e: e.tensor_tensor(out=out, in0=in0, in1=in1, op=op), reads, writes)

    def ts(self, eng, out, in0, s1, s2, op0, op1, reads, writes):
        if s2 is None:
            self.P.op(eng, lambda e: e.tensor_scalar(out=out, in0=in0, scalar1=s1, scalar2=None, op0=op0),
                      reads, writes)
        else:
            self.P.op(eng, lambda e: e.tensor_scalar(out=out, in0=in0, scalar1=s1, scalar2=s2, op0=op0, op1=op1),
                      reads, writes)

    def stt(self, eng, out, in0, scalar, in1, op0, op1, reads, writes):
        self.P.op(eng, lambda e: e.scalar_tensor_tensor(out=out, in0=in0, scalar=scalar, in1=in1,
                                                         op0=op0, op1=op1), reads, writes)

    def cp(self, eng, out, in_, reads, writes):
        if eng == "act":
            self.P.op("act", lambda e: e.copy(out=out, in_=in_), reads, writes)
        else:
            self.P.op(eng, lambda e: e.tensor_copy(out=out, in_=in_), reads, writes)

    def memset(self, eng, out, val, writes):
        self.P.op(eng, lambda e: e.memset(out, val), (), writes)

    def recip(self, out, in_, reads, writes):
        self.P.op("dve", lambda e: e.reciprocal(out=out, in_=in_), reads, writes)

    def reduce(self, out, in_, op, reads, writes, eng="dve"):
        self.P.op(eng, lambda e: e.tensor_reduce(out=out, in_=in_, axis=AX.X, op=op), reads, writes)


def is_own(s):
    return s % 2 == 1


def q_block(ob):
    return 4 * (2 * (ob // 4) + 1) + ob % 4


def bc(ap, shape):
    return ap.to_broadcast(list(shape))


SYNC_SAME_ENGINE_WAR = False
ARENA_ELEMS = 106400
P0_BASE = 74272


def build(debug=(), stop_after="D"):
    nc = bass.Bass("TRN2", target_bir_lowering=False)
    P = Prog(nc, ARENA_ELEMS)
    b = B(P)
    T = P.tile
    R = P.res
    dbg_out = {}

    def din(name, shape, dt=F32):
        return nc.dram_tensor(name, list(shape), dt, kind="ExternalInput").ap()

    def dscr(name, shape, dt):
        return nc.dram_tensor(name, list(shape), dt, kind="Internal").ap()

    def dump(name, ap_sb, shape, reads, dt=F32, eng="sp"):
        if name not in debug:
            return
        o = nc.dram_tensor("dbg_" + name, list(shape), dt, kind="ExternalOutput").ap()
        dbg_out[name] = o
        P.dma(eng, o, ap_sb, reads=reads, writes=[R("dbg_" + name)])

    x_all = din("x_all", [S_CTX + S_OWN, D])
    w_in = din("w_in", [D, 4096])
    norm1_g = din("norm1_g", [1, D])
    lamre_t = din("lamre_t", [64, 32])
    lamim_t = din("lamim_t", [64, 32])
    log_dt = din("log_dt", [1, 32])
    b_re_t = din("b_re_t", [64, 32, 16])
    b_im_t = din("b_im_t", [64, 32, 16])
    c_re_t = din("c_re_t", [64, 32, 16])
    c_im_t = din("c_im_t", [64, 32, 16])
    ssm_d = din("ssm_d", [1, 512])
    w_glu = din("w_glu", [512, 512])
    b_glu = din("b_glu", [1, 512])
    q_norm_g = din("q_norm_g", [1, 64])
    k_norm_g = din("k_norm_g", [1, 64])
    w_proj_ssm = din("w_proj_ssm", [512, D])
    w_proj_attn = din("w_proj_attn", [512, D])
    w_out = din("w_out", [D, D])
    norm2_g = din("norm2_g", [1, D])
    w_router = din("w_router", [D, 36])
    b_router = din("b_router", [1, 36])
    w_gate = din("w_gate", [32, D, 128])
    w_up = din("w_up", [32, D, 128])
    w_down = din("w_down", [32, 128, D])
    ident_in = din("ident_in", [128, 128])
    cs_tab_in = din("cs_tab", [128, 64 * 16])
    pbrep_in = din("pbrep", [128, 16 * 32])
    onehot_in = din("onehot_k", [32, 8192])
    tri_in = din("tri", [128, 128])
    rconst_in = din("rconst", [128, 32 + 96 + 1])
    y_out = nc.dram_tensor("y_out", [S_OWN, D], F32, kind="ExternalOutput").ap()

    kT_scr = dscr("kT_scr", [8, 64, 8192], BF16)
    qT_scr = dscr("qT_scr", [8, 64, 4096], BF16)
    v_scr = dscr("v_scr", [8, 128, 64 * 65], BF16)
    z_scr = dscr("z_scr", [NST_OWN, 128, 8 * 512], BF16)
    att_scr = dscr("att_scr", [S_OWN, 512], BF16)
    wgu_scr = dscr("wgu_scr", [32, 128, 2, 8, 128], BF16)
    wd_scr = dscr("wd_scr", [32, 128, D], BF16)
    hs_scr = dscr("hs_scr", [96 * 128, D], BF16)
    h2_scr = dscr("h2_scr", [S_OWN, D], BF16)
    ys_scr = dscr("ys_scr", [96 * 128, D], F32)
    r_kT_scr = [R(f"kTs{h}") for h in range(8)]
    r_qT_scr = [R(f"qTs{h}") for h in range(8)]
    r_v_scr = [R(f"vs{h}") for h in range(8)]
    r_z_scr = [R(f"zs{s}") for s in range(NST_OWN)]
    r_att_scr = R("atts")
    _rw = [R(f"wscr{i}") for i in range(4)]
    r_wgu = [_rw[e % 4] for e in range(32)]
    r_wd = [_rw[e % 4] for e in range(32)]

    pd = [P.stack.enter_context(nc.psum_tensor(f"pd{i}", [128, 1024], F32)) for i in range(4)]
    pr = [[R(f"pd{i}a"), R(f"pd{i}b")] for i in range(4)]

    def pbank(i):
        return pd[i // 2][:, (i % 2) * 512:(i % 2) * 512 + 512], pr[i // 2][i % 2]

    def pbank_bf(i):
        return pd[i // 2][:, (i % 2) * 512:(i % 2) * 512 + 512].bitcast(BF16), pr[i // 2][i % 2]

    identf = T([128], F32); r_identf = R("identf")
    identb = T([128], BF16); r_identb = R("identb")
    P.dma("sp", identf, ident_in, writes=[r_identf])
    P.dma("pool", identb, ident_in, writes=[r_identb])
    g1rep = T([D], F32); r_g1 = R("g1rep")
    P.dma("sp", g1rep, norm1_g[0:1, :].partition_broadcast(128), writes=[r_g1])
    ksum = T([8, 64], F32); r_ksum = R("ksum")
    m_ssm = P.mark()
    gqk = T([2, 64], F32); r_gqk = R("gqk")
    P.dma("sp", gqk[:, 0, :], q_norm_g[0:1, :].partition_broadcast(128), writes=[r_gqk])
    P.dma("sp", gqk[:, 1, :], k_norm_g[0:1, :].partition_broadcast(128), writes=[r_gqk])
    cs_tab = T([64, 16], F32); r_cs = R("cs_tab")
    P.dma("sp", cs_tab, cs_tab_in.rearrange("p (t f) -> p t f", f=16), writes=[r_cs])
    drep = T([512], F32); r_drep = R("drep")
    P.dma("sp", drep, ssm_d[0:1, :].partition_broadcast(128), writes=[r_drep])


    W_e = T([32, 2, 64], BF16); r_We = R("W_e")
    Tz = T([32, 128], BF16); r_Tz = R("Tz")
    W_cr = T([16, 8, 16], BF16); r_Wc = R("W_c")
    W_ci = T([16, 8, 16], BF16)
    ER = T([16, 129], F32); r_E = R("E")
    EI = T([16, 129], F32)
    rho = T([16], F32); r_rho = R("rho")
    SR0 = T([16], F32); r_S0 = R("S0")
    SI0 = T([16], F32)
    b.memset("dve", SR0, 0.0, [r_S0])
    b.memset("dve", SI0, 0.0, [r_S0])

    m0 = P.mark()
    P.arena_off = P0_BASE
    P.recording = []
    r_p = R("prm")

    def hn_load(dst, src, extra=""):
        for gh in range(2):
            P.dma("sp", dst[64 * gh:64 * gh + 64], src[:, 16 * gh:16 * gh + 16], writes=[r_p])

    LR = T([16], F32); LI = T([16], F32); DT = T([16], F32)
    hn_load(LR, lamre_t); hn_load(LI, lamim_t)
    for gh in range(2):
        P.dma("sp", DT[64 * gh:64 * gh + 64], log_dt[0:1, 16 * gh:16 * gh + 16].partition_broadcast(64), writes=[r_p])
    BR = T([16, 16], F32); BI = T([16, 16], F32); CR = T([16, 16], F32); CI = T([16, 16], F32)
    hn_load(BR, b_re_t); hn_load(BI, b_im_t); hn_load(CR, c_re_t); hn_load(CI, c_im_t)
    rp = [r_p]
    ey = T([16], F32); ep = T([16], F32)

    def exp_acc(dst, src, extra_w=()):
        b.ts("dve", ey, src, 0.125, None, ALU.mult, None, rp, rp)
        b.ts("dve", ep, ey, 1.0 / 10, 1.0, ALU.mult, ALU.add, rp, rp)
        for n in range(9, 0, -1):
            b.tt("dve", ep, ep, ey, ALU.mult, rp, rp)
            b.ts("dve", ep, ep, 1.0 / n, 1.0, ALU.mult, ALU.add, rp, rp)
        b.tt("dve", ep, ep, ep, ALU.mult, rp, rp)
        b.tt("dve", ep, ep, ep, ALU.mult, rp, rp)
        b.tt("dve", dst, ep, ep, ALU.mult, rp, rp + list(extra_w))

    exp_acc(DT, DT)
    b.ts("dve", LR, LR, -1e-4, None, ALU.min, None, rp, rp)
    LRDT = T([16], F32); ANG = T([16], F32)
    b.tt("dve", LRDT, LR, DT, ALU.mult, rp, rp)
    b.tt("dve", ANG, LI, DT, ALU.mult, rp, rp)
    MAG = T([16], F32)
    exp_acc(MAG, LRDT)
    b.tt("dve", rho, MAG, MAG, ALU.mult, rp, [r_p, r_rho])
    b.tt("dve", rho, rho, rho, ALU.mult, [r_p, r_rho], [r_p, r_rho])
    b.tt("dve", rho, rho, rho, ALU.mult, [r_p, r_rho], [r_p, r_rho])
    SN = T([16], F32); CS = T([16], F32)
    tq = T([16], F32); ki = T([16], I32); kf = T([16], F32); rr = T([16], F32)
    for shift, outt in ((0.0, SN), (math.pi / 2, CS)):
        b.ts("dve", tq, ANG, 1.0 / TWO_PI, shift / TWO_PI, ALU.mult, ALU.add, rp, rp)
        b.cp("dve", ki, tq, rp, rp)
        b.cp("dve", kf, ki, rp, rp)
        b.stt("dve", rr, kf, -6.28125, ANG, ALU.mult, ALU.add, rp, rp)
        b.stt("dve", rr, kf, -(TWO_PI - 6.28125), rr, ALU.mult, ALU.add, rp, rp)
        b.ts("dve", rr, rr, shift, math.pi, ALU.add, ALU.min, rp, rp)
        b.ts("dve", rr, rr, -math.pi, None, ALU.max, None, rp, rp)
        b.act(outt, rr, AF.Sin, rp, rp)
    PR = T([9, 16], F32); PI = T([9, 16], F32)
    b.memset("dve", PR[:, 0, :], 1.0, rp)
    b.memset("dve", PI[:, 0, :], 0.0, rp)
    b.tt("dve", PR[:, 1, :], MAG, CS, ALU.mult, rp, rp)
    b.tt("dve", PI[:, 1, :], MAG, SN, ALU.mult, rp, rp)
    t1 = T([4, 16], F32); t2 = T([4, 16], F32)

    def cmul_pow(dst0, n, src0, m):
        a_r = PR[:, src0:src0 + n, :]; a_i = PI[:, src0:src0 + n, :]
        b_r = bc(PR[:, m:m + 1, :], [128, n, 16]); b_i = bc(PI[:, m:m + 1, :], [128, n, 16])
        b.tt("dve", t1[:, 0:n, :], a_r, b_r, ALU.mult, rp, rp)
        b.tt("dve", t2[:, 0:n, :], a_i, b_i, ALU.mult, rp, rp)
        b.tt("dve", PR[:, dst0:dst0 + n, :], t1[:, 0:n, :], t2[:, 0:n, :], ALU.subtract, rp, rp)
        b.tt("dve", t1[:, 0:n, :], a_r, b_i, ALU.mult, rp, rp)
        b.tt("dve", t2[:, 0:n, :], a_i, b_r, ALU.mult, rp, rp)
        b.tt("dve", PI[:, dst0:dst0 + n, :], t1[:, 0:n, :], t2[:, 0:n, :], ALU.add, rp, rp)

    cmul_pow(2, 1, 1, 1)
    cmul_pow(3, 2, 1, 2)
    cmul_pow(5, 4, 1, 4)
    DEN = T([16], F32); NR = T([16], F32); CRc = T([16], F32); CIc = T([16], F32); tmp = T([16], F32)
    b.tt("dve", DEN, LR, LR, ALU.mult, rp, rp)
    b.tt("dve", tmp, LI, LI, ALU.mult, rp, rp)
    b.tt("dve", DEN, DEN, tmp, ALU.add, rp, rp)
    b.recip(DEN, DEN, rp, rp)
    b.ts("dve", NR, PR[:, 1, :], -1.0, None, ALU.add, None, rp, rp)
    AIc = PI[:, 1, :]
    b.tt("dve", CRc, NR, LR, ALU.mult, rp, rp)
    b.tt("dve", tmp, AIc, LI, ALU.mult, rp, rp)
    b.tt("dve", CRc, CRc, tmp, ALU.add, rp, rp)
    b.tt("dve", CRc, CRc, DEN, ALU.mult, rp, rp)
    b.tt("dve", CIc, AIc, LR, ALU.mult, rp, rp)
    b.tt("dve", tmp, NR, LI, ALU.mult, rp, rp)
    b.tt("dve", CIc, CIc, tmp, ALU.subtract, rp, rp)
    b.tt("dve", CIc, CIc, DEN, ALU.mult, rp, rp)
    BBR = T([16, 16], F32); BBI = T([16, 16], F32); tb1 = T([16, 16], F32); tb2 = T([16, 16], F32)
    crb = bc(CRc.unsqueeze(2), [128, 16, 16]); cib = bc(CIc.unsqueeze(2), [128, 16, 16])
    b.tt("dve", tb1, BR, crb, ALU.mult, rp, rp)
    b.tt("dve", tb2, BI, cib, ALU.mult, rp, rp)
    b.tt("dve", BBR, tb1, tb2, ALU.subtract, rp, rp)
    b.tt("dve", tb1, BI, crb, ALU.mult, rp, rp)
    b.tt("dve", tb2, BR, cib, ALU.mult, rp, rp)
    b.tt("dve", BBI, tb1, tb2, ALU.add, rp, rp)
    mpad_off = P.mark()
    Mpad = T([16, 2, 240], F32)
    b.memset("pool", Mpad, 0.0, rp)
    for s in range(8):
        tau = 7 - s
        prb = bc(PR[:, tau, :].unsqueeze(2), [128, 16, 16]); pib = bc(PI[:, tau, :].unsqueeze(2), [128, 16, 16])
        b.tt("dve", tb1, BBR, prb, ALU.mult, rp, rp)
        b.tt("dve", tb2, BBI, pib, ALU.mult, rp, rp)
        b.tt("dve", Mpad[:, :, 0, 16 * s:16 * s + 16], tb1, tb2, ALU.subtract, rp, rp)
        b.tt("dve", tb1, BBI, prb, ALU.mult, rp, rp)
        b.tt("dve", tb2, BBR, pib, ALU.mult, rp, rp)
        b.tt("dve", Mpad[:, :, 1, 16 * s:16 * s + 16], tb1, tb2, ALU.add, rp, rp)
    NCI = T([16, 16], F32)
    b.ts("dve", NCI, CI, -1.0, None, ALU.mult, None, rp, rp)
    for g0 in range(0, 32, 4):
        pv, prs = pbank(6 + g0 // 4 % 2)
        for gg in range(4):
            g = g0 + gg
            gh, gl = g // 16, g % 16
            for ri in range(2):
                b.tr(pv[:, (gg * 2 + ri) * 64:(gg * 2 + ri) * 64 + 64], Mpad[64 * gh:64 * gh + 64, gl, ri, 0:128],
                     identf[64 * gh:64 * gh + 64, 64 * gh:64 * gh + 64], [r_p, r_identf], [prs])
        b.cp("act", W_e[:, g0:g0 + 4, :, :], pv.rearrange("p (g r n) -> p g r n", g=4, r=2), [prs], [r_We])
    MpadB = T([16, 2, 240], BF16); CRb = T([16, 16], BF16); NCIb = T([16, 16], BF16)
    b.cp("pool", MpadB, Mpad, rp, rp)
    b.cp("pool", CRb, CR, rp, rp)
    b.cp("pool", NCIb, NCI, rp, rp)
    for g0 in range(0, 32, 4):
        pv, prs = pbank(6 + g0 // 4 % 2)
        for gg in range(4):
            g = g0 + gg
            gh, gl = g // 16, g % 16
            sl = slice(64 * gh, 64 * gh + 64)
            for i in range(8):
                o = pv[:, gg * 128 + i * 16:gg * 128 + i * 16 + 16]
                b.mm(o, MpadB[sl, gl, 0, 16 * (7 - i):16 * (7 - i) + 128], CRb[sl, gl, :], True, False, [r_p], [prs])
                b.mm(o, MpadB[sl, gl, 1, 16 * (7 - i):16 * (7 - i) + 128], NCIb[sl, gl, :], False, True, [r_p], [prs])
        b.cp("act", Tz[:, g0:g0 + 4, :], pv.rearrange("p (g x) -> p g x", g=4), [prs], [r_Tz])
    _cur = P.mark()
    P.arena_off = mpad_off
    tc1 = T([16, 8, 16], F32); tc2 = T([16, 8, 16], F32)
    crB = bc(CR.unsqueeze(2), [128, 16, 8, 16]); ciB = bc(CI.unsqueeze(2), [128, 16, 8, 16]); nciB = bc(NCI.unsqueeze(2), [128, 16, 8, 16])
    PRi = bc(PR[:, 1:9, :].rearrange("p i g -> p g i").unsqueeze(3), [128, 16, 8, 16])
    PIi = bc(PI[:, 1:9, :].rearrange("p i g -> p g i").unsqueeze(3), [128, 16, 8, 16])
    b.tt("dve", tc1, crB, PRi, ALU.mult, rp, rp)
    b.tt("dve", tc2, ciB, PIi, ALU.mult, rp, rp)
    b.tt("dve", W_cr, tc1, tc2, ALU.subtract, rp, [r_p, r_Wc])
    b.tt("dve", tc1, nciB, PRi, ALU.mult, rp, rp)
    b.tt("dve", tc2, crB, PIi, ALU.mult, rp, rp)
    b.tt("dve", W_ci, tc1, tc2, ALU.subtract, rp, [r_p, r_Wc])
    RINV = T([16], F32)
    b.recip(RINV, rho, [r_rho], rp)
    rE = [r_p, r_E]
    b.memset("dve", ER[:, :, 0:1], 1.0, rE)
    b.memset("dve", EI[:, :, 0:1], 0.0, rE)
    b.tt("dve", ER[:, :, 1], PR[:, 8, :], RINV, ALU.mult, rp, rE)
    b.tt("dve", EI[:, :, 1], PI[:, 8, :], RINV, ALU.mult, rp, rE)
    te1 = T([16, 64], F32); te2 = T([16, 64], F32)
    assert P.arena_off <= mpad_off + 16 * 2 * 240 * 2
    P.arena_off = _cur
    m = 1
    while m < 128:
        n = m
        a_r = ER[:, :, 1:1 + n]; a_i = EI[:, :, 1:1 + n]
        b_r = bc(ER[:, :, m:m + 1], [128, 16, n]); b_i = bc(EI[:, :, m:m + 1], [128, 16, n])
        b.tt("dve", te1[:, :, 0:n], a_r, b_r, ALU.mult, rE, rp)
        b.tt("pool", te2[:, :, 0:n], a_i, b_i, ALU.mult, rE, rp)
        b.tt("dve", ER[:, :, m + 1:m + 1 + n], te1[:, :, 0:n], te2[:, :, 0:n], ALU.subtract, rp, rE)
        b.tt("dve", te1[:, :, 0:n], a_r, b_i, ALU.mult, rE, rp)
        b.tt("pool", te2[:, :, 0:n], a_i, b_r, ALU.mult, rE, rp)
        b.tt("dve", EI[:, :, m + 1:m + 1 + n], te1[:, :, 0:n], te2[:, :, 0:n], ALU.add, rp, rE)
        m *= 2
    dump("W_e", W_e, [128, 32, 2, 64], [r_We], BF16)
    dump("Tz", Tz, [128, 32, 128], [r_Tz], BF16)
    dump("W_cr", W_cr, [128, 16, 8, 16], [r_Wc], BF16)
    dump("W_ci", W_ci, [128, 16, 8, 16], [r_Wc], BF16)
    dump("ER", ER, [128, 16, 129], [r_E])
    dump("EI", EI, [128, 16, 129], [r_E])
    dump("rho", rho, [128, 16], [r_rho])
    dump("PR", PR, [128, 9, 16], rp)
    dump("PI", PI, [128, 9, 16], rp)
    print("phase 0 temporaries end", P.arena_peak)
    p0_ops = P.recording
    P.recording = None
    P.release(m0)
    ctx = dict(nc=nc, P=P, b=b, T=T, R=R, dump=dump, dbg_out=dbg_out, pbank=pbank, pbank_bf=pbank_bf, pd=pd, pr=pr)
    ctx.update(locals())
    if stop_after == "0":
        return finish(ctx)
    phase_A(ctx)
    if stop_after == "A":
        return finish(ctx)
    phase_B(ctx)
    if stop_after == "B":
        return finish(ctx)
    phase_CD(ctx)
    return finish(ctx)


def finish(ctx):
    P = ctx["P"]
    P.finalize()
    return ctx["nc"], ctx["dbg_out"]


def run_interleaved(*gens, weights=None):
    gens = list(gens)
    w = {id(g): (weights[i] if weights else 1) for i, g in enumerate(gens)}
    while gens:
        for g in list(gens):
            for _ in range(w[id(g)]):
                try:
                    next(g)
                except StopIteration:
                    gens.remove(g)
                    break


def phase_A(c):
    nc, P, b, T, R, dump = c["nc"], c["P"], c["b"], c["T"], c["R"], c["dump"]
    pbank, pbank_bf = c["pbank"], c["pbank_bf"]
    identb, r_identb, g1rep, r_g1, gqk, r_gqk, cs_tab, r_cs = (c[k] for k in
        ("identb", "r_identb", "g1rep", "r_g1", "gqk", "r_gqk", "cs_tab", "r_cs"))
    W_e, r_We, Tz, r_Tz, W_cr, W_ci, r_Wc, ER, EI, r_E, rho, r_rho, SR0, SI0, r_S0 = (c[k] for k in
        ("W_e", "r_We", "Tz", "r_Tz", "W_cr", "W_ci", "r_Wc", "ER", "EI", "r_E", "rho", "r_rho", "SR0", "SI0", "r_S0"))
    drep, r_drep = c["drep"], c["r_drep"]
    x_all, w_in = c["x_all"], c["w_in"]
    kT_scr, qT_scr, v_scr, z_scr = c["kT_scr"], c["qT_scr"], c["v_scr"], c["z_scr"]
    r_kT_scr, r_qT_scr, r_v_scr, r_z_scr = c["r_kT_scr"], c["r_qT_scr"], c["r_v_scr"], c["r_z_scr"]
    pd, pr = c["pd"], c["pr"]
    ksum, r_ksum = c["ksum"], c["r_ksum"]
    mA = P.mark()
    c["mA"] = mA

    WinA = T([8, 2048], BF16); r_WinA = R("WinA")
    for kc in range(8):
        P.dma("pool", WinA[:, kc, :], w_in[kc * 128:(kc + 1) * 128, 0:2048], writes=[r_WinA])

    xt = [T([D], F32) for _ in range(2)]; r_xt = [R("xt0"), R("xt1")]
    hb = [T([D], BF16) for _ in range(2)]; r_hb = [R("hb0"), R("hb1")]
    ss = T([16], F32); rs = T([16], F32); r_ss = [R(f"ss{i}") for i in range(16)]
    hT = T([8, 1024], BF16); r_hT = [R(f"hT{i}") for i in range(8)]
    pad = [None, [T([8, 64], BF16) for _ in range(2)]]
    r_pad = [[R(f"pad{w}{i}") for i in range(2)] for w in range(2)]
    ktt = [None, [T([8, 128], BF16) for _ in range(2)]]
    r_ktt = [[R(f"ktt{w}{i}") for i in range(2)] for w in range(2)]
    v_st = [T([8, 8, 65], BF16) for _ in range(2)]; r_vst = [R("v_st0"), R("v_st1")]
    for i in range(2):
        b.memset("pool", v_st[i], 1.0, [r_vst[i]])
    sqt = [T([8, 64], F32) for _ in range(2)]; r_sqt = [R("sqt0"), R("sqt1")]
    qn = [T([8, 64], F32) for _ in range(2)]; r_qn = [R("qn0"), R("qn1")]
    ssq = [T([8], F32) for _ in range(2)]; rq = [T([8], F32) for _ in range(2)]
    ra = [T([8, 8], F32) for _ in range(2)]; rb = [T([8, 8], F32) for _ in range(2)]
    assert P.arena_off <= P0_BASE, P.arena_off
    P.arena_off = P0_BASE
    pad[0] = [T([8, 64], BF16) for _ in range(2)]
    ktt[0] = [T([8, 128], BF16) for _ in range(2)]
    Ukj = T([32, 8, 16], BF16); r_Ukj = R("Ukj")
    U8 = T([32, 128], BF16); r_U8 = R("U8")
    NQ = 4
    SRf = T([NQ, 129], F32); SIf = T([NQ, 129], F32); r_Sf = R("Sf")
    zt_r = SRf[:, :, 0:128]; zt_i = SIf[:, :, 0:128]; r_zt = r_Sf
    srm = [T([NQ, 129], F32) for _ in range(2)]; r_srm = [R("srm0"), R("srm1")]
    ztm = [srm[0][:, :, 0:128], srm[1][:, :, 0:128]]; r_ztm = r_srm
    ZR = T([NQ, 129], F32); ZI = T([NQ, 129], F32); r_Z = R("Z")
    S_re = T([NQ, 128], BF16); S_im = T([NQ, 128], BF16); r_S = R("Sbf")
    du = [T([4, 128], F32) for _ in range(2)]; r_du = [R("du0"), R("du1")]
    ys = [T([4, 128], F32) for _ in range(2)]; y2 = [T([4, 128], F32) for _ in range(2)]
    r_ys = [R("ys0"), R("ys1")]; r_y2 = [R("y20"), R("y21")]
    Zst = T([8, 32, 16], BF16); r_Zst = R("Zst")
    print("phase A arena", P.arena_off)

    cnt = {"qkv": 0, "qk": 0}
    outs = {}

    def load_x(s, tt, par):
        row0 = s * 1024 + tt * 128
        P.dma("sp", xt[par], x_all[row0:row0 + 128, :], writes=[r_xt[par]])

    busy = {}

    def qkv_mm(tt, col0):
        i = 1 + cnt["qkv"] % 4
        cnt["qkv"] += 1
        assert not busy.get(i, False)
        psv, prs = pbank(i)
        for kc in range(8):
            b.mm(psv, hT[:, kc, tt * 128:(tt + 1) * 128], WinA[:, kc, col0:col0 + 512], kc == 0, kc == 7,
                 [r_hT[tt], r_WinA], [prs])
        return psv, prs

    def qkv_mm_nb(tt, col0):
        i = 1 + cnt["qkv"] % 4
        cnt["qkv"] += 1
        psv, prs = pbank(i)
        for kc in range(8):
            b.mm(psv, hT[:, kc, tt * 128:(tt + 1) * 128], WinA[:, kc, col0:col0 + 512], kc == 0, kc == 7,
                 [r_hT[tt], r_WinA], [prs])
        return psv, prs

    BACK_LAG = 3
    fstate = {"tick": 0, "done": -1}

    def gen_front(s):
        for _ in gen_front_(s):
            fstate["tick"] += 1
            yield
        fstate["done"] = s

    def gen_front_(s):
        own = is_own(s)
        for tt in range(8):
            par = tt % 2
            if tt < 7:
                load_x(s, tt + 1, 1 - par)
            elif s + 1 < NST:
                load_x(s + 1, 0, 1 - par)
            si = (s % 2) * 8 + tt
            b.act(hb[par], xt[par], AF.Square, [r_xt[par]], [r_hb[par], r_ss[si]], accum=ss[:, si:si + 1])
            b.act(rs[:, si:si + 1], ss[:, si:si + 1], AF.Sqrt, [r_ss[si]], [r_ss[si]], bias=EPS, scale=1.0 / D)
            b.recip(rs[:, si:si + 1], rs[:, si:si + 1], [r_ss[si]], [r_ss[si]])
            b.stt("dve", hb[par], xt[par], rs[:, si:si + 1], g1rep, ALU.mult, ALU.mult,
                  [r_xt[par], r_ss[si], r_g1], [r_hb[par]])
            yield
            ptv, ptr_ = pbank_bf(0)
            for kc in range(8):
                b.tr(ptv[:, kc * 128:(kc + 1) * 128], hb[par][:, kc * 128:(kc + 1) * 128], identb,
                     [r_hb[par], r_identb], [ptr_])
            b.cp("act", hT[:, :, tt * 128:(tt + 1) * 128], ptv.rearrange("p (k t) -> p k t", t=128),
                 [ptr_], [r_hT[tt]])
            yield
            while busy.get(1 + cnt["qkv"] % 4, False):
                yield
            busy[1 + cnt["qkv"] % 4] = True
            outs[(s, tt, 1)] = (1 + cnt["qkv"] % 4,) + tuple(qkv_mm_nb(tt, 1024)) + (fstate["tick"],)
            yield
            while busy.get(1 + cnt["qkv"] % 4, False):
                yield
            psv, prs = qkv_mm(tt, 1536)
            b.cp("act", v_st[s % 2][:, :, tt, 0:64], psv.rearrange("p (h d) -> p h d", d=64), [prs], [r_vst[s % 2]])
            yield
            if own:
                while busy.get(1 + cnt["qkv"] % 4, False):
                    yield
                busy[1 + cnt["qkv"] % 4] = True
                outs[(s, tt, 0)] = (1 + cnt["qkv"] % 4,) + tuple(qkv_mm_nb(tt, 512)) + (fstate["tick"],)
                yield
        P.dma("sp", v_scr[:, :, s * 8 * 65:(s + 1) * 8 * 65].rearrange("h p x -> p h x"),
              v_st[s % 2].rearrange("p h t d -> p h (t d)"), reads=[r_vst[s % 2]], writes=r_v_scr)

    def gen_back(s, which):
        own = is_own(s)
        for tt in range(8):
            ti = s * 8 + tt
            if True:
                while (s, tt, which) not in outs:
                    yield
                while fstate["done"] < s and fstate["tick"] < outs[(s, tt, which)][3] + BACK_LAG:
                    yield
                bank_i, psv, prs, _tk = outs.pop((s, tt, which))
                k_ = which if own else tt % 2
                ps3 = psv.rearrange("p (h d) -> p h d", d=64)
                b.act(sqt[k_], ps3, AF.Square, [prs], [r_sqt[k_]])
                b.reduce(ssq[k_], sqt[k_], ALU.add, [r_sqt[k_]], [r_sqt[k_]])
                b.act(rq[k_], ssq[k_], AF.Sqrt, [r_sqt[k_]], [r_sqt[k_]], bias=EPS, scale=1.0 / 64)
                b.recip(rq[k_], rq[k_], [r_sqt[k_]], [r_sqt[k_]])
                b.tt("dve", qn[k_], ps3, bc(rq[k_].unsqueeze(2), [128, 8, 64]), ALU.mult, [prs, r_sqt[k_]], [r_qn[k_]])
                busy[bank_i] = False
                yield
                b.tt("pool", qn[k_], qn[k_], bc(gqk[:, which, :].unsqueeze(1), [128, 8, 64]), ALU.mult,
                     [r_qn[k_], r_gqk], [r_qn[k_]])
                cosb = bc(cs_tab[:, ti, 0:8].unsqueeze(1), [128, 8, 8])
                sinb = bc(cs_tab[:, ti, 8:16].unsqueeze(1), [128, 8, 8])
                x1 = qn[k_][:, :, 0:8]; x2 = qn[k_][:, :, 8:16]
                pd_ = pad[which][k_]; rpd = r_pad[which][k_]
                rd = [r_qn[k_], r_cs]
                b.tt("pool", ra[k_], x1, cosb, ALU.mult, rd, [r_qn[k_]])
                b.tt("pool", rb[k_], x2, sinb, ALU.mult, rd, [r_qn[k_]])
                b.tt("pool", pd_[:, :, 0:8], ra[k_], rb[k_], ALU.subtract, [r_qn[k_]], [rpd])
                yield
                b.tt("pool", ra[k_], x2, cosb, ALU.mult, rd, [r_qn[k_]])
                b.tt("pool", rb[k_], x1, sinb, ALU.mult, rd, [r_qn[k_]])
                b.tt("pool", pd_[:, :, 8:16], ra[k_], rb[k_], ALU.add, [r_qn[k_]], [rpd])
                b.cp("act", pd_[:, :, 16:64], qn[k_][:, :, 16:64], [r_qn[k_]], [rpd])
                yield
                ptv, ptr_ = pbank_bf(5)
                for h in range(8):
                    b.tr(ptv[0:64, h * 128:(h + 1) * 128], pd_[:, h, :], identb, [rpd, r_identb], [ptr_])
                kt = ktt[which][k_]; rkt = r_ktt[which][k_]
                b.cp("dve", kt[0:64], ptv[0:64].rearrange("p (h t) -> p h t", t=128), [ptr_], [rkt])
                yield
                if which == 1:
                    b.reduce(ksum[0:64, :, ti], kt[0:64], ALU.add, [rkt], [r_ksum])
                    P.dma("sp", kT_scr[:, :, ti * 128:(ti + 1) * 128].rearrange("h d t -> d h t"), kt[0:64],
                          reads=[rkt], writes=r_kT_scr)
                else:
                    oti = (s // 2) * 8 + tt
                    P.dma("sp", qT_scr[:, :, oti * 128:(oti + 1) * 128].rearrange("h d t -> d h t"), kt[0:64],
                          reads=[rkt], writes=r_qT_scr)
                yield

    def gen_ssm_u(s):
        hT8 = hT.rearrange("p k (c j) -> p k j c", j=8)
        for j in range(8):
            psv, prs = pbank(6 + j % 2)
            for kc in range(8):
                b.mm(psv, hT8[:, kc, j, :], WinA[:, kc, 0:512], kc == 0, kc == 7, r_hT + [r_WinA], [prs])
            b.cp("act" if j % 2 else "dve", Ukj[:, :, j, :], psv.rearrange("p (g c) -> p g c", c=16), [prs], [r_Ukj])
            yield
        if "Ukj" in c["debug"] and s == 1:
            dump("Ukj", Ukj, [128, 32, 8, 16], [r_Ukj], BF16)

    def gen_ssm(s):
        own = is_own(s)
        for g0 in range(0, 32, 8):
            ptv, ptr_ = pbank_bf(6 + (g0 // 8) % 2)
            for gg in range(8):
                b.tr(ptv[:, gg * 128:(gg + 1) * 128], Ukj[:, g0 + gg, :, :].rearrange("p j c -> p (j c)"), identb,
                     [r_Ukj, r_identb], [ptr_])
            b.cp("dve", U8[:, g0:g0 + 8, :], ptv.rearrange("p (g k) -> p g k", k=128), [ptr_], [r_U8])
            yield
        for glq in range(16 // NQ):
            e_re, pre = pbank(6)
            e_im, pim = pbank(7)
            e_re = e_re.rearrange("p (g k) -> p g k", k=128)
            e_im = e_im.rearrange("p (g k) -> p g k", k=128)
            for gi in range(NQ):
                for gh in range(2):
                    g = gh * 16 + glq * NQ + gi
                    sl = slice(64 * gh, 64 * gh + 64)
                    b.mm(e_re[sl, gi, :], W_e[:, g, 0, :], U8[:, g, :], True, True, [r_We, r_U8], [pre])
                    b.mm(e_im[sl, gi, :], W_e[:, g, 1, :], U8[:, g, :], True, True, [r_We, r_U8], [pim])
            yield
            gsl = slice(glq * NQ, glq * NQ + NQ)
            E1r = ER[:, gsl, 1:129]; E1i = EI[:, gsl, 1:129]
            b.tt("dve", ztm[0], e_re, E1r, ALU.mult, [pre, r_E], [r_ztm[0]])
            b.tt("dve", ztm[1], e_im, E1i, ALU.mult, [pim, r_E], [r_ztm[1]])
            b.tt("pool", zt_r, ztm[0], ztm[1], ALU.add, r_ztm, [r_zt])
            yield
            b.tt("dve", ztm[0], e_im, E1r, ALU.mult, [pim, r_E], [r_ztm[0]])
            b.tt("dve", ztm[1], e_re, E1i, ALU.mult, [pre, r_E], [r_ztm[1]])
            b.tt("pool", zt_i, ztm[0], ztm[1], ALU.subtract, r_ztm, [r_zt])
            b.cp("pool", ZR[:, :, 0], SR0[:, gsl], [r_S0], [r_Z])
            b.cp("pool", ZI[:, :, 0], SI0[:, gsl], [r_S0], [r_Z])
            yield
            for gi in range(NQ):
                gl = glq * NQ + gi
                for (Zx, S0x, ztx) in ((ZR, SR0, zt_r), (ZI, SI0, zt_i)):
                    P.op("dve", (lambda Zx=Zx, S0x=S0x, ztx=ztx, gi=gi, gl=gl: (lambda e: e.tensor_tensor_scan(
                        out=Zx[:, gi, 1:129], data0=bc(rho[:, gl:gl + 1], [128, 128]), data1=ztx[:, gi, :],
                        initial=S0x[:, gl:gl + 1], op0=ALU.mult, op1=ALU.add)))(),
                        [r_zt, r_rho, r_S0], [r_Z])
                yield
            E0r = ER[:, gsl, :]; E0i = EI[:, gsl, :]
            b.tt("dve", srm[0], ZR, E0r, ALU.mult, [r_Z, r_E], [r_srm[0]])
            b.tt("pool", srm[1], ZI, E0i, ALU.mult, [r_Z, r_E], [r_srm[1]])
            b.tt("dve", SRf, srm[0], srm[1], ALU.subtract, r_srm, [r_Sf])
            yield
            b.tt("dve", srm[0], ZR, E0i, ALU.mult, [r_Z, r_E], [r_srm[0]])
            b.tt("pool", srm[1], ZI, E0r, ALU.mult, [r_Z, r_E], [r_srm[1]])
            b.tt("dve", SIf, srm[0], srm[1], ALU.add, r_srm, [r_Sf])
            b.cp("act", SR0[:, gsl], SRf[:, :, 128], [r_Sf], [r_S0])
            b.cp("act", SI0[:, gsl], SIf[:, :, 128], [r_Sf], [r_S0])
            yield
            if not own:
                continue
            b.cp("act", S_re, SRf[:, :, 0:128], [r_Sf], [r_S])
            b.cp("act", S_im, SIf[:, :, 0:128], [r_Sf], [r_S])
            for gh in range(2):
                psv, prs = pbank(6 + gh)
                sl = slice(64 * gh, 64 * gh + 64)
                gbase = gh * 16 + glq * NQ
                k_ = gh
                for gi in range(NQ):
                    g = gbase + gi
                    o = psv[:, gi * 128:(gi + 1) * 128]
                    b.mm(o, U8[:, g, :], Tz[:, g, :], True, False, [r_U8, r_Tz], [prs])
                    b.mm(o, S_re[sl, gi, :], W_cr[sl, glq * NQ + gi, :, :].rearrange("p i c -> p (i c)"),
                         False, False, [r_S, r_Wc], [prs])
                    b.mm(o, S_im[sl, gi, :], W_ci[sl, glq * NQ + gi, :, :].rearrange("p i c -> p (i c)"),
                         False, True, [r_S, r_Wc], [prs])
                b.tt("pool", du[k_].rearrange("p g (j c) -> p g j c", c=16), Ukj[:, gbase:gbase + 4, :, :],
                     bc(drep[:, gbase * 16:gbase * 16 + 64].rearrange("p (g c) -> p g c", c=16).unsqueeze(2),
                        [128, 4, 8, 16]), ALU.mult, [r_Ukj, r_drep], [r_du[k_]])
                yield
                b.tt("dve", ys[k_], psv.rearrange("p (g x) -> p g x", x=128), du[k_], ALU.add, [prs, r_du[k_]], [r_ys[k_]])
                b.tt("pool", y2[k_], ys[k_], ys[k_], ALU.mult, [r_ys[k_]], [r_y2[k_]])
                b.ts("pool", y2[k_], y2[k_], 0.044715, 1.0, ALU.mult, ALU.add, [r_y2[k_]], [r_y2[k_]])
                b.tt("pool", y2[k_], y2[k_], ys[k_], ALU.mult, [r_y2[k_], r_ys[k_]], [r_y2[k_]])
                yield
                b.act(y2[k_], y2[k_], AF.Sigmoid, [r_y2[k_]], [r_y2[k_]], scale=1.5957691216057308)
                b.tt("dve", Zst[:, :, gbase:gbase + 4, :].rearrange("p i g c -> p g i c"),
                     ys[k_].rearrange("p g (i c) -> p g i c", c=16), y2[k_].rearrange("p g (i c) -> p g i c", c=16),
                     ALU.mult, [r_ys[k_], r_y2[k_]], [r_Zst])
                yield
        if own:
            so = s // 2
            P.dma("sp", z_scr[so], Zst.rearrange("p i g c -> p (i g c)"), reads=[r_Zst], writes=[r_z_scr[so]])
            if "Zst" in c["debug"] and so == 0:
                dump("Zst", Zst, [128, 8, 32, 16], [r_Zst], BF16)

    def gen_p0():
        for k, th in enumerate(c["p0_ops"]):
            th()
            if k % 4 == 3:
                yield

    load_x(0, 0, 0)
    for s in range(NST):
        gens = [gen_front(s), gen_back(s, 1)]
        wts = [1, 1]
        if is_own(s):
            gens.append(gen_back(s, 0))
            wts.append(1)
        if s > 0:
            gens.append(gen_ssm(s - 1))
            wts.append(2 if is_own(s - 1) else 1)
        else:
            gens.append(gen_p0())
            wts.append(3)
        run_interleaved(*gens, weights=wts)
        if s == 0:
            P.barrier()
        run_interleaved(gen_ssm_u(s))
    run_interleaved(gen_ssm(NST - 1))
    dump("kT", kT_scr, [8, 64, 8192], r_kT_scr, BF16)
    dump("qT", qT_scr, [8, 64, 4096], r_qT_scr, BF16)
    dump("v", v_scr, [8, 128, 64 * 65], r_v_scr, BF16)
    dump("z", z_scr, [NST_OWN, 128, 4096], r_z_scr, BF16)
    dump("ksum", ksum, [128, 8, 64], [r_ksum])
    P.barrier()
    P.release(mA)


def prefetch_C(c):
    P, b, T, R = c["P"], c["b"], c["T"], c["R"]
    w_in, hs_scr = c["w_in"], c["hs_scr"]
    cw = {}
    zrow = T([D], BF16); r_zrow = R("czrow")
    b.memset("dve", zrow, 0.0, [r_zrow])
    hz_stream = P.stream("hz")
    r_hz = []
    for j in range(96):
        rj = R(f"chz{j}")
        rj.stream = hz_stream
        r_hz.append(rj)
        P.dma("sp", hs_scr[j * 128:(j + 1) * 128, :], zrow, reads=[r_zrow], writes=[rj])
    WinC = T([8, 2048], BF16); r_WinC = R("WinC")
    for kc in range(8):
        P.dma("pool", WinC[:, kc, :], w_in[kc * 128:(kc + 1) * 128, 2048:4096], writes=[r_WinC])
    Wglu = T([4, 512], BF16); Wps = T([4, 1024], BF16); Wpa = T([4, 1024], BF16); Wo = T([8, 1024], BF16)
    Wr = T([8, 36], BF16); r_W = R("Wsmall")
    P.dma("pool", Wglu, c["w_glu"].rearrange("(kc p) n -> p kc n", p=128), writes=[r_W])
    P.dma("pool", Wps, c["w_proj_ssm"].rearrange("(kc p) n -> p kc n", p=128), writes=[r_W])
    P.dma("pool", Wpa, c["w_proj_attn"].rearrange("(kc p) n -> p kc n", p=128), writes=[r_W])
    P.dma("pool", Wo, c["w_out"].rearrange("(kc p) n -> p kc n", p=128), writes=[r_W])
    P.dma("pool", Wr, c["w_router"].rearrange("(kc p) n -> p kc n", p=128), writes=[r_W])
    brow = T([512 + 36], BF16)
    P.dma("pool", brow[0:1, 0:512], c["b_glu"], writes=[r_W])
    P.dma("pool", brow[0:1, 512:548], c["b_router"], writes=[r_W])
    g2rep = T([D], F32)
    P.dma("pool", g2rep, c["norm2_g"][0:1, :].partition_broadcast(128), writes=[r_W])
    for k in ("r_hz", "WinC", "r_WinC", "Wglu", "Wps", "Wpa", "Wo", "Wr", "r_W", "brow", "g2rep"):
        cw[k] = locals()[k]
    c["cw"] = cw
    print("prefetch_C arena", P.arena_off)


def phase_B(c):
    nc, P, b, T, R, dump = c["nc"], c["P"], c["b"], c["T"], c["R"], c["dump"]
    pbank, pbank_bf = c["pbank"], c["pbank_bf"]
    identb, r_identb = c["identb"], c["r_identb"]
    ksum, r_ksum = c["ksum"], c["r_ksum"]
    kT_scr, qT_scr, v_scr, att_scr = c["kT_scr"], c["qT_scr"], c["v_scr"], c["att_scr"]
    r_kT_scr, r_qT_scr, r_v_scr, r_att_scr = c["r_kT_scr"], c["r_qT_scr"], c["r_v_scr"], c["r_att_scr"]
    P.release(c["m_ssm"])
    mB = P.mark()
    cvf = [T([1024], F32) for _ in range(3)]; r_cvf = [R(f"cvf{i}") for i in range(3)]
    cvb = [T([1024], BF16) for _ in range(3)]; r_cvb = [R(f"cvb{i}") for i in range(3)]
    r_wgu = c["r_wgu"]; r_wd = c["r_wd"]

    def conv_steps():
        jobs = []
        for e in range(32):
            jobs.append((c["w_gate"][e].rearrange("(kc p) f -> p kc f", p=128), c["wgu_scr"][e, :, 0], r_wgu[e], True))
            jobs.append((c["w_up"][e].rearrange("(kc p) f -> p kc f", p=128), c["wgu_scr"][e, :, 1], r_wgu[e], True))
            jobs.append((c["w_down"][e], c["wd_scr"][e], r_wd[e], False))
        n = len(jobs)
        for k in range(n + 2):
            if k < n:
                src, dst, rr_, three = jobs[k]
                o = cvf[k % 3].rearrange("p (kc f) -> p kc f", f=128) if three else cvf[k % 3]
                P.dma("sp", o, src, writes=[r_cvf[k % 3]])
            if 0 <= k - 1 < n:
                j = k - 1
                b.cp("pool", cvb[j % 3], cvf[j % 3], [r_cvf[j % 3]], [r_cvb[j % 3]])
            if 0 <= k - 2 < n:
                j = k - 2
                src, dst, rr_, three = jobs[j]
                i_ = cvb[j % 3].rearrange("p (kc f) -> p kc f", f=128) if three else cvb[j % 3]
                P.dma("sp", dst, i_, reads=[r_cvb[j % 3]], writes=[rr_])
            yield

    conv = conv_steps()

    kaug = [T([8192], BF16) for _ in range(2)]; r_kaug = [R("kaug0"), R("kaug1")]
    r_k1h = [R("k1h0"), R("k1h1")]
    qaug = [T([4096], BF16) for _ in range(2)]
    r_qd = [R("qd0"), R("qd1")]
    r_qb = [[R(f"qb{i}_{t}") for t in range(32)] for i in range(2)]
    vb = [T([64, 65], BF16) for _ in range(2)]; r_vb = [R("vb0"), R("vb1")]
    att_tok = T([32, 512], BF16); r_att = R("att_tok")
    kmeanT = T([8, 32], BF16); r_km = R("kmeanT")
    kmf = T([8, 32], F32)
    pbrep = T([16, 32], F32); pbm = T([16, 32], F32); r_pb = R("pb")
    tri = T([128], BF16); r_tri = R("tri")
    biaspad = [T([96], BF16) for _ in range(2)]; r_bp = [R("bp0"), R("bp1")]
    sm = [T([32], F32) for _ in range(2)]; mx8 = [T([8], F32) for _ in range(2)]; sel = [T([32], F32) for _ in range(2)]
    r_sm = [R("sm0"), R("sm1")]
    PTb = [T([512], BF16) for _ in range(4)]; r_PT = [R(f"PT{i}") for i in range(4)]
    rinv = [T([2], F32) for _ in range(2)]; r_rinv = [R("rinv0"), R("rinv1")]
    print("phase B arena", P.arena_off)
    c["cw_base"] = P.arena_off

    P.dma("sp", pbrep, c["pbrep_in"].rearrange("p (a b) -> p a b", b=32), writes=[r_pb])
    b.ts("dve", pbm, pbrep, NEG, None, ALU.add, None, [r_pb], [r_pb])
    P.dma("pool", tri, c["tri_in"], writes=[r_tri])
    for i in range(2):
        P.dma("pool", kaug[i][64:96, :], c["onehot_in"], writes=[r_k1h[i]])
        b.memset("dve", biaspad[i], 0.0, [r_bp[i]])
    b.reduce(kmf[0:64], ksum[0:64].rearrange("p h (n two) -> p h n two", two=2), ALU.add, [r_ksum], [r_km])
    b.ts("dve", kmeanT[0:64], kmf[0:64], 1.0 / 256, None, ALU.mult, None, [r_km], [r_km])

    def load_head(h):
        hb_ = h % 2
        P.dma("sp", kaug[hb_][0:64, :], kT_scr[h], reads=[r_kT_scr[h]], writes=[r_kaug[hb_]])
        P.dma("sp", qaug[hb_][0:64, :], qT_scr[h], reads=[r_qT_scr[h]], writes=[r_qd[hb_]])
        P.dma("sp", vb[hb_], v_scr[h].rearrange("p (t d) -> p t d", d=65), reads=[r_v_scr[h]], writes=[r_vb[hb_]])

    rt_cnt = {"n": 0}

    def route_stage1(h, t):
        hb_ = h % 2
        k_ = rt_cnt["n"] % 2
        rt_cnt["n"] += 1
        ob = t // 2
        psv, prs = pbank(0)
        sc = psv[:, (t % 8) * 32:(t % 8) * 32 + 32]
        b.mm(sc, qaug[hb_][0:64, t * 128:(t + 1) * 128], kmeanT[0:64, h, :], True, True, [r_qd[hb_], r_km], [prs])
        b.tt("dve", sm[k_], sc, pbrep[:, ob, :], ALU.add, [prs, r_pb], [r_sm[k_]])
        P.op("dve", lambda e: e.max(out=mx8[k_], in_=sm[k_]), [r_sm[k_]], [r_sm[k_]])
        b.ts("dve", sel[k_], sm[k_], mx8[k_][:, 2:3], None, ALU.is_ge, None, [r_sm[k_]], [r_sm[k_]])
        b.stt("dve", biaspad[k_][:, 64:96], sel[k_], -NEG, pbm[:, ob, :], ALU.mult, ALU.add, [r_sm[k_], r_pb], [r_bp[k_]])
        return k_

    def route_stage2(h, t, k_):
        hb_ = h % 2
        ptv, ptr_ = pbank_bf(0)
        o = ptv[0:96, 512 + (t % 4) * 128:512 + (t % 4) * 128 + 128]
        b.tr(o, biaspad[k_], identb, [r_bp[k_], r_identb], [ptr_])
        b.cp("dve", qaug[hb_][64:96, t * 128:(t + 1) * 128], o[64:96], [ptr_], [r_qb[hb_][t]])

    NSTB = 5
    LA = 4

    def emitS(h, ob, n, diag, slot):
        hb_ = h % 2
        psv, prs = pbank(1 + slot % NSTB)
        qc = slice(256 * ob, 256 * ob + 256)
        for half in range(2):
            kc = slice(n * 256 + half * 128, n * 256 + half * 128 + 128)
            if diag:
                b.mm(psv[:, half * 256:half * 256 + 256], kaug[hb_][0:64, kc], qaug[hb_][0:64, qc], True, True,
                     [r_kaug[hb_], r_qd[hb_]], [prs])
            else:
                b.mm(psv[:, half * 256:half * 256 + 256], kaug[hb_][0:96, kc], qaug[hb_][0:96, qc], True, True,
                     [r_kaug[hb_], r_k1h[hb_], r_qd[hb_], r_qb[hb_][2 * ob], r_qb[hb_][2 * ob + 1]], [prs])

    def emitPV(h, ob, n, diag, slot, first):
        hb_ = h % 2
        psv, prs = pbank(1 + slot % NSTB)
        pt = PTb[slot % 4]; rpt = r_PT[slot % 4]
        b.act(pt, psv, AF.Exp, [prs], [rpt], scale=0.125)
        pov, pors = pbank(6 + ob % 2)
        if diag:
            b.tt("pool", pt[:, 0:128], pt[:, 0:128], tri, ALU.mult, [rpt, r_tri], [rpt])
            b.tt("pool", pt[:, 384:512], pt[:, 384:512], tri, ALU.mult, [rpt, r_tri], [rpt])
        for j in range(2):
            for half in range(2):
                if diag and j == 0 and half == 1:
                    continue
                last = diag and (half == 1 or j == 0)
                b.mm(pov[:, j * 128:j * 128 + 65], pt[:, half * 256 + j * 128:half * 256 + j * 128 + 128],
                     vb[hb_][:, n * 2 + half, :], first and half == 0 and j == 0, last, [rpt, r_vb[hb_]], [pors])
        if diag:
            k_ = ob % 2
            po3 = pov[:, 0:256].rearrange("p (j x) -> p j x", x=128)
            b.recip(rinv[k_], po3[:, :, 64], [pors], [r_rinv[k_]])
            for j in range(2):
                b.ts("dve", att_tok[:, 2 * ob + j, h * 64:(h + 1) * 64], pov[:, j * 128:j * 128 + 64],
                     rinv[k_][:, j:j + 1], None, ALU.mult, None, [pors, r_rinv[k_]], [r_att])

    load_head(0)
    for t in range(32):
        k_ = route_stage1(0, t)
        route_stage2(0, t, k_)
    for h in range(8):
        if h + 1 < 8:
            load_head(h + 1)
        if h == 0:
            prefetch_C(c)
        items = []
        for ob in range(16):
            for n in range(q_block(ob)):
                items.append((ob, n, False, n == 0))
            items.append((ob, q_block(ob), True, False))
        pend = {}
        nxt_t = 0
        for i in range(len(items) + LA):
            if i < len(items):
                ob, n, diag, first = items[i]
                emitS(h, ob, n, diag, i)
            if i - LA >= 0:
                ob, n, diag, first = items[i - LA]
                emitPV(h, ob, n, diag, i - LA, first)
            if i % 12 == 6:
                next(conv, None)
            if h + 1 < 8:
                if i % 9 == 0 and nxt_t < 32:
                    pend[i + 5] = (nxt_t, route_stage1(h + 1, nxt_t))
                    nxt_t += 1
                if i in pend:
                    t_, k_ = pend.pop(i)
                    route_stage2(h + 1, t_, k_)
        for i in sorted(pend):
            t_, k_ = pend[i]
            route_stage2(h + 1, t_, k_)
        assert nxt_t == 32 or h == 7
    for _ in conv:
        pass
    P.dma("sp", att_scr.rearrange("(t p) c -> p t c", p=128), att_tok, reads=[r_att], writes=[r_att_scr])
    dump("att", att_tok, [128, 32, 512], [r_att], BF16)
    dump("qaug7", qaug[1], [128, 4096], [r_qd[1]] + r_qb[1], BF16)
    dump("kmeanT", kmeanT, [128, 8, 32], [r_km], BF16)
    P.barrier()
    P.release(mB)


def phase_CD(c):
    nc, P, b, T, R, dump = c["nc"], c["P"], c["b"], c["T"], c["R"], c["dump"]
    pbank, pbank_bf = c["pbank"], c["pbank_bf"]
    identb, r_identb, g1rep, r_g1 = c["identb"], c["r_identb"], c["g1rep"], c["r_g1"]
    x_all, w_in, y_out = c["x_all"], c["w_in"], c["y_out"]
    z_scr, att_scr, r_z_scr, r_att_scr = c["z_scr"], c["att_scr"], c["r_z_scr"], c["r_att_scr"]
    wgu_scr, wd_scr, r_wgu, r_wd = c["wgu_scr"], c["wd_scr"], c["r_wgu"], c["r_wd"]
    hs_scr, ys_scr = c["hs_scr"], c["ys_scr"]
    P.release(c["m_ssm"])
    NT = 32
    NSL = 96

    M1all = T([NT, 32], F32); M2all = T([NT, 32], F32); r_M = R("cMall")
    w12 = T([2, NT], F32); r_w12 = R("cw12")
    h2_scr = c["h2_scr"]
    h2s_stream = P.stream("h2s")
    r_h2s = []
    for t in range(NT):
        rj = R(f"ch2s{t}")
        rj.stream = h2s_stream
        r_h2s.append(rj)
    slot_i = T([2, NT], I32); r_slot = R("cslot")
    widx_i = T([NSL], I32); r_widx = R("cwidx")
    rconst = T([32 + NSL + 1], F32); r_rc = R("crconst")
    P.dma("sp", rconst, c["rconst_in"], writes=[r_rc])
    tri2 = T([128], BF16); r_tri2 = R("ctri2")
    P.dma("pool", tri2, c["tri_in"], writes=[r_tri2])
    ones = T([128], BF16); r_ones = R("cones")
    b.memset("dve", ones, 1.0, [r_ones])
    cw = c["cw"]
    r_hz, WinC, r_WinC, Wglu, Wps, Wpa, Wo, Wr, r_W, brow, g2rep = (cw[k] for k in
        ("r_hz", "WinC", "r_WinC", "Wglu", "Wps", "Wpa", "Wo", "Wr", "r_W", "brow", "g2rep"))
    mC = P.mark()

    xt = [T([D], F32) for _ in range(2)]; r_xt = [R("cxt0"), R("cxt1")]
    zt = [T([512], BF16) for _ in range(2)]; r_zt = [R("czt0"), R("czt1")]
    at = [T([512], BF16) for _ in range(2)]; r_at = [R("cat0"), R("cat1")]
    x1 = [T([D], F32) for _ in range(2)]; r_x1 = [R("cx1a"), R("cx1b")]
    r_y = [R(f"y_out{i}") for i in range(32)]
    y_stream = P.stream("ypark")
    for r_ in r_y:
        r_.stream = y_stream

    class PB:
        pass
    pbs = []
    for q in range(2):
        o = PB()
        o.hb = T([D], BF16); o.r_hb = R(f"chb{q}")
        o.st = T([8], F32); o.r_st = R(f"cst{q}")
        o.hTt = T([8, 128], BF16); o.r_hTt = R(f"chTt{q}")
        o.sgt = T([2048], BF16); o.r_sgt = R(f"csg{q}")
        o.zT = T([4, 128], BF16); o.r_zT = R(f"czT{q}")
        o.sgl = T([512], F32); o.r_sgl = R(f"csgl{q}")
        o.glu = T([512], BF16); o.r_glu = R(f"cglu{q}")
        o.gluT = T([4, 128], BF16); o.r_gluT = R(f"cgluT{q}")
        o.attT = T([4, 128], BF16); o.r_attT = R(f"cattT{q}")
        o.mg = T([D], BF16); o.r_mg = R(f"cmg{q}")
        o.mT = T([8, 128], BF16); o.r_mT = R(f"cmT{q}")
        o.h2 = T([D], BF16); o.r_h2 = R(f"ch2{q}")
        o.h2Tt = T([8, 128], BF16); o.r_h2Tt = R(f"ch2Tt{q}")
        o.lg = T([36], F32); o.rw = T([16], F32); o.gm = T([4], F32); o.em = T([4, 8], F32); o.mx = T([8], F32)
        o.r_rt = R(f"crt{q}")
        o.mm1 = T([512], F32); o.mm2 = T([512], F32); o.r_m1 = R(f"cm1{q}"); o.r_m2 = R(f"cm2{q}")
        o.bT, o.bM0, o.bM1, o.bX = (4 * q, 4 * q + 1, 4 * q + 2, 4 * q + 3)
        pbs.append(o)
    print("phase C arena", P.arena_off)
    assert P.arena_off <= c["cw_base"]

    def tile_src(base_ap, S, i):
        return base_ap[1024 * S:1024 * S + 1024].rearrange("(k j) d -> j k d", j=8)[i]

    def load_tile(tidx):
        S, i = tiles[tidx]
        par = tidx % 2
        P.dma("sp", xt[par], tile_src(x_all, 2 * S + 1, i), writes=[r_xt[par]])
        P.dma("sp", zt[par], z_scr[S][:, i * 512:(i + 1) * 512], reads=[r_z_scr[S]], writes=[r_zt[par]])
        P.dma("sp", at[par], tile_src(att_scr, S, i), reads=[r_att_scr], writes=[r_at[par]])

    def transposes(dst, r_dst, src, r_src, n, bank):
        ptv, ptr_ = pbank_bf(bank)
        for kc in range(n):
            b.tr(ptv[:, kc * 128:(kc + 1) * 128], src[:, kc * 128:(kc + 1) * 128], identb, [r_src, r_identb], [ptr_])
        b.cp("act", dst, ptv[:, 0:n * 128].rearrange("p (k t) -> p k t", t=128), [ptr_], [r_dst])

    tiles = [(S, i) for S in range(NST_OWN) for i in range(8)]

    def gen_C(par):
        o = pbs[par]
        for tidx in range(par, NT, 2):
            S, i = tiles[tidx]
            b.act(o.hb, xt[par], AF.Square, [r_xt[par]], [o.r_hb, o.r_st], accum=o.st[:, 0:1])
            b.act(o.st[:, 1:2], o.st[:, 0:1], AF.Sqrt, [o.r_st], [o.r_st], bias=EPS, scale=1.0 / D)
            b.recip(o.st[:, 1:2], o.st[:, 1:2], [o.r_st], [o.r_st])
            b.stt("dve", o.hb, xt[par], o.st[:, 1:2], g1rep, ALU.mult, ALU.mult, [r_xt[par], o.r_st, r_g1], [o.r_hb])
            yield
            transposes(o.hTt, o.r_hTt, o.hb, o.r_hb, 8, o.bT)
            yield
            for cc in range(4):
                psv, prs = pbank(o.bM0 if cc % 2 == 0 else o.bM1)
                for kc in range(8):
                    b.mm(psv, o.hTt[:, kc, :], WinC[:, kc, cc * 512:(cc + 1) * 512], kc == 0, kc == 7, [o.r_hTt, r_WinC], [prs])
                b.act(o.sgt[:, cc * 512:(cc + 1) * 512], psv, AF.Sigmoid, [prs], [o.r_sgt])
                yield
            transposes(o.zT, o.r_zT, zt[par], r_zt[par], 4, o.bX)
            yield
            psv, prs = pbank(o.bM0)
            for kc in range(4):
                b.mm(psv, o.zT[:, kc, :], Wglu[:, kc, :], kc == 0, False, [o.r_zT, r_W], [prs])
            b.mm(psv, ones[0:1, :], brow[0:1, 0:512], False, True, [r_W, r_ones], [prs])
            b.act(o.sgl, psv, AF.Sigmoid, [prs], [o.r_sgl])
            b.tt("pool", o.glu, zt[par], o.sgl, ALU.mult, [r_zt[par], o.r_sgl], [o.r_glu])
            transposes(o.attT, o.r_attT, at[par], r_at[par], 4, o.bT)
            yield
            transposes(o.gluT, o.r_gluT, o.glu, o.r_glu, 4, o.bX)
            yield
            for cc in range(2):
                ps1, pr1 = pbank(o.bM0)
                ps2, pr2 = pbank(o.bM1)
                for kc in range(4):
                    b.mm(ps2, o.attT[:, kc, :], Wpa[:, kc, cc * 512:(cc + 1) * 512], kc == 0, kc == 3, [o.r_attT, r_W], [pr2])
                for kc in range(4):
                    b.mm(ps1, o.gluT[:, kc, :], Wps[:, kc, cc * 512:(cc + 1) * 512], kc == 0, kc == 3, [o.r_gluT, r_W], [pr1])
                b.tt("dve", o.mm2, ps2, o.sgt[:, 1024 + cc * 512:1024 + (cc + 1) * 512], ALU.mult, [pr2, o.r_sgt], [o.r_m2])
                b.tt("dve", o.mm1, ps1, o.sgt[:, cc * 512:(cc + 1) * 512], ALU.mult, [pr1, o.r_sgt], [o.r_m1])
                b.tt("pool", o.mg[:, cc * 512:(cc + 1) * 512], o.mm1, o.mm2, ALU.add, [o.r_m1, o.r_m2], [o.r_mg])
                yield
            transposes(o.mT, o.r_mT, o.mg, o.r_mg, 8, o.bT)
            yield
            for cc in range(2):
                psv, prs = pbank(o.bM0 if cc == 0 else o.bM1)
                for kc in range(8):
                    b.mm(psv, o.mT[:, kc, :], Wo[:, kc, cc * 512:(cc + 1) * 512], kc == 0, kc == 7, [o.r_mT, r_W], [prs])
                b.tt("dve", x1[par][:, cc * 512:(cc + 1) * 512], psv, xt[par][:, cc * 512:(cc + 1) * 512], ALU.add,
                     [prs, r_xt[par]], [r_x1[par]])
            P.dma("sp", tile_src(y_out, S, i), x1[par], reads=[r_x1[par]], writes=[r_y[tidx]])
            if tidx + 2 < NT:
                load_tile(tidx + 2)
            yield
            h2 = o.h2; r_h2 = o.r_h2
            b.act(h2, x1[par], AF.Square, [r_x1[par]], [r_h2, o.r_st], accum=o.st[:, 2:3])
            b.act(o.st[:, 3:4], o.st[:, 2:3], AF.Sqrt, [o.r_st], [o.r_st], bias=EPS, scale=1.0 / D)
            b.recip(o.st[:, 3:4], o.st[:, 3:4], [o.r_st], [o.r_st])
            b.stt("dve", h2, x1[par], o.st[:, 3:4], g2rep, ALU.mult, ALU.mult, [r_x1[par], o.r_st, r_W], [r_h2])
            P.dma("sp", h2_scr[tidx * 128:(tidx + 1) * 128, :], h2, reads=[r_h2], writes=[r_h2s[tidx]])
            yield
            transposes(o.h2Tt, o.r_h2Tt, h2, r_h2, 8, o.bX)
            yield
            psv, prs = pbank(o.bM0)
            for kc in range(8):
                b.mm(psv[:, 0:36], o.h2Tt[:, kc, :], Wr[:, kc, :], kc == 0, False, [o.r_h2Tt, r_W], [prs])
            b.mm(psv[:, 0:36], ones[0:1, :], brow[0:1, 512:548], False, True, [r_W, r_ones], [prs])
            rt = [o.r_rt]
            lg, rw, gm, em, mx = o.lg, o.rw, o.gm, o.em, o.mx
            b.cp("dve", lg, psv[:, 0:36], [prs], rt)
            b.reduce(rw[:, 0:1], lg[:, 0:4], ALU.max, rt, rt)
            b.ts("dve", rw[:, 1:2], rw[:, 0:1], -1.0, None, ALU.mult, None, rt, rt)
            b.act(gm, lg[:, 0:4], AF.Exp, rt, rt, bias=rw[:, 1:2], scale=1.0, accum=rw[:, 2:3])
            b.recip(rw[:, 3:4], rw[:, 2:3], rt, rt)
            yield
            b.ts("dve", gm, lg[:, 0:4], rw[:, 0:1], None, ALU.is_ge, None, rt, rt)
            b.ts("dve", gm, gm, 10000.0, -10000.0, ALU.mult, ALU.add, rt, rt)
            b.tt("dve", em, lg[:, 4:36].rearrange("p (g e) -> p g e", e=8), bc(gm.unsqueeze(2), [128, 4, 8]), ALU.add, rt, rt)
            P.op("dve", (lambda mx=mx, em=em: (lambda e: e.max(out=mx, in_=em.rearrange("p g e -> p (g e)"))))(), rt, rt)
            b.tt("dve", rw[:, 4:5], mx[:, 0:1], mx[:, 1:2], ALU.subtract, rt, rt)
            b.act(rw[:, 5:6], rw[:, 4:5], AF.Sigmoid, rt, rt)
            yield
            b.tt("dve", w12[:, 0, tidx:tidx + 1], rw[:, 5:6], rw[:, 3:4], ALU.mult, rt, [r_w12])
            b.tt("dve", w12[:, 1, tidx:tidx + 1], rw[:, 3:4], w12[:, 0, tidx:tidx + 1], ALU.subtract, rt + [r_w12], [r_w12])
            emf = em.rearrange("p g e -> p (g e)")
            b.ts("dve", M1all[:, tidx, :], emf, mx[:, 0:1], None, ALU.is_equal, None, rt, [r_M])
            b.ts("dve", M2all[:, tidx, :], emf, mx[:, 1:2], None, ALU.is_equal, None, rt, [r_M])
            yield

    load_tile(0)
    load_tile(1)
    g0 = gen_C(0)
    g1 = gen_C(1)
    for _ in range(8):
        next(g0)
    run_interleaved(g0, g1)
    P.barrier()
    P.release(mC)

    rr = R("cR"); rt = [rr]
    M1f = M1all.rearrange("p t e -> p (t e)"); M2f = M2all.rearrange("p t e -> p (t e)")
    Mf = T([1024], F32); Mb = T([1024], BF16); r_Mb = R("cMb")
    cumS = T([1024], F32); totS = T([1024], F32); r_cum = R("ccum"); r_tot = R("ctot")
    b.tt("dve", Mf, M1f, M2f, ALU.add, [r_M], rt)
    b.cp("dve", Mb, Mf, rt, [r_Mb])
    for hf in range(2):
        sl = slice(hf * 512, hf * 512 + 512)
        pv, prs = pbank(hf)
        b.mm(pv, tri2, Mb[:, sl], True, True, [r_tri2, r_Mb], [prs])
        b.cp("act", cumS[:, sl], pv, [prs], [r_cum])
        pv2, prs2 = pbank(2 + hf)
        b.mm(pv2, ones, Mb[:, sl], True, True, [r_ones, r_Mb], [prs2])
        b.cp("dve", totS[:, sl], pv2, [prs2], [r_tot])
    thr = rconst[:, 0:32]; jv = rconst[:, 32:32 + NSL]; pidx = rconst[:, 32 + NSL:33 + NSL]
    cnt_e = T([32], F32); ntile = T([32], F32); base = T([32], F32)
    cmp3 = T([NSL, 32], F32)
    pbuf = [T([1024], F32) for _ in range(2)]
    sbuf_ = [T([32], F32) for _ in range(2)]

    def prefix(src, n, unit, bufs):
        cur = src; s = 1; k = 0
        L = n * unit
        while s < n:
            dst = bufs[k % 2]; k += 1
            sh = s * unit
            b.cp("dve", dst[:, 0:sh], cur[:, 0:sh], rt + [r_tot], rt)
            b.tt("dve", dst[:, sh:L], cur[:, sh:L], cur[:, 0:L - sh], ALU.add, rt + [r_tot], rt)
            cur = dst; s *= 2
        return cur

    b.reduce(cnt_e, totS.rearrange("p (t e) -> p e t", e=32), ALU.add, [r_tot], rt)
    c3 = cmp3[:, 0:32, :]
    b.tt("dve", c3, bc(cnt_e.unsqueeze(2), [128, 32, 32]), bc(thr.unsqueeze(1), [128, 32, 32]), ALU.is_gt, rt + [r_rc], rt)
    b.reduce(ntile, c3, ALU.add, rt, rt)
    cti = prefix(ntile, 32, 1, sbuf_)
    b.tt("dve", base, cti, ntile, ALU.subtract, rt, rt)
    b.ts("dve", base, base, 128.0, None, ALU.mult, None, rt, rt)
    inc_t = prefix(totS, 32, 32, pbuf)
    G = T([1024], F32)
    b.tt("dve", G, inc_t, totS, ALU.subtract, rt + [r_tot], rt)
    G3 = G.rearrange("p (t e) -> p t e", e=32)
    b.tt("dve", G3, G3, bc(base.unsqueeze(1), [128, 32, 32]), ALU.add, rt, rt)
    b.tt("dve", G, G, cumS, ALU.add, rt + [r_cum], rt)
    sl_f = T([2, NT], F32)
    q1 = T([1024], F32)
    for k, Mk in ((0, M1f), (1, M2f)):
        b.tt("dve", q1, G, Mk, ALU.mult, rt + [r_M], rt)
        b.reduce(sl_f[:, k, :], q1.rearrange("p (t e) -> p t e", e=32), ALU.add, rt, rt)
    b.ts("dve", sl_f, sl_f, -1.0, float(NSL * 128 - 1), ALU.add, ALU.min, rt, rt)
    b.ts("dve", sl_f, sl_f, 0.0, None, ALU.max, None, rt, rt)
    b.cp("dve", slot_i, sl_f, rt, [r_slot])
    b.tt("dve", cmp3, bc(cti.unsqueeze(1), [128, NSL, 32]), bc(jv.unsqueeze(2), [128, NSL, 32]), ALU.is_le, rt + [r_rc], rt)
    eid = T([NSL], F32)
    b.reduce(eid, cmp3, ALU.add, rt, rt)
    b.ts("dve", eid, eid, 31.0, 128.0, ALU.min, ALU.mult, rt, rt)
    b.tt("dve", eid, eid, bc(pidx, [128, NSL]), ALU.add, rt + [r_rc], rt)
    b.cp("dve", widx_i, eid, rt, [r_widx])
    dump("slot_i", slot_i, [128, 2, NT], [r_slot], I32)
    dump("widx_i", widx_i, [128, NSL], [r_widx], I32)
    dump("w12", w12, [128, 2, NT], [r_w12])
    dump("M1all", M1all, [128, NT, 32], [r_M])
    dump("M2all", M2all, [128, NT, 32], [r_M])

    sc_stream = P.stream("sc")
    r_sc = []
    h2l = [T([D], BF16) for _ in range(8)]; r_h2l = [R(f"ch2l{i}") for i in range(8)]
    for t in range(NT):
        P.dma("sp", h2l[t % 8], h2_scr[t * 128:(t + 1) * 128, :], reads=r_h2s, writes=[r_h2l[t % 8]])
        for k in range(2):
            rj = R(f"csc{t}_{k}")
            rj.stream = sc_stream
            r_sc.append(rj)
            P.idma(hs_scr[:, :], h2l[t % 8], slot_i[:, k, t:t + 1], False,
                   reads=[r_h2l[t % 8], r_slot] + r_hz, writes=[rj])

    NB = 6
    PF = 4
    hs = [T([D], BF16) for _ in range(NB)]; r_hs = [R(f"dhs{i}") for i in range(NB)]
    wgu = [T([2, 8, 128], BF16) for _ in range(NB)]; r_wgs = [R(f"dwgu{i}") for i in range(NB)]
    wdn = [T([D], BF16) for _ in range(NB)]; r_wds = [R(f"dwdn{i}") for i in range(NB)]
    hsT = [T([8, 128], BF16) for _ in range(2)]; r_hsT = [R("dhsT0"), R("dhsT1")]
    sg = [T([128], F32) for _ in range(2)]; r_sg = [R("dsg0"), R("dsg1")]
    actb = [T([128], BF16) for _ in range(2)]; r_actb = [R("dact0"), R("dact1")]
    yt = [T([D], F32) for _ in range(2)]; r_yt = [R("dyt0"), R("dyt1")]
    ys_stream = P.stream("ys")
    r_ys = []
    wgu2d = wgu_scr.rearrange("e p g k f -> (e p) (g k f)")
    wd2d = wd_scr.rearrange("e p d -> (e p) d")
    print("phase D arena", P.arena_off)

    def d_load(j):
        s4 = j % NB
        P.dma("sp", hs[s4], hs_scr[j * 128:(j + 1) * 128, :], reads=r_sc, writes=[r_hs[s4]])
        P.idma(wgu[s4].rearrange("p g k f -> p (g k f)"), wgu2d, widx_i[:, j:j + 1], True,
               reads=[r_widx] + list(set(r_wgu)), writes=[r_wgs[s4]])
        P.idma(wdn[s4], wd2d, widx_i[:, j:j + 1], True, reads=[r_widx] + list(set(r_wd)), writes=[r_wds[s4]])

    def d_T(j):
        s4 = j % NB; s2 = j % 2
        ptv, ptr_ = pbank_bf(s2)
        for kc in range(8):
            b.tr(ptv[:, kc * 128:(kc + 1) * 128], hs[s4][:, kc * 128:(kc + 1) * 128], identb, [r_hs[s4], r_identb], [ptr_])
        b.cp("act", hsT[s2], ptv.rearrange("p (k t) -> p k t", t=128), [ptr_], [r_hsT[s2]])

    def d_GU(j):
        s4 = j % NB; s2 = j % 2
        psv, prs = pbank(2 + s2)
        for g in range(2):
            for kc in range(8):
                b.mm(psv[:, g * 128:(g + 1) * 128], wgu[s4][:, g, kc, :], hsT[s2][:, kc, :], kc == 0, kc == 7,
                     [r_wgs[s4], r_hsT[s2]], [prs])
        b.act(sg[s2], psv[:, 0:128], AF.Silu, [prs], [r_sg[s2]])
        b.tt("dve", actb[s2], psv[:, 128:256], sg[s2], ALU.mult, [prs, r_sg[s2]], [r_actb[s2]])

    def d_DN(j):
        s4 = j % NB; s2 = j % 2
        for cc in range(2):
            psv, prs = pbank(4 + 2 * s2 + cc)
            b.mm(psv, actb[s2], wdn[s4][:, cc * 512:(cc + 1) * 512], True, True, [r_actb[s2], r_wds[s4]], [prs])
            if cc == 0:
                b.cp("act", yt[s2][:, 0:512], psv, [prs], [r_yt[s2]])
            else:
                b.cp("dve", yt[s2][:, 512:1024], psv, [prs], [r_yt[s2]])
        rj = R(f"dys{j}")
        rj.stream = ys_stream
        r_ys.append(rj)
        P.dma("sp", ys_scr[j * 128:(j + 1) * 128, :], yt[s2], reads=[r_yt[s2]], writes=[rj])

    for j in range(PF):
        d_load(j)
    for it in range(NSL + 2):
        if it < NSL:
            d_T(it)
        if 0 <= it - 1 < NSL:
            d_GU(it - 1)
        if 0 <= it - 2 < NSL:
            d_DN(it - 2)
        if it + PF < NSL:
            d_load(it + PF)

    xb = [T([D], F32) for _ in range(4)]; r_xb = [R(f"exb{i}") for i in range(4)]
    ya = [T([D], F32) for _ in range(4)]; r_ya = [R(f"eya{i}") for i in range(4)]
    yb = [T([D], F32) for _ in range(4)]; r_yb = [R(f"eyb{i}") for i in range(4)]
    print("phase E arena", P.arena_off)

    def e_load(t):
        S, i = tiles[t]
        s2 = t % 4
        P.dma("sp", xb[s2], tile_src(y_out, S, i), reads=[r_y[t]], writes=[r_xb[s2]])
        P.idma(ya[s2], ys_scr[:, :], slot_i[:, 0, t:t + 1], True, reads=[r_slot] + r_ys, writes=[r_ya[s2]])
        P.idma(yb[s2], ys_scr[:, :], slot_i[:, 1, t:t + 1], True, reads=[r_slot] + r_ys, writes=[r_yb[s2]])

    e_load(0)
    e_load(1)
    e_load(2)
    for t in range(NT):
        S, i = tiles[t]
        s2 = t % 4
        if t + 3 < NT:
            e_load(t + 3)
        b.stt("dve", ya[s2], ya[s2], w12[:, 0, t:t + 1], xb[s2], ALU.mult, ALU.add, [r_ya[s2], r_xb[s2], r_w12], [r_ya[s2]])
        b.stt("dve", ya[s2], yb[s2], w12[:, 1, t:t + 1], ya[s2], ALU.mult, ALU.add, [r_ya[s2], r_yb[s2], r_w12], [r_ya[s2]])
        P.dma("sp", tile_src(y_out, S, i), ya[s2], reads=[r_ya[s2]], writes=[r_y[t]])
    P.barrier()

def _const_tables(p):
    f32 = np.float32
    half = 8
    inv_freq = (np.float32(500000.0) ** (-(np.arange(half, dtype=f32) * f32(2.0) / f32(16.0)))).astype(f32)
    pos = (np.arange(8192) - (0 if p == 1 else 1024)).astype(f32)
    ang = (pos[:, None] * inv_freq[None, :]).astype(f32)
    cs = np.concatenate([np.cos(ang), np.sin(ang)], axis=1).astype(f32)
    cs_tab = cs.reshape(64, 128, 16).transpose(1, 0, 2).reshape(128, 64 * 16)
    pb = np.zeros((16, 32), f32)
    for ob in range(16):
        pb[ob, q_block(ob):] = NEG
        if p == 0:
            pb[ob, :4] = NEG
    pbrep = np.broadcast_to(pb.reshape(1, 512), (128, 512)).copy()
    onehot = np.zeros((32, 8192), f32)
    for n in range(32):
        onehot[n, n * 256:(n + 1) * 256] = 1.0
    tri = (np.arange(128)[:, None] <= np.arange(128)[None, :]).astype(f32)
    rconst = np.zeros((128, 32 + 96 + 1), f32)
    rconst[:, 0:32] = 128.0 * np.arange(32, dtype=f32)[None, :]
    rconst[:, 32:128] = np.arange(96, dtype=f32)[None, :]
    rconst[:, 128] = np.arange(128, dtype=f32)
    return dict(cs_tab=np.ascontiguousarray(cs_tab), pbrep=pbrep, onehot_k=onehot, tri=tri,
                rconst=rconst, ident_in=np.eye(128, dtype=f32))


def make_in_maps(inputs):
    f32 = np.float32
    x = np.asarray(inputs["x"], f32)
    g = lambda k: np.ascontiguousarray(np.asarray(inputs[k], f32)[0])
    shared = dict(
        w_in=g("w_in"), norm1_g=g("norm1_g").reshape(1, D),
        lamre_t=np.ascontiguousarray(g("lam_re").T), lamim_t=np.ascontiguousarray(g("lam_im").T),
        log_dt=g("log_dt").reshape(1, 32),
        b_re_t=np.ascontiguousarray(g("ssm_b_re").transpose(1, 0, 2)),
        b_im_t=np.ascontiguousarray(g("ssm_b_im").transpose(1, 0, 2)),
        c_re_t=np.ascontiguousarray(g("ssm_c_re").transpose(2, 0, 1)),
        c_im_t=np.ascontiguousarray(g("ssm_c_im").transpose(2, 0, 1)),
        ssm_d=g("ssm_d").reshape(1, 512), w_glu=g("w_glu"), b_glu=g("b_glu").reshape(1, 512),
        q_norm_g=g("q_norm_g").reshape(1, 64), k_norm_g=g("k_norm_g").reshape(1, 64),
        w_proj_ssm=g("w_proj_ssm"), w_proj_attn=g("w_proj_attn"), w_out=g("w_out"),
        norm2_g=g("norm2_g").reshape(1, D),
        w_router=np.ascontiguousarray(np.concatenate([g("w_router_group"), g("w_router_expert")], axis=1)),
        b_router=np.concatenate([g("b_router_group"), g("b_router_expert")]).reshape(1, 36),
        w_gate=g("w_gate"), w_up=g("w_up"), w_down=g("w_down"),
    )
    tabs = [_const_tables(0), _const_tables(1)]
    zeros = np.zeros((1024, D), f32)
    in_maps = []
    for c in range(NCORES):
        bi, p = c // 2, c % 2
        m = dict(shared)
        m.update(tabs[p])
        m["x_all"] = np.ascontiguousarray(x[bi]) if p == 1 else np.concatenate([zeros, x[bi, 0:7168]], axis=0)
        in_maps.append(m)
    return in_maps


_CACHE = {}


def kernel(**inputs):
    in_maps = make_in_maps(inputs)
    if "nc" not in _CACHE:
        _CACHE["nc"] = build()[0]
    nc = _CACHE["nc"]
    res = run_bass_kernel_spmd(nc, in_maps, core_ids=list(range(NCORES)))
    x = np.asarray(inputs["x"])
    out = np.empty(x.shape, np.float32)
    for c in range(NCORES):
        bi, p = c // 2, c % 2
        y = res.results[c]["y_out"]
        for S in range(NST_OWN):
            g0 = 1024 * (2 * S + p)
            out[bi, g0:g0 + 1024] = y[1024 * S:1024 * S + 1024]
    return out
```
